# Optimizing a Trainium2 kernel written in Bass

```python
import jax
import jax.numpy as jnp
from jax import lax
import numpy as np

D_MODEL = 1024
BATCH = 8
SEQ = 2048
DEPTH = 1

CHUNK = 64
ATT_HEADS = 8
ATT_HEAD_DIM = 64
ATT_WIDTH = ATT_HEADS * ATT_HEAD_DIM
LEFT_CHUNKS = 8
BAND_CHUNKS = LEFT_CHUNKS + 1
BAND = BAND_CHUNKS * CHUNK
REL_CLIP = 64
N_REL = 2 * REL_CLIP + 1
RWKV_HEADS = 8
RWKV_HEAD_DIM = 64
RWKV_WIDTH = RWKV_HEADS * RWKV_HEAD_DIM
DECAY_LORA = 64
AAA_LORA = 64
GATE_LORA = 128
GN_EPS = 64e-5
ATT_PROJ = 3 * ATT_WIDTH
RWKV_PROJ = 3 * RWKV_WIDTH + DECAY_LORA + AAA_LORA + GATE_LORA
D_IN = ATT_PROJ + RWKV_PROJ + 2 * D_MODEL
N_GROUPS = 4
EXPERTS_PER_GROUP = 8
N_EXPERTS = N_GROUPS * EXPERTS_PER_GROUP
TOP_K = 2
D_EXPERT = 256

RMS_EPS = 1e-6

kernel_name = 'hybrid_chunk_attn_rwkv7_hier_moe'


def rmsnorm(x, g):
    xf = x.astype(jnp.float32)
    y = xf * lax.rsqrt(jnp.mean(xf * xf, axis=-1, keepdims=True) + RMS_EPS)
    return (y * g.astype(jnp.float32)).astype(x.dtype)


def token_shift(p, mu):
    prev = jnp.pad(p[:, :-1], ((0, 0), (1, 0), (0, 0)))
    return p + (prev - p) * mu


def chunk_band_attention(q, k, v, rel_table):
    bsz, seq, _ = q.shape
    nc = seq // CHUNK
    shp = (bsz, nc, CHUNK, ATT_HEADS, ATT_HEAD_DIM)
    qc = q.reshape(shp) * (ATT_HEAD_DIM ** -0.5)
    pad = ((0, 0), (LEFT_CHUNKS, 0), (0, 0), (0, 0), (0, 0))
    kp = jnp.pad(k.reshape(shp), pad)
    vp = jnp.pad(v.reshape(shp), pad)
    band_idx = jnp.arange(nc)[:, None] + jnp.arange(BAND_CHUNKS)[None, :]
    kb = kp[:, band_idx].reshape(bsz, nc, BAND, ATT_HEADS, ATT_HEAD_DIM)
    vb = vp[:, band_idx].reshape(bsz, nc, BAND, ATT_HEADS, ATT_HEAD_DIM)
    scores = jnp.einsum('bcqhd,bckhd->bhcqk', qc, kb).astype(jnp.float32)
    qpos = jnp.arange(CHUNK)
    kpos = jnp.arange(BAND)
    rel = LEFT_CHUNKS * CHUNK + qpos[:, None] - kpos[None, :]
    rel_idx = jnp.clip(rel, -REL_CLIP, REL_CLIP) + REL_CLIP
    bias = rel_table.astype(jnp.float32)[:, rel_idx]
    valid = (jnp.arange(nc)[:, None] - LEFT_CHUNKS + kpos[None, :] // CHUNK) >= 0
    scores = jnp.where(valid[None, None, :, None, :], scores + bias[None, :, None],
                       jnp.finfo(jnp.float32).min)
    probs = jax.nn.softmax(scores, axis=-1).astype(v.dtype)
    out = jnp.einsum('bhcqk,bckhd->bcqhd', probs, vb)
    return out.reshape(bsz, seq, ATT_WIDTH)


def rwkv7_step(state, inp):
    r_t, w_t, k_t, v_t, kk_t, a_t = inp
    sa = jnp.einsum('bhvk,bhk->bhv', state, -kk_t)
    state = (state * w_t[:, :, None, :]
             + sa[..., None] * (kk_t * a_t)[:, :, None, :]
             + v_t[..., None] * k_t[:, :, None, :])
    y_t = jnp.einsum('bhvk,bhk->bhv', state, r_t)
    return state, y_t


def rwkv7_time_mix(r, k, v, w_lo, a_lo, g_lo, w0, w2, a0, a2, g2, k_k, k_a, r_k, gn_g, gn_b):
    out_dtype = r.dtype
    f32 = jnp.float32
    r, k, v, w_lo, a_lo, g_lo = [t.astype(f32) for t in (r, k, v, w_lo, a_lo, g_lo)]
    bsz, seq, _ = r.shape
    log_w = -jax.nn.softplus(-(w0 + jnp.tanh(w_lo) @ w2)) - 0.5
    decay = jnp.exp(-jnp.exp(log_w))
    a = jax.nn.sigmoid(a0 + a_lo @ a2)
    g = jax.nn.sigmoid(g_lo) @ g2
    heads = lambda t: t.reshape(bsz, seq, RWKV_HEADS, RWKV_HEAD_DIM)
    kk = heads(k * k_k)
    kk = kk / jnp.maximum(jnp.sqrt(jnp.sum(kk * kk, axis=-1, keepdims=True)), 1e-12)
    k = k * (1.0 + (a - 1.0) * k_a)
    rh, wh, kh, vh, ah = [heads(t) for t in (r, decay, k, v, a)]
    xs = tuple(jnp.moveaxis(t, 1, 0) for t in (rh, wh, kh, vh, kk, ah))
    state0 = jnp.zeros((bsz, RWKV_HEADS, RWKV_HEAD_DIM, RWKV_HEAD_DIM), f32)
    _, y = lax.scan(rwkv7_step, state0, xs)
    y = jnp.moveaxis(y, 0, 1)
    mean = jnp.mean(y, axis=-1, keepdims=True)
    var = jnp.mean(jnp.square(y - mean), axis=-1, keepdims=True)
    y = ((y - mean) * lax.rsqrt(var + GN_EPS)).reshape(bsz, seq, RWKV_WIDTH) * gn_g + gn_b
    bonus = jnp.sum(rh * kh * r_k, axis=-1, keepdims=True) * vh
    y = (y + bonus.reshape(bsz, seq, RWKV_WIDTH)) * g
    return y.astype(out_dtype)


def hierarchical_moe(h, rg_w, rg_b, re_w, re_b, w_gate, w_up, w_down):
    bsz, seq, d = h.shape
    f32 = jnp.float32
    hf = h.reshape(bsz * seq, d)
    group_logits = (hf @ rg_w).astype(f32) + rg_b.astype(f32)
    group_prob = jax.nn.softmax(group_logits, axis=-1)
    g_prob, g_idx = lax.top_k(group_prob, 1)
    expert_logits = ((hf @ re_w).astype(f32) + re_b.astype(f32)).reshape(-1, N_GROUPS, EXPERTS_PER_GROUP)
    in_group = jnp.take_along_axis(expert_logits, g_idx[:, :, None], axis=1)[:, 0]
    e_logit, e_idx = lax.top_k(in_group, TOP_K)
    gate = jax.nn.softmax(e_logit, axis=-1) * g_prob
    flat_idx = g_idx * EXPERTS_PER_GROUP + e_idx
    combine = jnp.einsum('nk,nke->ne', gate, jax.nn.one_hot(flat_idx, N_EXPERTS, dtype=f32)).astype(h.dtype)
    y = jnp.zeros_like(hf)
    for grp in range(N_GROUPS):
        sl = slice(grp * EXPERTS_PER_GROUP, (grp + 1) * EXPERTS_PER_GROUP)
        hg = jnp.einsum('nd,edf->nef', hf, w_gate[sl])
        hu = jnp.einsum('nd,edf->nef', hf, w_up[sl])
        act = jax.nn.silu(hg) * hu * combine[:, sl, None]
        y = y + jnp.einsum('nef,efd->nd', act, w_down[sl])
    return y.reshape(bsz, seq, d)


def setup_inputs(seed: int = 0) -> dict:
    key = jax.random.key(seed)
    ks = jax.random.split(key, 27)
    f32 = jnp.float32
    nrm = lambda k, shape, scale: scale * jax.random.normal(k, shape, f32)
    return {
        'x': nrm(ks[0], (BATCH, SEQ, D_MODEL), 1.0),
        'ln_mix_g': 1.0 + nrm(ks[1], (DEPTH, D_MODEL), 0.02),
        'w_in': nrm(ks[2], (DEPTH, D_MODEL, D_IN), D_MODEL ** -0.5),
        'att_rel_bias': nrm(ks[3], (DEPTH, ATT_HEADS, N_REL), 0.1),
        'rwkv_mu': jax.random.uniform(ks[4], (DEPTH, RWKV_PROJ), f32),
        'rwkv_w0': jax.random.uniform(ks[5], (DEPTH, RWKV_WIDTH), f32, -6.5, -1.5),
        'rwkv_w2': nrm(ks[6], (DEPTH, DECAY_LORA, RWKV_WIDTH), 0.1 * DECAY_LORA ** -0.5),
        'rwkv_a0': nrm(ks[7], (DEPTH, RWKV_WIDTH), 0.1),
        'rwkv_a2': nrm(ks[8], (DEPTH, AAA_LORA, RWKV_WIDTH), 0.1 * AAA_LORA ** -0.5),
        'rwkv_g2': nrm(ks[9], (DEPTH, GATE_LORA, RWKV_WIDTH), GATE_LORA ** -0.5),
        'rwkv_k_k': 0.85 + nrm(ks[10], (DEPTH, RWKV_WIDTH), 0.05),
        'rwkv_k_a': 1.0 + nrm(ks[11], (DEPTH, RWKV_WIDTH), 0.05),
        'rwkv_r_k': nrm(ks[12], (DEPTH, RWKV_HEADS, RWKV_HEAD_DIM), 0.1),
        'rwkv_gn_g': 1.0 + nrm(ks[13], (DEPTH, RWKV_WIDTH), 0.02),
        'rwkv_gn_b': nrm(ks[14], (DEPTH, RWKV_WIDTH), 0.02),
        'w_branch_att': nrm(ks[15], (DEPTH, ATT_WIDTH, D_MODEL), ATT_WIDTH ** -0.5),
        'w_branch_rwkv': nrm(ks[16], (DEPTH, RWKV_WIDTH, D_MODEL), RWKV_WIDTH ** -0.5),
        'w_out': nrm(ks[17], (DEPTH, D_MODEL, D_MODEL), D_MODEL ** -0.5),
        'ln_ffn_g': 1.0 + nrm(ks[18], (DEPTH, D_MODEL), 0.02),
        'router_group_w': nrm(ks[19], (DEPTH, D_MODEL, N_GROUPS), D_MODEL ** -0.5),
        'router_group_b': nrm(ks[20], (DEPTH, N_GROUPS), 0.01),
        'router_expert_w': nrm(ks[21], (DEPTH, D_MODEL, N_EXPERTS), D_MODEL ** -0.5),
        'router_expert_b': nrm(ks[22], (DEPTH, N_EXPERTS), 0.01),
        'expert_w_gate': nrm(ks[23], (DEPTH, N_EXPERTS, D_MODEL, D_EXPERT), D_MODEL ** -0.5),
        'expert_w_up': nrm(ks[24], (DEPTH, N_EXPERTS, D_MODEL, D_EXPERT), D_MODEL ** -0.5),
        'expert_w_down': nrm(ks[25], (DEPTH, N_EXPERTS, D_EXPERT, D_MODEL), D_EXPERT ** -0.5),
        'ln_final_g': 1.0 + nrm(ks[26], (D_MODEL,), 0.02),
    }


def reference(x, ln_mix_g, w_in, att_rel_bias, rwkv_mu, rwkv_w0, rwkv_w2, rwkv_a0, rwkv_a2,
              rwkv_g2, rwkv_k_k, rwkv_k_a, rwkv_r_k, rwkv_gn_g, rwkv_gn_b, w_branch_att,
              w_branch_rwkv, w_out, ln_ffn_g, router_group_w, router_group_b, router_expert_w,
              router_expert_b, expert_w_gate, expert_w_up, expert_w_down, ln_final_g):
    W = RWKV_WIDTH
    o_w = 3 * W
    o_a = o_w + DECAY_LORA
    o_g = o_a + AAA_LORA
    for l in range(DEPTH):
        h = rmsnorm(x, ln_mix_g[l])
        proj = h @ w_in[l]
        q = proj[..., :ATT_WIDTH]
        k = proj[..., ATT_WIDTH:2 * ATT_WIDTH]
        v = proj[..., 2 * ATT_WIDTH:ATT_PROJ]
        rw = token_shift(proj[..., ATT_PROJ:ATT_PROJ + RWKV_PROJ], rwkv_mu[l])
        gates = jax.nn.sigmoid(proj[..., ATT_PROJ + RWKV_PROJ:])
        att = chunk_band_attention(q, k, v, att_rel_bias[l])
        rwkv = rwkv7_time_mix(rw[..., :W], rw[..., W:2 * W], rw[..., 2 * W:3 * W],
                              rw[..., o_w:o_a], rw[..., o_a:o_g], rw[..., o_g:],
                              rwkv_w0[l], rwkv_w2[l], rwkv_a0[l], rwkv_a2[l], rwkv_g2[l],
                              rwkv_k_k[l], rwkv_k_a[l], rwkv_r_k[l], rwkv_gn_g[l], rwkv_gn_b[l])
        merged = (gates[..., :D_MODEL] * (att @ w_branch_att[l])
                  + gates[..., D_MODEL:] * (rwkv @ w_branch_rwkv[l]))
        x = x + merged @ w_out[l]
        h = rmsnorm(x, ln_ffn_g[l])
        x = x + hierarchical_moe(h, router_group_w[l], router_group_b[l], router_expert_w[l],
                                 router_expert_b[l], expert_w_gate[l], expert_w_up[l],
                                 expert_w_down[l])
    return rmsnorm(x, ln_final_g)
```

```python
import math
import os
from contextlib import ExitStack

import numpy as np
import concourse.bass as bass
import concourse.mybir as mybir
from concourse.bass_utils import run_bass_kernel_spmd

F32 = mybir.dt.float32
BF16 = mybir.dt.bfloat16
AF = mybir.ActivationFunctionType
ALU = mybir.AluOpType
AX = mybir.AxisListType

T = 2048
NT = 16
D = 1024
KC = 8
DIN = 5376
CDEC = math.exp(-0.5)
NEG = -1.0e30
DEBUG = bool(os.environ.get("MK_DEBUG"))
STOP = os.environ.get("MK_STOP", "")
PUMP = int(os.environ.get("MK_PUMP", "6"))
NGRP = int(os.environ.get("MK_GRP", "2"))


class _Stop(Exception):
    pass


class Trk:
    __slots__ = ("w", "r")

    def __init__(self):
        self.w = {}
        self.r = {}


class K:
    def __init__(self, nc, es):
        self.nc = nc
        self.es = es
        self.E = dict(pe=nc.tensor, act=nc.scalar, dve=nc.vector, pool=nc.gpsimd, sp=nc.sync)
        self.sems = {}
        self.cnt = {}
        self.seen = {e: {} for e in self.E}
        for e in ("pe", "act", "dve", "pool"):
            self.newsem(e)

    def newsem(self, key):
        if key not in self.sems:
            self.sems[key] = self.es.enter_context(self.nc.semaphore("s_" + key))
            self.cnt[key] = 0
        return key

    def _waits(self, eng, reads, writes):
        need = {}
        for t in reads:
            for k, v in t.w.items():
                if need.get(k, 0) < v:
                    need[k] = v
        for t in writes:
            for d in (t.w, t.r):
                for k, v in d.items():
                    if need.get(k, 0) < v:
                        need[k] = v
        e = self.E[eng]
        seen = self.seen[eng]
        if eng == "pe":
            need.pop("pe", None)
        for k, v in need.items():
            if seen.get(k, 0) < v:
                e.wait_ge(self.sems[k], v)
                seen[k] = v
        return e

    def op(self, eng, fn, reads=(), writes=(), inc=True):
        e = self._waits(eng, reads, writes)
        inst = fn(e)
        if inc:
            self.cnt[eng] += 1
            inst.then_inc(self.sems[eng], 1)
            tk = self.cnt[eng]
        else:
            tk = self.cnt[eng] + 1
        for t in reads:
            if t.r.get(eng, 0) < tk:
                t.r[eng] = tk
        for t in writes:
            t.w = {eng: tk}
            t.r = {}
        return inst

    def dma(self, q, out, in_, key, reads=(), writes=()):
        self.newsem(key)
        e = self._waits(q, reads, writes)
        self.cnt[key] += 16
        e.dma_start(out=out, in_=in_).then_inc(self.sems[key], 16)
        tk = self.cnt[key]
        for t in reads:
            t.r[key] = tk
        for t in writes:
            t.w = {key: tk}
            t.r = {}

    def barrier(self, skip=()):
        for eng, e in self.E.items():
            seen = self.seen[eng]
            for k, v in self.cnt.items():
                if k[:2] in skip:
                    continue
                if v > 0 and seen.get(k, 0) < v:
                    e.wait_ge(self.sems[k], v)
                    seen[k] = v


class Arena:
    def __init__(self, ap_bf16, nbytes):
        self.ap = ap_bf16
        self.n = nbytes
        self.off = 0

    def mark(self):
        return self.off

    def reset(self, m):
        self.off = m

    def get(self, shape, dt):
        esz = 4 if dt == F32 else 2
        nel = 1
        for s in shape[1:]:
            nel *= s
        nb = (nel * esz + 63) // 64 * 64
        assert self.off + nb <= self.n, ("arena overflow", self.off, nb, self.n)
        v = self.ap[0:shape[0], self.off // 2:(self.off + nel * esz) // 2]
        self.off += nb
        if dt == F32:
            v = v.bitcast(F32)
        if len(shape) == 3:
            v = v.rearrange("p (a b) -> p a b", a=shape[1])
        elif len(shape) == 4:
            v = v.rearrange("p (a b c) -> p a b c", a=shape[1], b=shape[2])
        return v


def build_program(debug_names=()):
    nc = bass.Bass("TRN2", target_bir_lowering=False)

    def din(name, shape):
        return nc.dram_tensor(name, list(shape), F32, kind="ExternalInput").ap()

    x = din("x", [T, D])
    ln_mix_g = din("ln_mix_g", [1, D])[0]
    w_in = din("w_in", [1, D, DIN])[0]
    biasg = din("biasg", [128, 5 * 8 * 128])
    amask = din("amask", [128, 5 * 128])
    rwkv_mu = din("rwkv_mu", [1, 1792])[0]
    rwkv_w0 = din("rwkv_w0", [1, 512])[0]
    rwkv_w2 = din("rwkv_w2", [1, 64, 512])[0]
    rwkv_a0 = din("rwkv_a0", [1, 512])[0]
    rwkv_a2 = din("rwkv_a2", [1, 64, 512])[0]
    rwkv_g2 = din("rwkv_g2", [1, 128, 512])[0]
    rwkv_k_k = din("rwkv_k_k", [1, 512])[0]
    rwkv_k_a = din("rwkv_k_a", [1, 512])[0]
    rwkv_r_k = din("rwkv_r_k", [1, 512])[0]
    rwkv_gn_g = din("rwkv_gn_g", [1, 512])[0]
    rwkv_gn_b = din("rwkv_gn_b", [1, 512])[0]
    w_branch_att = din("w_branch_att", [1, 512, D])[0]
    w_branch_rwkv = din("w_branch_rwkv", [1, 512, D])[0]
    w_out = din("w_out", [1, D, D])[0]
    ln_ffn_g = din("ln_ffn_g", [1, D])[0]
    router_group_w = din("router_group_w", [1, D, 4])[0]
    router_group_b = din("router_group_b", [1, 4])[0]
    router_expert_w = din("router_expert_w", [1, D, 32])[0]
    router_expert_b = din("router_expert_b", [1, 32])[0]
    expert_w_gate = din("expert_w_gate", [1, 32, D, 256])[0]
    expert_w_up = din("expert_w_up", [1, 32, D, 256])[0]
    expert_w_down = din("expert_w_down", [1, 32, 256, D])[0]
    ln_final_g = din("ln_final_g", [D])
    c_ident = din("c_ident", [128, 128])
    c_masks = din("c_masks", [128, 4 * 128])
    c_i2 = din("c_i2", [128, 512])
    out = nc.dram_tensor("out", [T, D], F32, kind="ExternalOutput").ap()
    dbg = {}
    for nm, shp in debug_names:
        dbg[nm] = nc.dram_tensor("dbg_" + nm, list(shp), F32, kind="ExternalOutput").ap()

    with ExitStack() as es:
        k = K(nc, es)
        NB = 207 * 1024
        arena_t = es.enter_context(nc.sbuf_tensor("arena", [128, NB // 2], BF16))
        ar = Arena(arena_t[:, :], NB)
        ps = es.enter_context(nc.psum_tensor("ps", [128, 8, 512], F32))
        pst = [Trk() for _ in range(8)]
        rot = {"i": 0, "n": 6}

        held = []

        def bank():
            while True:
                b = rot["i"] % rot["n"]
                rot["i"] += 1
                if b not in held:
                    return b, pst[b]

        rotB = {"i": 0}

        def bankB():
            b, t = bank()
            held.append(b)
            if len(held) > 3:
                held.pop(0)
            return b, t

        def psf(b):
            return ps[:, b, :]

        def psb(b):
            return ps[:, b, :].bitcast(BF16)

        dbg_stage = {}

        def dump(name, ap_f32, trk):
            if name in dbg:
                k.dma("sp", dbg[name], ap_f32, "dbg_" + name, reads=[trk])

        try:
            ident_b = ar.get([128, 128], BF16)
            masks_f = ar.get([128, 4, 128], F32)
            t_const = Trk()
            t_const2 = Trk()
            k.dma("pool", ident_b, c_ident, "c1", writes=[t_const2])
            t_masks = Trk()
            k.dma("sp", masks_f.rearrange("p a b -> p (a b)"), c_masks, "c2", writes=[t_masks])
            h_mark = ar.mark()
            hT = ar.get([128, KC, T + 1], BF16)
            t_hT = [Trk() for _ in range(NT)]
            t_hz = Trk()
            k.op("dve", lambda e: e.memset(hT[:, :, 0:1], 0.0), writes=[t_hz])
            base_mark = ar.mark()

            def rms_tile(src_ap, t_src, gbc, t_g, ss, t_ss, hb, t_hb, sq_junk, t_junk, j, dstT, t_dst, col0, eps=1e-6):
                k.op("act", lambda e: e.activation(out=sq_junk, in_=src_ap, func=AF.Square, accum_out=ss[:, j:j + 1]),
                     reads=[t_src], writes=[t_junk, t_ss])
                k.op("dve", lambda e: e.tensor_scalar(out=ss[:, j:j + 1], in0=ss[:, j:j + 1], scalar1=1.0 / D, scalar2=eps,
                                                      op0=ALU.mult, op1=ALU.add), reads=[t_ss], writes=[t_ss])
                k.op("act", lambda e: e.sqrt(ss[:, j:j + 1], ss[:, j:j + 1]), reads=[t_ss], writes=[t_ss])
                k.op("dve", lambda e: e.reciprocal(ss[:, j:j + 1], ss[:, j:j + 1]), reads=[t_ss], writes=[t_ss])
                k.op("dve", lambda e: e.scalar_tensor_tensor(out=hb, in0=src_ap, scalar=ss[:, j:j + 1], in1=gbc,
                                                             op0=ALU.mult, op1=ALU.mult),
                     reads=[t_src, t_ss, t_g], writes=[t_hb])
                b, tb = bank()
                for c in range(KC):
                    k.op("pe", lambda e, c=c: e.transpose(psb(b)[:, c * 128:(c + 1) * 128], hb[:, c * 128:(c + 1) * 128], ident_b),
                         reads=[t_hb, t_const2], writes=[tb], inc=(c == KC - 1))
                k.op("act", lambda e: e.activation(out=dstT[:, :, col0:col0 + 128],
                                                   in_=psb(b).rearrange("p (a b) -> p a b", a=KC), func=AF.Copy),
                     reads=[tb], writes=[t_dst])

            def TT(eng, out, in0, in1, op, reads, writes):
                k.op(eng, lambda e: e.tensor_tensor(out=out, in0=in0, in1=in1, op=op), reads=reads, writes=writes)

            def ACT(out, in_, func, reads, writes, scale=1.0):
                k.op("act", lambda e: e.activation(out=out, in_=in_, func=func, scale=scale), reads=reads, writes=writes)

            def CP(on_act, out, in_, reads, writes):
                if on_act:
                    k.op("act", lambda e: e.activation(out=out, in_=in_, func=AF.Copy), reads=reads, writes=writes)
                else:
                    k.op("dve", lambda e: e.tensor_copy(out, in_), reads=reads, writes=writes)

            def h8(ap):
                return ap.rearrange("p (h d) -> p h d", h=8)

            def bc8(ap8):
                return ap8.unsqueeze(2).to_broadcast([128, 8, 64])

            a_save = ar.mark()
            ar.reset(NB - 20 * 1024)
            gbc = ar.get([128, D], F32)
            t_g = Trk()
            k.dma("sp", gbc, ln_mix_g.partition_broadcast(128), "gbc", writes=[t_g])
            ssA = ar.get([128, NT], F32)
            t_ssl = [Trk() for _ in range(NT)]
            k.op("dve", lambda e: e.memset(ssA, 0.0), writes=t_ssl)
            xs = [ar.get([128, D], F32) for _ in range(2)]
            t_xs = [Trk() for _ in range(2)]
            hb = [ar.get([128, D], BF16) for _ in range(2)]
            t_hb = [Trk() for _ in range(2)]
            junk = ar.get([128, D], BF16)
            t_junk = Trk()
            ar.reset(a_save)
            for j in range(NT):
                s = j % 2
                k.dma("sp", xs[s], x[j * 128:(j + 1) * 128, :], "xs%d" % s, writes=[t_xs[s]])
                rms_tile(xs[s], t_xs[s], gbc, t_g, ssA, t_ssl[j], hb[s], t_hb[s], junk, t_junk, j, hT, t_hT[j], 1 + j * 128)
            if "hT" in dbg:
                st = ar.get([128, T], F32)
                tt = Trk()
                k.op("dve", lambda e: e.tensor_copy(st, hT[:, 0, 1:T + 1]), reads=t_hT + [t_hz], writes=[tt])
                dump("hT", st, tt)
            ar.reset(base_mark)
            t_hall = Trk()
            top_limit = NB - 20 * 1024

            def hT_deps(t_lo, t_hi):
                j0 = max(0, (t_lo - 1) // 128)
                j1 = (t_hi - 1) // 128
                return [t_hz] + [t_hT[jj] for jj in range(j0, j1 + 1)]

            if STOP == 'A':
                raise _Stop()
            rwkvT = ar.get([128, 4, T], BF16)
            t_rwT = [Trk() for _ in range(NT)]
            c_mark = ar.mark()
            rkv = ar.get([128, NT, 1536], BF16)
            t_rkv = Trk()
            w2a2 = ar.get([128, 512], BF16)
            g2w = ar.get([128, 512], BF16)
            t_sw = Trk()
            k.dma("pool", w2a2[0:64, :], rwkv_w2, "sw0", writes=[t_sw])
            k.dma("pool", w2a2[64:128, :], rwkv_a2, "sw1", writes=[t_sw])
            k.dma("pool", g2w, rwkv_g2, "sw2", writes=[t_sw])
            bcs = {}
            t_bc = Trk()
            for i, (nm, src) in enumerate([("w0", rwkv_w0), ("a0", rwkv_a0), ("kk", rwkv_k_k), ("ka", rwkv_k_a),
                                           ("rk", rwkv_r_k), ("gng", rwkv_gn_g), ("gnb", rwkv_gn_b)]):
                bcs[nm] = ar.get([128, 512], F32)
                k.dma("sp", bcs[nm], src.partition_broadcast(128), "bc%d" % i, writes=[t_bc])
            i2rep = ar.get([128, 512], F32)
            k.dma("sp", i2rep, c_i2, "bci2", writes=[t_bc])
            loraT = ar.get([128, 2, T], BF16)
            t_lora = Trk()
            m0 = ar.mark()
            W1 = ar.get([128, KC, 1536], BF16)
            t_W = Trk()
            k.dma("pool", W1, w_in[:, 1536:3072].rearrange("(c p) n -> p c n", p=128), "wrkv", writes=[t_W])
            mu_bc = ar.get([128, 1792], F32)
            t_mu = Trk()
            k.dma("sp", mu_bc, rwkv_mu.partition_broadcast(128), "mu", writes=[t_mu])
            WL1 = ar.get([128, KC, 256], BF16)
            WL2 = ar.get([128, KC, 256], BF16)
            t_WL = Trk()
            omu_l = ar.get([128, 256], F32)
            stg = [ar.get([128, 256], F32) for _ in range(2)]
            t_stg = [Trk() for _ in range(2)]
            t_omu = Trk()
            k.op("dve", lambda e: e.tensor_scalar(out=omu_l, in0=mu_bc[:, 1536:1792], scalar1=-1.0, scalar2=1.0, op0=ALU.mult, op1=ALU.add),
                 reads=[t_mu], writes=[t_omu])
            for c in range(KC):
                s = c % 2
                k.dma("sp", stg[s], w_in[c * 128:(c + 1) * 128, 3072:3328], "stg%d" % s, writes=[t_stg[s]])
                k.op("dve", lambda e, c=c, s=s: e.tensor_tensor(out=WL1[:, c, :], in0=stg[s], in1=omu_l, op=ALU.mult),
                     reads=[t_stg[s], t_omu], writes=[t_WL])
                k.op("pool", lambda e, c=c, s=s: e.tensor_tensor(out=WL2[:, c, :], in0=stg[s], in1=mu_bc[:, 1536:1792], op=ALU.mult),
                     reads=[t_stg[s], t_mu], writes=[t_WL])
            for tb in range(4):
                for lc in range(2):
                    b, tbk = bank()
                    c0 = lc * 128
                    n = 0
                    for w, sh in ((WL1, 1), (WL2, 0)):
                        for c in range(KC):
                            k.op("pe", lambda e, w=w, sh=sh, c=c, n=n: e.matmul(
                                psf(b), w[:, c, c0:c0 + 128], hT[:, c, sh + tb * 512: sh + tb * 512 + 512],
                                start=(n == 0), stop=(n == 15)), reads=[t_WL] + hT_deps(tb * 512, (tb + 1) * 512), writes=[tbk], inc=(n == 15))
                            n += 1
                    if lc == 0:
                        k.op("act", lambda e: e.activation(out=loraT[0:64, 0, tb * 512:(tb + 1) * 512], in_=psf(b)[0:64, :],
                                                           func=AF.Tanh), reads=[tbk], writes=[t_lora])
                        k.op("act", lambda e: e.activation(out=loraT[64:128, 0, tb * 512:(tb + 1) * 512], in_=psf(b)[64:128, :],
                                                           func=AF.Copy), reads=[tbk], writes=[t_lora])
                    else:
                        k.op("act", lambda e: e.activation(out=loraT[:, 1, tb * 512:(tb + 1) * 512], in_=psf(b),
                                                           func=AF.Sigmoid), reads=[tbk], writes=[t_lora])
            xb_ = [(ar.get([128, 512], F32), Trk()) for _ in range(2)]
            db_ = [(ar.get([128, 512], F32), Trk()) for _ in range(2)]
            it = 0
            for j in range(NT):
                cur = hT[:, :, 1 + j * 128: 1 + (j + 1) * 128]
                prv = hT[:, :, j * 128:(j + 1) * 128]
                for blk in range(3):
                    bP, tP = bank()
                    for c in range(KC):
                        k.op("pe", lambda e, c=c: e.matmul(
                            psf(bP), cur[:, c, :], W1[:, c, blk * 512:(blk + 1) * 512], start=(c == 0), stop=(c == KC - 1)),
                            reads=[t_W] + hT_deps(j * 128, (j + 1) * 128), writes=[tP], inc=(c == KC - 1))
                    bQ, tQ = bank()
                    for c in range(KC):
                        k.op("pe", lambda e, c=c: e.matmul(
                            psf(bQ), prv[:, c, :], W1[:, c, blk * 512:(blk + 1) * 512], start=(c == 0), stop=(c == KC - 1)),
                            reads=[t_W] + hT_deps(j * 128, (j + 1) * 128), writes=[tQ], inc=(c == KC - 1))
                    (x_, t_x), (dq, t_dq) = xb_[it % 2], db_[it % 2]
                    it += 1
                    ACT(x_, psf(bP), AF.Copy, [tP], [t_x])
                    TT("dve", dq, psf(bQ), x_, ALU.subtract, [tQ, t_x], [t_dq])
                    TT("pool", dq, dq, mu_bc[:, blk * 512:(blk + 1) * 512], ALU.mult, [t_dq, t_mu], [t_dq])
                    TT("pool", rkv[:, j, blk * 512:(blk + 1) * 512], x_, dq, ALU.add, [t_x, t_dq], [t_rkv])
            assert ar.mark() <= top_limit, (ar.mark(), top_limit)
            k.barrier()
            ar.reset(m0)
            t_rkv = Trk()

            if STOP == 'C0':
                raise _Stop()

            def f32t():
                return ar.get([128, 512], F32), Trk()

            def bf16t():
                return ar.get([128, 512], BF16), Trk()

            NBUF = 2
            tm = {}
            for nm in ("kmod", "tz", "sg", "Lp", "eLm", "eLC", "kk", "tmp", "kkn"):
                tm[nm] = [f32t()]
            tm["g"] = [f32t(), f32t()]
            tm["eL"] = tm["kkn"]
            tm["GD"] = tm["tz"]
            tm["a"] = tm["tz"]
            tm["eLm1"] = tm["sg"]
            tm["Ysb"] = tm["Lp"]
            tm["ka"] = tm["kk"]
            tb_ = {}
            for nm in ("aT", "bT", "kT", "rT", "bh", "kh", "GDb", "GDl"):
                tb_[nm] = [bf16t() for _ in range(NBUF)]
            tb_["y4"] = [bf16t()] * 2
            sm = {}
            for nm in ("ss8", "bs8", "ysum", "yss", "mean", "rstd"):
                sm[nm] = [(ar.get([128, 8], F32), Trk()) for _ in range(NBUF)]
            NU = 8 // NGRP
            ub = []
            for u in range(NU):
                d_ = {}
                d_["FM"] = (ar.get([64, 512], BF16), Trk())
                d_["NAKA"] = (ar.get([128, 512], BF16), Trk())
                d_["NN"] = [(ar.get([128, 256], BF16), Trk()) for _ in range(2)]
                d_["T"] = [(ar.get([128, 128], BF16), Trk()) for _ in range(2)]
                d_["X"] = (ar.get([128, 64], BF16), Trk())
                d_["DU"] = (ar.get([128, 128], BF16), Trk())
                d_["MB"] = (ar.get([64, 256], F32), Trk())
                d_["RQ"] = (ar.get([64, 2, 128], BF16), Trk())
                k.op("dve", lambda e, d_=d_: e.memset(d_["RQ"][0], 0.0), writes=[d_["RQ"][1]])
                ub.append(d_)
            Sst = [[(ar.get([64, 64], F32), Trk()) for _ in range(2)] for _ in range(8)]
            Sb = [[(ar.get([64, 64], BF16), Trk()) for _ in range(2)] for _ in range(8)]
            for h in range(8):
                k.op("dve", lambda e, h=h: e.memset(Sst[h][0][0], 0.0), writes=[Sst[h][0][1]])
            s_cur = [0] * 8
            ucount = 0
            YB = [6, 7]
            rot["n"] = 7


            def prep_gen(j):
                s = j % NBUF
                cur = hT[:, :, 1 + j * 128: 1 + (j + 1) * 128]
                prv = hT[:, :, j * 128:(j + 1) * 128]
                r_, k_, v_ = rkv[:, j, 0:512], rkv[:, j, 512:1024], rkv[:, j, 1024:1536]
                t_r = t_k = t_v = t_rkv
                tok = slice(j * 128, (j + 1) * 128)
                b, tbk = bankB()
                k.op("pe", lambda e: e.matmul(psf(b), loraT[0:64, 0, tok], w2a2[0:64, :], start=True, stop=True),
                     reads=[t_lora, t_sw], writes=[tbk])
                yield
                tz, t_tz = tm["tz"][0]
                TT("dve", tz, psf(b), bcs["w0"], ALU.add, [tbk, t_bc], [t_tz])
                yield
                sg, t_sg = tm["sg"][0]
                ACT(sg, tz, AF.Sigmoid, [t_tz], [t_sg])
                yield
                b, tbk = bankB()
                k.op("pe", lambda e: e.matmul(psf(b), loraT[64:128, 0, tok], w2a2[64:128, :], start=True, stop=True),
                     reads=[t_lora, t_sw], writes=[tbk])
                yield
                TT("dve", tz, psf(b), bcs["a0"], ALU.add, [tbk, t_bc], [t_tz])
                yield
                a_, t_a = tm["a"][0]
                ACT(a_, tz, AF.Sigmoid, [t_tz], [t_a])
                yield
                b, tbk = bankB()
                k.op("pe", lambda e: e.matmul(psf(b), loraT[:, 1, tok], g2w, start=True, stop=True),
                     reads=[t_lora, t_sw], writes=[tbk])
                yield
                g_, t_g_ = tm["g"][s]
                ACT(g_, psf(b), AF.Copy, [tbk], [t_g_])
                yield
                bL, tL = bankB()
                k.op("pe", lambda e: e.matmul(psf(bL), masks_f[:, 1, :], sg, start=True, stop=True),
                     reads=[t_masks, t_sg], writes=[tL])
                yield
                bC, tC = bankB()
                k.op("pe", lambda e: e.matmul(psf(bC), masks_f[:, 3, :], sg, start=True, stop=True),
                     reads=[t_masks, t_sg], writes=[tC])
                yield
                Lp, t_Lp = tm["Lp"][0]
                ACT(Lp, psf(bL), AF.Copy, [tL], [t_Lp])
                yield
                eL, t_eL = tm["eL"][0]
                ACT(eL, psf(bL), AF.Exp, [tL], [t_eL], scale=-CDEC)
                yield
                rT, t_rT = tb_["rT"][s]
                TT("dve", rT, r_, eL, ALU.mult, [t_r, t_eL], [t_rT])
                yield
                eLm, t_eLm = tm["eLm"][0]
                ACT(eLm, psf(bL), AF.Exp, [tL], [t_eLm], scale=CDEC)
                yield
                tmp, t_tmp = tm["tmp"][0]
                TT("dve", tmp, Lp, sg, ALU.subtract, [t_Lp, t_sg], [t_tmp])
                yield
                eLm1, t_eLm1 = tm["eLm1"][0]
                ACT(eLm1, tmp, AF.Exp, [t_tmp], [t_eLm1], scale=-CDEC)
                yield
                TT("dve", tmp, psf(bC), Lp, ALU.subtract, [tC, t_Lp], [t_tmp])
                yield
                eLC, t_eLC = tm["eLC"][0]
                ACT(eLC, tmp, AF.Exp, [t_tmp], [t_eLC], scale=-CDEC)
                yield
                kk, t_kk = tm["kk"][0]
                TT("dve", kk, k_, bcs["kk"], ALU.mult, [t_k, t_bc], [t_kk])
                yield
                TT("dve", tmp, kk, kk, ALU.mult, [t_kk], [t_tmp])
                yield
                ss8, t_ss8 = sm["ss8"][s]
                k.op("dve", lambda e: e.tensor_reduce(out=ss8, in_=h8(tmp), axis=AX.X, op=ALU.add), reads=[t_tmp], writes=[t_ss8])
                yield
                k.op("dve", lambda e: e.tensor_scalar(out=ss8, in0=ss8, scalar1=1e-24, scalar2=None, op0=ALU.max),
                     reads=[t_ss8], writes=[t_ss8])
                yield
                k.op("act", lambda e: e.sqrt(ss8, ss8), reads=[t_ss8], writes=[t_ss8])
                yield
                k.op("dve", lambda e: e.reciprocal(ss8, ss8), reads=[t_ss8], writes=[t_ss8])
                yield
                kkn, t_kkn = tm["kkn"][0]
                TT("dve", h8(kkn), h8(kk), bc8(ss8), ALU.mult, [t_kk, t_ss8], [t_kkn])
                yield
                kmod, t_kmod = tm["kmod"][0]
                k.op("dve", lambda e: e.scalar_tensor_tensor(out=tmp, in0=a_, scalar=-1.0, in1=bcs["ka"], op0=ALU.add, op1=ALU.mult),
                     reads=[t_a, t_bc], writes=[t_tmp])
                yield
                k.op("dve", lambda e: e.scalar_tensor_tensor(out=kmod, in0=tmp, scalar=1.0, in1=k_, op0=ALU.add, op1=ALU.mult),
                     reads=[t_tmp, t_k], writes=[t_kmod])
                yield
                ka, t_ka = tm["ka"][0]
                TT("dve", ka, kkn, a_, ALU.mult, [t_kkn, t_a], [t_ka])
                yield
                GD, t_GD = tm["GD"][0]
                ACT(GD, psf(bC), AF.Exp, [tC], [t_GD], scale=-CDEC)
                yield
                GDb, t_GDb = tb_["GDb"][s]
                TT("dve", GD, GD, i2rep, ALU.mult, [t_GD, t_bc], [t_GD])
                yield
                GDl, t_GDl = tb_["GDl"][s]
                k.op("dve", lambda e: e.tensor_copy(GDb, GD), reads=[t_GD], writes=[t_GDb])
                yield
                TT("dve", GD, GD, GDb, ALU.subtract, [t_GD, t_GDb], [t_GD])
                yield
                k.op("dve", lambda e: e.tensor_copy(GDl, GD), reads=[t_GD], writes=[t_GDl])
                yield
                aT, t_aT = tb_["aT"][s]
                k.op("dve", lambda e: e.scalar_tensor_tensor(out=aT, in0=kkn, scalar=-1.0, in1=eLm1, op0=ALU.mult, op1=ALU.mult),
                     reads=[t_kkn, t_eLm1], writes=[t_aT])
                yield
                bT, t_bT = tb_["bT"][s]
                TT("dve", bT, ka, eLm, ALU.mult, [t_ka, t_eLm], [t_bT])
                yield
                kT_, t_kT = tb_["kT"][s]
                TT("dve", kT_, kmod, eLm, ALU.mult, [t_kmod, t_eLm], [t_kT])
                yield
                bh, t_bh = tb_["bh"][s]
                TT("dve", bh, ka, eLC, ALU.mult, [t_ka, t_eLC], [t_bh])
                yield
                kh, t_kh = tb_["kh"][s]
                TT("dve", kh, kmod, eLC, ALU.mult, [t_kmod, t_eLC], [t_kh])
                yield
                TT("dve", tmp, r_, kmod, ALU.mult, [t_r, t_kmod], [t_tmp])
                yield
                TT("dve", tmp, tmp, bcs["rk"], ALU.mult, [t_tmp, t_bc], [t_tmp])
                yield
                bs8, t_bs8 = sm["bs8"][s]
                k.op("dve", lambda e: e.tensor_reduce(out=bs8, in_=h8(tmp), axis=AX.X, op=ALU.add), reads=[t_tmp], writes=[t_bs8])
                yield
                if j == 0:
                    dump("sg", sg, t_sg)
                    yield
                    dump("kkn", kkn, t_kkn)
                    yield
                    dump("kmod", kmod, t_kmod)
                    yield
                    dump("eLm1", eLm1, t_eLm1)
                    yield
                    dump("eLC", eLC, t_eLC)
                    yield

                if STOP == 'C1':
                    raise _Stop()
            def post_gen(j):
                s = j % NBUF
                yb = 7
                t_yb = pst[yb]
                v_, t_v = rkv[:, j, 1024:1536], t_rkv
                g_, t_g_ = tm['g'][s]
                bs8, t_bs8 = sm['bs8'][s]
                tmp, t_tmp = tm['tmp'][0]
                Ysb, t_Y = tm["Ysb"][0]
                ACT(Ysb, psf(yb), AF.Copy, [t_yb], [t_Y])
                yield
                if j == 0:
                    dump("Y0", Ysb, t_Y)
                    yield
                ysum, t_ysum = sm["ysum"][s]
                k.op("dve", lambda e: e.tensor_reduce(out=ysum, in_=h8(Ysb), axis=AX.X, op=ALU.add), reads=[t_Y], writes=[t_ysum])
                yield
                TT("dve", tmp, Ysb, Ysb, ALU.mult, [t_Y], [t_tmp])
                yield
                yss, t_yss = sm["yss"][s]
                k.op("dve", lambda e: e.tensor_reduce(out=yss, in_=h8(tmp), axis=AX.X, op=ALU.add), reads=[t_tmp], writes=[t_yss])
                yield
                mean, t_mean = sm["mean"][s]
                k.op("dve", lambda e: e.tensor_scalar(out=mean, in0=ysum, scalar1=1.0 / 64, scalar2=None, op0=ALU.mult),
                     reads=[t_ysum], writes=[t_mean])
                yield
                rstd, t_rstd = sm["rstd"][s]
                TT("dve", rstd, mean, mean, ALU.mult, [t_mean], [t_rstd])
                yield
                k.op("dve", lambda e: e.scalar_tensor_tensor(out=rstd, in0=yss, scalar=1.0 / 64, in1=rstd, op0=ALU.mult, op1=ALU.subtract),
                     reads=[t_yss, t_rstd], writes=[t_rstd])
                yield
                k.op("dve", lambda e: e.tensor_scalar(out=rstd, in0=rstd, scalar1=64e-5, scalar2=None, op0=ALU.add),
                     reads=[t_rstd], writes=[t_rstd])
                yield
                k.op("act", lambda e: e.sqrt(rstd, rstd), reads=[t_rstd], writes=[t_rstd])
                yield
                k.op("dve", lambda e: e.reciprocal(rstd, rstd), reads=[t_rstd], writes=[t_rstd])
                yield
                TT("dve", h8(Ysb), h8(Ysb), bc8(mean), ALU.subtract, [t_Y, t_mean], [t_Y])
                yield
                TT("dve", h8(Ysb), h8(Ysb), bc8(rstd), ALU.mult, [t_Y, t_rstd], [t_Y])
                yield
                TT("dve", Ysb, Ysb, bcs["gng"], ALU.mult, [t_Y, t_bc], [t_Y])
                yield
                TT("dve", Ysb, Ysb, bcs["gnb"], ALU.add, [t_Y, t_bc], [t_Y])
                yield
                TT("dve", h8(tmp), h8(v_), bc8(bs8), ALU.mult, [t_v, t_bs8], [t_tmp])
                yield
                TT("dve", Ysb, Ysb, tmp, ALU.add, [t_Y, t_tmp], [t_Y])
                yield
                y4, t_y4 = tb_["y4"][s]
                TT("dve", y4, Ysb, g_, ALU.mult, [t_Y, t_g_], [t_y4])
                yield
                if j == 0:
                    st = tm["eLC"][0][0]
                    k.op("dve", lambda e: e.tensor_copy(st, y4), reads=[t_y4], writes=[tm["eLC"][0][1]])
                    yield
                    dump("rwkv0", st, tm["eLC"][0][1])
                    yield
                b, tbk = bankB()
                for c in range(4):
                    k.op("pe", lambda e, c=c: e.transpose(psb(b)[:, c * 128:(c + 1) * 128], y4[:, c * 128:(c + 1) * 128], ident_b),
                         reads=[t_y4, t_const2], writes=[tbk], inc=(c == 3))
                    yield
                k.op("act", lambda e: e.activation(out=rwkvT[:, :, j * 128:(j + 1) * 128],
                                                   in_=psb(b)[:, 0:512].rearrange("p (a b) -> p a b", a=4), func=AF.Copy),
                     reads=[tbk], writes=[t_rwT[j]])
                yield
            def units(j, pump):
                s = j % NBUF
                aT, t_aT = tb_['aT'][s]
                bT, t_bT = tb_['bT'][s]
                kT_, t_kT = tb_['kT'][s]
                rT, t_rT = tb_['rT'][s]
                bh, t_bh = tb_['bh'][s]
                kh, t_kh = tb_['kh'][s]
                vb, t_vb = rkv[:, j, 1024:1536], t_rkv
                GDb, t_GDb = tb_['GDb'][s]
                GDl, t_GDl = tb_['GDl'][s]
                YB1 = 7
                yb = YB1
                t_yb = pst[yb]
                for grp in range(NGRP):
                    HPG = 8 // NGRP
                    heads = [HPG * grp + i_ for i_ in range(HPG)]
                    UH = [(ub[i_], heads[i_], slice(heads[i_] * 64, heads[i_] * 64 + 64)) for i_ in range(HPG)]
                    pump()
                    for U, h, hs in UH:
                        FM, t_FM = U["FM"]
                        b, tbk = bank()
                        for i, (src, tsrc) in enumerate(((aT, t_aT), (rT, t_rT), (bT, t_bT), (kT_, t_kT))):
                            k.op("pe", lambda e, i=i, src=src: e.transpose(psb(b)[0:64, i * 128:(i + 1) * 128], src[:, hs], ident_b),
                                 reads=[tsrc, t_const2], writes=[tbk], inc=(i == 3))
                        CP(h % 2 == 0, FM, psb(b)[0:64, 0:512], [tbk], [t_FM])
                    pump()
                    for U, h, hs in UH:
                        FM, t_FM = U["FM"]
                        b, tbk = bank()
                        k.op("pe", lambda e: e.matmul(psf(b)[:, 0:256], FM[:, 256:384], FM[:, 0:256], start=True, stop=True),
                             reads=[t_FM], writes=[tbk], inc=False)
                        k.op("pe", lambda e: e.matmul(psf(b)[:, 256:512], FM[:, 384:512], FM[:, 0:256], start=True, stop=True),
                             reads=[t_FM], writes=[tbk])
                        NAKA, t_NAKA = U["NAKA"]
                        k.op("dve", lambda e: e.tensor_tensor(
                            out=NAKA.rearrange("p (r a b) -> p r a b", r=2, a=2), in0=psf(b).rearrange("p (r a b) -> p r a b", r=2, a=2),
                            in1=masks_f[:, 0:2, :].unsqueeze(1).to_broadcast([128, 2, 2, 128]), op=ALU.mult),
                            reads=[tbk, t_masks], writes=[t_NAKA])
                    for U, h, hs in UH:
                        FM, t_FM = U["FM"]
                        NAKA, t_NAKA = U["NAKA"]
                        b, tbk = bank()
                        k.op("pe", lambda e: e.matmul(psf(b)[:, 0:128], FM[:, 0:128], FM[:, 256:384], start=True, stop=True),
                             reads=[t_FM], writes=[tbk])
                        bx, tbx = bank()
                        k.op("pe", lambda e: e.matmul(psf(bx)[:, 128:192], NAKA[:, 256:384], vb[:, hs], start=True, stop=True),
                             reads=[t_NAKA, t_vb], writes=[tbx])
                        NN0, t_NN0 = U["NN"][0]
                        TT("dve", NN0[:, 0:128], psf(b)[:, 0:128], masks_f[:, 2, :], ALU.mult, [tbk, t_masks], [t_NN0])
                        k.op("pool", lambda e: e.tensor_copy(NN0[:, 128:256], NAKA[:, 0:128]), reads=[t_NAKA], writes=[t_NN0])
                        T0, t_T0 = U["T"][0]
                        TT("pool", T0, NAKA[:, 0:128], ident_b, ALU.add, [t_NAKA, t_const2], [t_T0])
                        X, t_X = U["X"]
                        CP(h % 2 == 1, X, psf(bx)[:, 128:192], [tbx], [t_X])
                    if STOP == 'C2':
                        raise _Stop()
                    pump()
                    for lv in range(1, 6):
                        bks = []
                        for U, h, hs in UH:
                            NNp, t_NNp = U["NN"][(lv - 1) % 2]
                            NNc, t_NNc = U["NN"][lv % 2]
                            b, tbk = bank()
                            bks.append((b, tbk))
                            k.op("pe", lambda e: e.matmul(psf(b)[:, 0:128], NNp[:, 128:256], NNp[:, 0:128], start=True, stop=True),
                                 reads=[t_NNp], writes=[tbk], inc=(lv == 5))
                            if lv < 5:
                                k.op("pe", lambda e: e.matmul(psf(b)[:, 128:256], NNp[:, 0:128], NNp[:, 128:256], start=True, stop=True),
                                     reads=[t_NNp], writes=[tbk])
                                ACT(NNc, psf(b)[:, 0:256], AF.Copy, [tbk], [t_NNc])
                            else:
                                ACT(NNc[:, 0:128], psf(b)[:, 0:128], AF.Copy, [tbk], [t_NNc])
                        for (U, h, hs), (b, tbk) in zip(UH, bks):
                            NNc, t_NNc = U["NN"][lv % 2]
                            Tp, t_Tp = U["T"][(lv - 1) % 2]
                            Tc, t_Tc = U["T"][lv % 2]
                            k.op("pe", lambda e: e.matmul(psf(b)[:, 256:384], NNc[:, 0:128], Tp, start=True, stop=True),
                                 reads=[t_NNc, t_Tp], writes=[tbk])
                            TT("dve", Tc, psf(b)[:, 256:384], Tp, ALU.add, [tbk, t_Tp], [t_Tc])
                    pump()
                    for U, h, hs in UH:
                        T5, t_T5 = U["T"][1]
                        X, t_X = U["X"]
                        DU, t_DU = U["DU"]
                        b, tbk = bank()
                        k.op("pe", lambda e: e.matmul(psf(b)[:, 0:64], T5, aT[:, hs], start=True, stop=True),
                             reads=[t_T5, t_aT], writes=[tbk], inc=False)
                        k.op("pe", lambda e: e.matmul(psf(b)[:, 64:128], T5, X, start=True, stop=True),
                             reads=[t_T5, t_X], writes=[tbk])
                        CP(h % 2 == 0, DU, psf(b)[:, 0:128], [tbk], [t_DU])
                    if STOP == 'C3':
                        raise _Stop()
                    pump()
                    for U, h, hs in UH:
                        DU, t_DU = U["DU"]
                        MB, t_MB = U["MB"]
                        for i in range(2):
                            b, tbk = bank()
                            rs = slice(64 * i, 64 * i + 64)
                            mms = [(0, DU[rs, 0:64], bh[rs, hs], [t_DU, t_bh]),
                                   (0, ident_b[rs, rs], GDb[rs, hs], [t_const2, t_GDb]),
                                   (0, ident_b[rs, rs], GDl[rs, hs], [t_const2, t_GDl]),
                                   (64, bh[rs, hs], DU[rs, 64:128], [t_DU, t_bh]),
                                   (64, kh[rs, hs], vb[rs, hs], [t_kh, t_vb])]
                            for n_, (co, l_, r_2, rd) in enumerate(mms):
                                first = (n_ == 0) or (mms[n_ - 1][0] != co)
                                lastg = (n_ == len(mms) - 1) or (mms[n_ + 1][0] != co)
                                k.op("pe", lambda e: e.matmul(psf(b)[0:64, co:co + 64], l_, r_2, start=first, stop=lastg),
                                     reads=rd, writes=[tbk], inc=(n_ == len(mms) - 1))
                            CP(i == 0, MB[:, i * 128:(i + 1) * 128], psf(b)[0:64, 0:128], [tbk], [t_MB])
                    if STOP == 'C4':
                        raise _Stop()
                    for U, h, hs in UH:
                        FM, t_FM = U["FM"]
                        DU, t_DU = U["DU"]
                        NAKA, t_NAKA = U["NAKA"]
                        RQ, t_RQ = U["RQ"]
                        b, tbk = bank()
                        k.op("pe", lambda e: e.matmul(psf(b)[0:64, 0:128], DU[:, 0:64], NAKA[:, 128:256], start=True, stop=True),
                             reads=[t_DU, t_NAKA], writes=[tbk])
                        k.op("dve", lambda e: e.tensor_tensor(
                            out=RQ.rearrange("p a b -> p (a b)")[:, 0:256].rearrange("p (a b) -> p a b", a=2)[:, :, 0:64] if False else RQ[:, 0, 0:64],
                            in0=psf(b)[0:64, 0:64], in1=FM[:, 128:192], op=ALU.add), reads=[tbk, t_FM], writes=[t_RQ])
                        k.op("dve", lambda e: e.tensor_tensor(out=RQ[:, 1, 64:128], in0=psf(b)[0:64, 64:128], in1=FM[:, 192:256], op=ALU.add),
                             reads=[tbk, t_FM], writes=[t_RQ])
                    pump()
                    for i in range(2):
                        for U, h, hs in UH:
                            MB, t_MB = U["MB"]
                            Sc, t_Sc = Sst[h][s_cur[h]]
                            Sn, t_Sn = Sst[h][1 - s_cur[h]]
                            Sbi, t_Sbi = Sb[h][i]
                            k.op("act", lambda e: e.activation(out=Sbi, in_=Sc, func=AF.Copy), reads=[t_Sc], writes=[t_Sbi])
                            b, tbk = bank()
                            k.op("pe", lambda e: e.matmul(psf(b)[0:64, 0:64], MB[:, i * 128:i * 128 + 64], Sc, start=True, stop=True),
                                 reads=[t_MB, t_Sc], writes=[tbk])
                            TT("dve", Sn, psf(b)[0:64, 0:64], MB[:, i * 128 + 64:i * 128 + 128], ALU.add, [tbk, t_MB], [t_Sn])
                            s_cur[h] = 1 - s_cur[h]
                    pump()
                    for U, h, hs in UH:
                        DU, t_DU = U["DU"]
                        NAKA, t_NAKA = U["NAKA"]
                        RQ, t_RQ = U["RQ"]
                        yo = psf(yb)[:, hs]
                        k.op("pe", lambda e: e.matmul(yo, RQ[:, 0, :], Sb[h][0][0], start=True, stop=False),
                             reads=[t_RQ, Sb[h][0][1]], writes=[t_yb], inc=False)
                        k.op("pe", lambda e: e.matmul(yo, RQ[:, 1, :], Sb[h][1][0], start=False, stop=False),
                             reads=[t_RQ, Sb[h][1][1]], writes=[t_yb], inc=False)
                        k.op("pe", lambda e: e.matmul(yo, NAKA[:, 128:256], DU[:, 64:128], start=False, stop=False),
                             reads=[t_NAKA, t_DU], writes=[t_yb], inc=False)
                        k.op("pe", lambda e: e.matmul(yo, NAKA[:, 384:512], vb[:, hs], start=False, stop=True),
                             reads=[t_NAKA, t_vb], writes=[t_yb])
                pump()
            def exhaust(g_):
                for _ in g_:
                    pass
            rot["n"] = 7
            exhaust(prep_gen(0))
            pend_post = None
            for j in range(NT):
                gens = []
                if pend_post is not None:
                    gens.append(pend_post)
                if j + 1 < NT:
                    gens.append(prep_gen(j + 1))
                def pump(n=PUMP):
                    for _ in range(n):
                        while gens:
                            try:
                                next(gens[0])
                                break
                            except StopIteration:
                                gens.pop(0)
                units(j, pump)
                for g_ in gens:
                    exhaust(g_)
                if STOP == 'C5':
                    raise _Stop()
                pend_post = post_gen(j)
                next(pend_post)
            exhaust(pend_post)
            del held[:]
            k.barrier()
            ar.reset(c_mark)

            if STOP == 'C':
                raise _Stop()
            attT = ar.get([128, 4, T], BF16)
            rot["n"] = 6
            b_mark = ar.mark()
            Wqk = ar.get([128, KC, 1024], BF16)
            Wv = ar.get([128, KC, 512], BF16)
            t_Wqkc = [Trk() for _ in range(8)]
            t_Wv = Trk()
            for c in range(8):
                k.dma("pool", Wqk[:, :, c * 128:(c + 1) * 128], w_in[:, c * 128:(c + 1) * 128].rearrange("(c p) n -> p c n", p=128),
                      "wqk%d" % c, writes=[t_Wqkc[c]])
            k.dma("pool", Wv, w_in[:, 1024:1536].rearrange("(c p) n -> p c n", p=128), "wv", writes=[t_Wv])
            biasT = ar.get([128, 5, 8, 128], F32)
            am = ar.get([128, 5, 128], F32)
            t_bias = Trk()
            t_am = Trk()
            k.dma("sp", biasT.rearrange("p a b c -> p (a b c)"), biasg, "bias", writes=[t_bias])
            k.dma("sp", am.rearrange("p a b -> p (a b)"), amask, "am", writes=[t_am])
            k.op("dve", lambda e: e.tensor_tensor(out=biasT, in0=biasT, in1=am.unsqueeze(2).to_broadcast([128, 5, 8, 128]), op=ALU.add),
                 reads=[t_bias, t_am], writes=[t_bias])
            qkT = ar.get([128, 8, T], BF16)
            t_qk = Trk()
            Vaug = ar.get([128, NT, 8, 65], BF16)
            t_V = Trk()
            k.op("dve", lambda e: e.memset(Vaug.rearrange("p a b c -> p (a b c)"), 1.0), writes=[t_V])
            for c in range(8):
                for tb in range(4):
                    b, tbk = bank()
                    for kc in range(KC):
                        k.op("pe", lambda e, kc=kc: e.matmul(psf(b), Wqk[:, kc, c * 128:(c + 1) * 128],
                                                             hT[:, kc, 1 + tb * 512:1 + (tb + 1) * 512],
                                                             start=(kc == 0), stop=(kc == KC - 1)),
                             reads=[t_Wqkc[c], t_hall], writes=[tbk], inc=(kc == KC - 1))
                    ACT(qkT[:, c, tb * 512:(tb + 1) * 512], psf(b), AF.Copy, [tbk], [t_qk])
            for j in range(NT):
                b, tbk = bank()
                for kc in range(KC):
                    k.op("pe", lambda e, kc=kc: e.matmul(psf(b), hT[:, kc, 1 + j * 128:1 + (j + 1) * 128], Wv[:, kc, :],
                                                         start=(kc == 0), stop=(kc == KC - 1)),
                         reads=[t_Wv, t_hall], writes=[tbk], inc=(kc == KC - 1))
                k.op("act", lambda e: e.activation(out=Vaug[:, j, :, 0:64], in_=h8(psf(b)), func=AF.Copy), reads=[tbk], writes=[t_V])
            rden_b = [(ar.get([128, 4], F32), Trk()) for _ in range(2)]
            atok = [(ar.get([128, 512], BF16), Trk()) for _ in range(2)]
            items_att = []
            for j in range(NT):
                o_first = max(0, 4 - j)
                for o in range(o_first, 5):
                    items_att.append((j, o, o_first))
            sc_b = [[(ar.get([128, 512], F32), Trk()) for _ in range(2)] for _ in range(2)]
            pt_b = [[(ar.get([128, 512], BF16), Trk()) for _ in range(3)] for _ in range(2)]
            att_bk = {}

            def att_S(idx):
                j, o, o_first = items_att[idx]
                kt = j - 4 + o
                bE, tE = bank()
                bO, tO = bank()
                att_bk[idx] = None
                for hh in range(4):
                    for half, (b, tbk) in enumerate(((bE, tE), (bO, tO))):
                        h = 2 * hh + half
                        pr = slice(64 * half, 64 * half + 64)
                        k.op("pe", lambda e, hh=hh, h=h, pr=pr, b=b: e.matmul(
                            psf(b)[:, hh * 128:(hh + 1) * 128], qkT[pr, 4 + h // 2, kt * 128:(kt + 1) * 128],
                            qkT[pr, h // 2, j * 128:(j + 1) * 128], start=True, stop=True),
                            reads=[t_qk], writes=[tbk], inc=(hh == 3))
                for half, (b, tbk) in enumerate(((bE, tE), (bO, tO))):
                    sc, t_sc = sc_b[half][idx % 2]
                    pt, t_pt = pt_b[half][idx % 3]
                    k.op("dve", lambda e, b=b, half=half, sc=sc: e.scalar_tensor_tensor(
                        out=sc.rearrange("p (a b) -> p a b", a=4), in0=psf(b).rearrange("p (a b) -> p a b", a=4), scalar=0.125,
                        in1=biasT[:, o, half::2, :],
                        op0=ALU.mult, op1=ALU.add), reads=[tbk, t_bias], writes=[t_sc])
                    ACT(pt, sc, AF.Exp, [t_sc], [t_pt])

            def att_P(idx):
                j, o, o_first = items_att[idx]
                kt = j - 4 + o
                at, t_at = atok[j % 2]
                for half in range(2):
                    pt, t_pt = pt_b[half][idx % 3]
                    ob = YB[half]
                    t_ob = pst[ob]
                    for hh in range(4):
                        h = 2 * hh + half
                        k.op("pe", lambda e, hh=hh, h=h, ob=ob, pt=pt: e.matmul(
                            psf(ob)[:, hh * 65:(hh + 1) * 65], pt[:, hh * 128:(hh + 1) * 128], Vaug[:, kt, h, :],
                            start=(o == o_first and hh == 0), stop=(o == 4), skip_group_check=True), reads=[t_pt, t_V], writes=[t_ob],
                            inc=(o == 4 and hh == 3))
                if o != 4:
                    return
                for half in range(2):
                    ob = YB[half]
                    t_ob = pst[ob]
                    rden, t_rden = rden_b[half]
                    ov = psf(ob)[:, 0:260].rearrange("p (a b) -> p a b", a=4)
                    k.op("dve", lambda e, ov=ov, rden=rden: e.reciprocal(rden, ov[:, :, 64]), reads=[t_ob], writes=[t_rden])
                    k.op("dve", lambda e, ov=ov, rden=rden, half=half: e.tensor_tensor(
                        out=at.rearrange("p (a b d) -> p a b d", a=4, b=2)[:, :, half, :], in0=ov[:, :, 0:64],
                        in1=rden.unsqueeze(2).to_broadcast([128, 4, 64]), op=ALU.mult),
                        reads=[t_ob, t_rden], writes=[t_at])
                if j == 5:
                    stt = sc_b[0][0][0]
                    k.op("dve", lambda e: e.tensor_copy(stt, at), reads=[t_at], writes=[sc_b[0][0][1]])
                    dump("att5", stt, sc_b[0][0][1])
                b, tbk = bank()
                for c in range(4):
                    k.op("pe", lambda e, c=c: e.transpose(psb(b)[:, c * 128:(c + 1) * 128], at[:, c * 128:(c + 1) * 128], ident_b),
                         reads=[t_at, t_const2], writes=[tbk], inc=(c == 3))
                k.op("act", lambda e: e.activation(out=attT[:, :, j * 128:(j + 1) * 128],
                                                   in_=psb(b)[:, 0:512].rearrange("p (a b) -> p a b", a=4), func=AF.Copy),
                     reads=[tbk], writes=[t_V])

            n_att = len(items_att)
            SK = 2
            for step in range(n_att + SK):
                if step < n_att:
                    att_S(step)
                if step >= SK:
                    att_P(step - SK)
            k.barrier()
            ar.reset(b_mark)

            if STOP == 'B':
                raise _Stop()
            md_mark = ar.mark()
            mergedT = ar.get([128, KC, T], BF16)
            t_mg = Trk()
            Wo = ar.get([128, KC, D], BF16)
            t_Wo = Trk()
            d_mark = ar.mark()
            Wg = ar.get([128, KC, 2048], BF16)
            Wba = ar.get([128, 4, D], BF16)
            Wbr = ar.get([128, 4, D], BF16)
            t_WgA = [Trk() for _ in range(8)]
            t_WgB = [Trk() for _ in range(8)]
            t_Wb = Trk()
            t_Wb2 = Trk()
            k.dma("pool", Wba, w_branch_att.rearrange("(c p) n -> p c n", p=128), "wba", writes=[t_Wb])
            k.dma("pool", Wbr, w_branch_rwkv.rearrange("(c p) n -> p c n", p=128), "wbr", writes=[t_Wb2])
            for m in range(8):
                for off in (0, 1024):
                    cs = slice(off + m * 128, off + (m + 1) * 128)
                    k.dma("pool", Wg[:, :, cs], w_in[:, 3328 + cs.start:3328 + cs.stop].rearrange("(c p) n -> p c n", p=128),
                          "wg%d_%d" % (m, off // 1024), writes=[t_WgA[m] if off == 0 else t_WgB[m]])
            k.dma("pool", Wo, w_out.rearrange("(c p) n -> p c n", p=128), "wo", writes=[t_Wo])
            ga_b = [(ar.get([128, 512], F32), Trk()) for _ in range(2)]
            gb_b = [(ar.get([128, 512], F32), Trk()) for _ in range(2)]
            t_all = Trk()
            it = 0
            rot["n"] = 8
            for tb in range(4):
                cols = slice(tb * 512, (tb + 1) * 512)
                hcols = slice(1 + tb * 512, 1 + (tb + 1) * 512)
                for m in range(8):
                    bA, tA = bank()
                    for c in range(4):
                        k.op("pe", lambda e, c=c: e.matmul(psf(bA), Wba[:, c, m * 128:(m + 1) * 128], attT[:, c, cols],
                                                           start=(c == 0), stop=(c == 3)), reads=[t_Wb, t_all], writes=[tA], inc=(c == 3))
                    bB, tB = bank()
                    for c in range(4):
                        k.op("pe", lambda e, c=c: e.matmul(psf(bB), Wbr[:, c, m * 128:(m + 1) * 128], rwkvT[:, c, cols],
                                                           start=(c == 0), stop=(c == 3)), reads=[t_Wb2, t_all], writes=[tB], inc=(c == 3))
                    bGA, tGA = bank()
                    for c in range(KC):
                        k.op("pe", lambda e, c=c: e.matmul(psf(bGA), Wg[:, c, m * 128:(m + 1) * 128], hT[:, c, hcols],
                                                           start=(c == 0), stop=(c == KC - 1)), reads=[t_WgA[m], t_all], writes=[tGA], inc=(c == KC - 1))
                    bGB, tGB = bank()
                    for c in range(KC):
                        k.op("pe", lambda e, c=c: e.matmul(psf(bGB), Wg[:, c, 1024 + m * 128:1024 + (m + 1) * 128], hT[:, c, hcols],
                                                           start=(c == 0), stop=(c == KC - 1)), reads=[t_WgB[m], t_all], writes=[tGB], inc=(c == KC - 1))
                    ga, t_ga = ga_b[it % 2]
                    gb, t_gb = gb_b[it % 2]
                    it += 1
                    ACT(ga, psf(bGA), AF.Sigmoid, [tGA], [t_ga])
                    ACT(gb, psf(bGB), AF.Sigmoid, [tGB], [t_gb])
                    TT("dve", ga, ga, psf(bA), ALU.mult, [t_ga, tA], [t_ga])
                    TT("dve", gb, gb, psf(bB), ALU.mult, [t_gb, tB], [t_gb])
                    TT("dve", mergedT[:, m, cols], ga, gb, ALU.add, [t_ga, t_gb], [t_mg])
            if "merged" in dbg:
                st = ga_b[0][0]
                k.op("dve", lambda e: e.tensor_copy(st, mergedT[:, 0, 0:512]), reads=[t_mg], writes=[ga_b[0][1]])
                dump("merged", st, ga_b[0][1])
            k.barrier()
            ar.reset(d_mark)

            if STOP == 'D1':
                raise _Stop()
            ar.reset(h_mark)
            x2 = ar.get([128, NT, D], F32)
            assert ar.mark() <= md_mark, (ar.mark(), md_mark)
            x2_end = ar.mark()
            t_x2 = [Trk() for _ in range(NT)]
            ar.reset(d_mark)
            xs = [ar.get([128, D], F32) for _ in range(2)]
            e0_mark = ar.mark()
            h2T = ar.get([128, KC, T], BF16)
            t_h2 = [Trk() for _ in range(NT)]
            ssE = ar.get([128, NT], F32)
            t_ssE = [Trk() for _ in range(NT)]
            k.op("dve", lambda e: e.memset(ssE, 0.0), writes=t_ssE)
            Wr = ar.get([128, KC, 36], BF16)
            rbc = ar.get([128, 36], F32)
            lg = ar.get([128, NT, 36], F32)
            NG = NT * 4
            gmax = ar.get([128, NT], F32)
            gsh = ar.get([128, NT, 4], F32)
            gm = ar.get([128, NT, 4], F32)
            gsum = ar.get([128, NT], F32)
            el = ar.get([128, NG, 8], F32)
            m1 = ar.get([128, NG], F32)
            m2 = ar.get([128, NG], F32)
            e1 = ar.get([128, NG, 8], F32)
            e2 = ar.get([128, NG, 8], F32)
            w2_ = ar.get([128, NG], F32)
            comb = ar.get([128, NG, 8], F32)
            g2bc = ar.get([128, D], F32)
            k.dma("sp", g2bc, ln_ffn_g.partition_broadcast(128), "gbc", writes=[t_g])
            hb = [ar.get([128, D], BF16) for _ in range(2)]
            junk = ar.get([128, D], BF16)
            ssF = ar.get([128, NT], F32)
            t_ssFl = [Trk() for _ in range(NT)]
            k.op("dve", lambda e: e.memset(ssF, 0.0), writes=t_ssFl)
            e_top = ar.mark()
            NS = 6
            GS = 3
            slots = []
            for s_ in range(2):
                slots.append(dict(gu=ar.get([128, KC, 512], BF16), d=ar.get([128, 2, D], BF16),
                                  tg=Trk(), tu=Trk(), td=Trk()))
            sl_b = [(ar.get([128, 256], F32), Trk()) for _ in range(3)]
            av_b = [(ar.get([128, 256], BF16), Trk()) for _ in range(3)]
            avT_b = [(ar.get([128, 256], BF16), Trk()) for _ in range(3)]

            def load_expert(e_):
                sl = slots[e_ % NS]
                k.dma("pool", sl["gu"][:, :, 0:256], expert_w_gate[e_].rearrange("(c p) f -> p c f", p=128), "eg%d" % (e_ % NS),
                      writes=[sl["tg"]])
                k.dma("pool", sl["gu"][:, :, 256:512], expert_w_up[e_].rearrange("(c p) f -> p c f", p=128), "eu%d" % (e_ % NS),
                      writes=[sl["tu"]])
                k.dma("pool", sl["d"], expert_w_down[e_].rearrange("(c p) n -> p c n", p=128), "ed%d" % (e_ % NS),
                      writes=[sl["td"]])

            t_Wr = Trk()
            with nc.allow_non_contiguous_dma(reason="tiny router weights"):
                k.dma("pool", Wr[:, :, 0:4], router_group_w.rearrange("(c p) n -> p c n", p=128), "wr0", writes=[t_Wr])
                k.dma("pool", Wr[:, :, 4:36], router_expert_w.rearrange("(c p) n -> p c n", p=128), "wr1", writes=[t_Wr])
            t_rb = Trk()
            k.dma("sp", rbc[:, 0:4], router_group_b.partition_broadcast(128), "rb0", writes=[t_rb])
            k.dma("sp", rbc[:, 4:36], router_expert_b.partition_broadcast(128), "rb1", writes=[t_rb])
            t_lg = Trk()
            rot["n"] = 8
            for e_ in range(2):
                load_expert(e_)

            def moe_in_tile(j):
                rms_tile(x2[:, j, :], t_x2[j], g2bc, t_g, ssE, t_ssE[j], hb[j % 2], t_hb[j % 2], junk, t_junk, j, h2T, t_h2[j], j * 128)
                b, tbk = bank()
                for c in range(KC):
                    k.op("pe", lambda e, c=c: e.matmul(psf(b)[:, 0:36], h2T[:, c, j * 128:(j + 1) * 128], Wr[:, c, :],
                                                       start=(c == 0), stop=(c == KC - 1)), reads=[t_h2[j], t_Wr], writes=[tbk], inc=(c == KC - 1))
                TT("dve", lg[:, j, :], psf(b)[:, 0:36], rbc, ALU.add, [tbk, t_rb], [t_lg])

            for j in range(NT):
                s = j % 2
                k.dma("sp", xs[s], x[j * 128:(j + 1) * 128, :], "xs%d" % s, writes=[t_xs[s]])
                for half in range(2):
                    b, tbk = bank()
                    for m in range(KC):
                        k.op("pe", lambda e, m=m: e.matmul(psf(b), mergedT[:, m, j * 128:(j + 1) * 128], Wo[:, m, half * 512:(half + 1) * 512],
                                                           start=(m == 0), stop=(m == KC - 1)), reads=[t_Wo, t_all], writes=[tbk], inc=(m == KC - 1))
                    TT("dve", x2[:, j, half * 512:(half + 1) * 512], psf(b), xs[s][:, half * 512:(half + 1) * 512], ALU.add,
                       [tbk, t_xs[s]], [t_x2[j]])
                if j >= 1:
                    moe_in_tile(j - 1)
            moe_in_tile(NT - 1)
            dump("x2", x2[:, 0, :], t_x2[0])

            if STOP == 'D2':
                raise _Stop()
            gl = lg[:, :, 0:4]
            t_r = Trk()
            R = [t_lg, t_r]

            def V(fn):
                k.op("dve", fn, reads=R, writes=[t_r])

            def bcg(ap):
                return ap.unsqueeze(2).to_broadcast([128, NT, 4])

            def bce(ap):
                return ap.unsqueeze(2).to_broadcast([128, NG, 8])

            V(lambda e: e.tensor_copy(el.rearrange("p (a g) x -> p a (g x)", a=NT), lg[:, :, 4:36]))
            V(lambda e: e.tensor_reduce(out=gmax, in_=gl, axis=AX.X, op=ALU.max))
            V(lambda e: e.tensor_tensor(out=gsh, in0=gl, in1=bcg(gmax), op=ALU.subtract))
            V(lambda e: e.tensor_tensor(out=gm, in0=gl, in1=bcg(gmax), op=ALU.is_equal))
            k.op("act", lambda e: e.activation(out=gsh, in_=gsh, func=AF.Exp), reads=R, writes=[t_r])
            V(lambda e: e.tensor_reduce(out=gsum, in_=gsh, axis=AX.X, op=ALU.add))
            V(lambda e: e.reciprocal(gsum, gsum))
            V(lambda e: e.tensor_tensor(out=gm, in0=gm, in1=bcg(gsum), op=ALU.mult))
            V(lambda e: e.tensor_reduce(out=m1, in_=el, axis=AX.X, op=ALU.max))
            V(lambda e: e.tensor_tensor(out=e1, in0=el, in1=bce(m1), op=ALU.is_equal))
            V(lambda e: e.scalar_tensor_tensor(out=e2, in0=e1, scalar=NEG, in1=el, op0=ALU.mult, op1=ALU.add))
            V(lambda e: e.tensor_reduce(out=m2, in_=e2, axis=AX.X, op=ALU.max))
            V(lambda e: e.tensor_tensor(out=e1, in0=el, in1=bce(m2), op=ALU.is_ge))
            V(lambda e: e.tensor_tensor(out=e2, in0=el, in1=bce(m1), op=ALU.subtract))
            k.op("act", lambda e: e.activation(out=e2, in_=e2, func=AF.Exp), reads=R, writes=[t_r])
            V(lambda e: e.tensor_tensor(out=e2, in0=e2, in1=e1, op=ALU.mult))
            V(lambda e: e.tensor_tensor(out=m2, in0=m2, in1=m1, op=ALU.subtract))
            k.op("act", lambda e: e.activation(out=m2, in_=m2, func=AF.Exp), reads=R, writes=[t_r])
            V(lambda e: e.tensor_scalar(out=m2, in0=m2, scalar1=1.0, scalar2=None, op0=ALU.add))
            V(lambda e: e.reciprocal(m2, m2))
            V(lambda e: e.tensor_tensor(out=w2_, in0=m2, in1=gm.rearrange("p a g -> p (a g)"), op=ALU.mult))
            V(lambda e: e.tensor_tensor(out=comb, in0=e2, in1=bce(w2_), op=ALU.mult))
            combv = comb.rearrange("p (a g) x -> p a (g x)", a=NT)
            dump("comb", combv[:, 0, :], t_r)
            k.barrier(skip=("eg", "eu", "ed"))
            t_h2a = Trk()
            ar.reset(x2_end)
            for s_ in range(4):
                slots.append(dict(gu=ar.get([128, KC, 512], BF16), d=ar.get([128, 2, D], BF16),
                                  tg=Trk(), tu=Trk(), td=Trk()))
            ost = [(ar.get([128, D], F32), Trk()) for _ in range(2)]
            assert ar.mark() <= e0_mark, (ar.mark(), e0_mark)
            for e_ in range(2, NS):
                load_expert(e_)
            gfbc = g2bc
            t_gf = Trk()
            k.dma("sp", gfbc, ln_final_g.partition_broadcast(128), "gbc", writes=[t_gf])

            def final_tile(j):
                o_, t_o = ost[j % 2]
                t_s = t_ssFl[j]
                k.op("act", lambda e: e.activation(out=junk, in_=x2[:, j, :], func=AF.Square, accum_out=ssF[:, j:j + 1]),
                     reads=[t_x2[j]], writes=[t_junk, t_s])
                k.op("dve", lambda e: e.tensor_scalar(out=ssF[:, j:j + 1], in0=ssF[:, j:j + 1], scalar1=1.0 / D, scalar2=1e-6,
                                                      op0=ALU.mult, op1=ALU.add), reads=[t_s], writes=[t_s])
                k.op("act", lambda e: e.sqrt(ssF[:, j:j + 1], ssF[:, j:j + 1]), reads=[t_s], writes=[t_s])
                k.op("dve", lambda e: e.reciprocal(ssF[:, j:j + 1], ssF[:, j:j + 1]), reads=[t_s], writes=[t_s])
                k.op("dve", lambda e: e.scalar_tensor_tensor(out=o_, in0=x2[:, j, :], scalar=ssF[:, j:j + 1], in1=gfbc,
                                                             op0=ALU.mult, op1=ALU.mult), reads=[t_x2[j], t_s, t_gf], writes=[t_o])
                k.dma("sp", out[j * 128:(j + 1) * 128, :], o_, "ost%d" % (j % 2), reads=[t_o])
            groups = []
            e0 = 0
            while e0 < 32:
                groups.append(list(range(e0, min(32, e0 + GS))))
                e0 += GS
            items = []
            for g in groups:
                for j in range(NT):
                    for ei, e_ in enumerate(g):
                        items.append((g, j, ei, e_))
            loaded = NS
            UB = [0, 1, 2]
            TBK = 3
            yb_of = lambda j: (4, 5) if j % 2 == 0 else (6, 7)
            st_ = {}

            def stage_U(idx):
                g, j, ei, e_ = items[idx]
                sl = slots[e_ % NS]
                b = UB[idx % 3]
                for c in range(KC):
                    k.op("pe", lambda e, c=c: e.matmul(psf(b), h2T[:, c, j * 128:(j + 1) * 128], sl["gu"][:, c, :],
                                                       start=(c == 0), stop=(c == KC - 1)),
                         reads=[t_h2a, sl["tg"], sl["tu"]], writes=[pst[b]], inc=(c == KC - 1))
                s_, t_s = sl_b[idx % 3]
                av, t_av = av_b[idx % 3]
                ACT(s_, psf(b)[:, 0:256], AF.Silu, [pst[b]], [t_s])
                k.op("dve", lambda e: e.scalar_tensor_tensor(out=av, in0=psf(b)[:, 256:512], scalar=combv[:, j, e_:e_ + 1], in1=s_,
                                                             op0=ALU.mult, op1=ALU.mult), reads=[pst[b], t_s, t_r], writes=[t_av])

            def stage_T(idx):
                av, t_av = av_b[idx % 3]
                avT, t_avT = avT_b[idx % 3]
                off = (idx % 2) * 256
                tt = st_.setdefault(("tt", idx % 2), Trk())
                for f in range(2):
                    k.op("pe", lambda e, f=f: e.transpose(psb(TBK)[:, off + f * 128:off + (f + 1) * 128], av[:, f * 128:(f + 1) * 128], ident_b),
                         reads=[t_av, t_const2], writes=[tt], inc=(f == 1))
                ACT(avT, psb(TBK)[:, off:off + 256], AF.Copy, [tt], [t_avT])

            def stage_D(idx):
                g, j, ei, e_ = items[idx]
                sl = slots[e_ % NS]
                avT, t_avT = avT_b[idx % 3]
                yb2 = yb_of(j)
                last = (ei == len(g) - 1)
                for f in range(2):
                    for half in range(2):
                        k.op("pe", lambda e, f=f, half=half: e.matmul(psf(yb2[half]), avT[:, f * 128:(f + 1) * 128],
                                                                        sl["d"][:, f, half * 512:(half + 1) * 512],
                                                                        start=(ei == 0 and f == 0), stop=(last and f == 1)),
                             reads=[t_avT, sl["td"]], writes=[pst[yb2[half]]], inc=(last and f == 1))
                if last:
                    for half in range(2):
                        TT("dve", x2[:, j, half * 512:(half + 1) * 512], x2[:, j, half * 512:(half + 1) * 512], psf(yb2[half]),
                           ALU.add, [pst[yb2[half]], t_x2[j]], [t_x2[j]])
                    if g is groups[-1]:
                        final_tile(j)

            nonlocal_loaded = [loaded]
            n_items = len(items)
            for step in range(n_items + 2):
                if step < n_items:
                    stage_U(step)
                if 1 <= step <= n_items:
                    stage_T(step - 1)
                if 2 <= step <= n_items + 1:
                    idx = step - 2
                    stage_D(idx)
                    g, j, ei, e_ = items[idx]
                    if j == NT - 1 and ei == len(g) - 1:
                        for _ in g:
                            if nonlocal_loaded[0] < 32:
                                load_expert(nonlocal_loaded[0])
                                nonlocal_loaded[0] += 1

            if STOP == 'E':
                raise _Stop()
            k.barrier()
        except _Stop:
            k.barrier()
    return nc


def _consts(att_rel_bias):
    ident = np.eye(128, dtype=np.float32)
    s = np.arange(128)[:, None]
    t = np.arange(128)[None, :]
    same = (s // 64) == (t // 64)
    Ms = (same & (s < t)).astype(np.float32)
    Mi = (same & (s <= t)).astype(np.float32)
    MsT = (same & (s > t)).astype(np.float32)
    Blk = same.astype(np.float32)
    masks = np.concatenate([Ms, Mi, MsT, Blk], axis=1)
    i2 = (np.arange(128)[:, None] % 64 == np.arange(64)[None, :]).astype(np.float32)
    i2rep = np.tile(i2, (1, 8))
    kl = np.arange(128)[:, None]
    ql = np.arange(128)[None, :]
    tab = np.asarray(att_rel_bias, dtype=np.float32)[0]
    biasg = np.zeros((128, 5, 8, 128), np.float32)
    am = np.zeros((128, 5, 128), np.float32)
    for o in range(5):
        bpos = 2 * o + kl // 64 - ql // 64
        kpos = bpos * 64 + kl % 64
        rel = 512 + (ql % 64) - kpos
        idx = np.clip(rel, -64, 64) + 64
        valid = (bpos >= 0) & (bpos <= 8)
        biasg[:, o, :, :] = np.transpose(tab[:, idx], (1, 0, 2))
        am[:, o, :] = np.where(valid, 0.0, NEG)
    return ident, masks, i2rep, biasg.reshape(128, -1), am.reshape(128, -1)


_DEBUG_SPEC = []


def kernel(**inputs):
    inp = {k_: np.asarray(v) for k_, v in inputs.items()}
    n = 8
    ident, masks, i2rep, biasg, am = _consts(inp["att_rel_bias"])
    nc = build_program(_DEBUG_SPEC)
    shared = {k_: np.ascontiguousarray(v, dtype=np.float32) for k_, v in inp.items() if k_ not in ("x", "att_rel_bias")}
    shared["rwkv_r_k"] = shared["rwkv_r_k"].reshape(1, 512)
    shared.update(c_ident=ident, c_masks=masks, c_i2=i2rep, biasg=biasg, amask=am)
    ncores = int(os.environ.get("MK_CORES", n))
    in_maps = []
    for b in range(ncores):
        m = dict(shared)
        m["x"] = np.ascontiguousarray(inp["x"][b], dtype=np.float32)
        in_maps.append(m)
    res = run_bass_kernel_spmd(nc, in_maps, core_ids=list(range(ncores)))
    if _DEBUG_SPEC:
        kernel.last = res.results
    outs = [r["out"] for r in res.results]
    while len(outs) < n:
        outs.append(np.zeros_like(outs[0]))
    return np.stack(outs, axis=0).astype(np.float32)
```

```python
import math
import os
from contextlib import ExitStack

import numpy as np
import concourse.bass as bass
import concourse.mybir as mybir
from concourse.bass_utils import run_bass_kernel_spmd

F32 = mybir.dt.float32
BF16 = mybir.dt.bfloat16
AF = mybir.ActivationFunctionType
ALU = mybir.AluOpType
AX = mybir.AxisListType

T = 2048
NT = 16
D = 1024
KC = 8
DIN = 5376
CDEC = math.exp(-0.5)
NEG = -1.0e30
DEBUG = bool(os.environ.get("MK_DEBUG"))
STOP = os.environ.get("MK_STOP", "")
PUMP = int(os.environ.get("MK_PUMP", "6"))
NGRP = int(os.environ.get("MK_GRP", "2"))


class _Stop(Exception):
    pass


class Trk:
    __slots__ = ("w", "r")

    def __init__(self):
        self.w = {}
        self.r = {}


class K:
    def __init__(self, nc, es):
        self.nc = nc
        self.es = es
        self.E = dict(pe=nc.tensor, act=nc.scalar, dve=nc.vector, pool=nc.gpsimd, sp=nc.sync)
        self.sems = {}
        self.cnt = {}
        self.seen = {e: {} for e in self.E}
        for e in ("pe", "act", "dve", "pool"):
            self.newsem(e)

    def newsem(self, key):
        if key not in self.sems:
            self.sems[key] = self.es.enter_context(self.nc.semaphore("s_" + key))
            self.cnt[key] = 0
        return key

    def _waits(self, eng, reads, writes):
        need = {}
        for t in reads:
            for k, v in t.w.items():
                if need.get(k, 0) < v:
                    need[k] = v
        for t in writes:
            for d in (t.w, t.r):
                for k, v in d.items():
                    if need.get(k, 0) < v:
                        need[k] = v
        e = self.E[eng]
        seen = self.seen[eng]
        if eng == "pe":
            need.pop("pe", None)
        for k, v in need.items():
            if seen.get(k, 0) < v:
                e.wait_ge(self.sems[k], v)
                seen[k] = v
        return e

    def op(self, eng, fn, reads=(), writes=(), inc=True):
        e = self._waits(eng, reads, writes)
        inst = fn(e)
        if inc:
            self.cnt[eng] += 1
            inst.then_inc(self.sems[eng], 1)
            tk = self.cnt[eng]
        else:
            tk = self.cnt[eng] + 1
        for t in reads:
            if t.r.get(eng, 0) < tk:
                t.r[eng] = tk
        for t in writes:
            t.w = {eng: tk}
            t.r = {}
        return inst

    def dma(self, q, out, in_, key, reads=(), writes=()):
        self.newsem(key)
        e = self._waits(q, reads, writes)
        self.cnt[key] += 16
        e.dma_start(out=out, in_=in_).then_inc(self.sems[key], 16)
        tk = self.cnt[key]
        for t in reads:
            t.r[key] = tk
        for t in writes:
            t.w = {key: tk}
            t.r = {}

    def barrier(self, skip=()):
        for eng, e in self.E.items():
            seen = self.seen[eng]
            for k, v in self.cnt.items():
                if k[:2] in skip:
                    continue
                if v > 0 and seen.get(k, 0) < v:
                    e.wait_ge(self.sems[k], v)
                    seen[k] = v


class Arena:
    def __init__(self, ap_bf16, nbytes):
        self.ap = ap_bf16
        self.n = nbytes
        self.off = 0

    def mark(self):
        return self.off

    def reset(self, m):
        self.off = m

    def get(self, shape, dt):
        esz = 4 if dt == F32 else 2
        nel = 1
        for s in shape[1:]:
            nel *= s
        nb = (nel * esz + 63) // 64 * 64
        assert self.off + nb <= self.n, ("arena overflow", self.off, nb, self.n)
        v = self.ap[0:shape[0], self.off // 2:(self.off + nel * esz) // 2]
        self.off += nb
        if dt == F32:
            v = v.bitcast(F32)
        if len(shape) == 3:
            v = v.rearrange("p (a b) -> p a b", a=shape[1])
        elif len(shape) == 4:
            v = v.rearrange("p (a b c) -> p a b c", a=shape[1], b=shape[2])
        return v


def build_program(debug_names=()):
    nc = bass.Bass("TRN2", target_bir_lowering=False)

    def din(name, shape):
        return nc.dram_tensor(name, list(shape), F32, kind="ExternalInput").ap()

    x = din("x", [T, D])
    ln_mix_g = din("ln_mix_g", [1, D])[0]
    w_in = din("w_in", [1, D, DIN])[0]
    biasg = din("biasg", [128, 5 * 8 * 128])
    amask = din("amask", [128, 5 * 128])
    rwkv_mu = din("rwkv_mu", [1, 1792])[0]
    rwkv_w0 = din("rwkv_w0", [1, 512])[0]
    rwkv_w2 = din("rwkv_w2", [1, 64, 512])[0]
    rwkv_a0 = din("rwkv_a0", [1, 512])[0]
    rwkv_a2 = din("rwkv_a2", [1, 64, 512])[0]
    rwkv_g2 = din("rwkv_g2", [1, 128, 512])[0]
    rwkv_k_k = din("rwkv_k_k", [1, 512])[0]
    rwkv_k_a = din("rwkv_k_a", [1, 512])[0]
    rwkv_r_k = din("rwkv_r_k", [1, 512])[0]
    rwkv_gn_g = din("rwkv_gn_g", [1, 512])[0]
    rwkv_gn_b = din("rwkv_gn_b", [1, 512])[0]
    w_branch_att = din("w_branch_att", [1, 512, D])[0]
    w_branch_rwkv = din("w_branch_rwkv", [1, 512, D])[0]
    w_out = din("w_out", [1, D, D])[0]
    ln_ffn_g = din("ln_ffn_g", [1, D])[0]
    router_group_w = din("router_group_w", [1, D, 4])[0]
    router_group_b = din("router_group_b", [1, 4])[0]
    router_expert_w = din("router_expert_w", [1, D, 32])[0]
    router_expert_b = din("router_expert_b", [1, 32])[0]
    expert_w_gate = din("expert_w_gate", [1, 32, D, 256])[0]
    expert_w_up = din("expert_w_up", [1, 32, D, 256])[0]
    expert_w_down = din("expert_w_down", [1, 32, 256, D])[0]
    ln_final_g = din("ln_final_g", [D])
    c_ident = din("c_ident", [128, 128])
    c_masks = din("c_masks", [128, 4 * 128])
    c_i2 = din("c_i2", [128, 512])
    out = nc.dram_tensor("out", [T, D], F32, kind="ExternalOutput").ap()
    dbg = {}
    for nm, shp in debug_names:
        dbg[nm] = nc.dram_tensor("dbg_" + nm, list(shp), F32, kind="ExternalOutput").ap()

    with ExitStack() as es:
        k = K(nc, es)
        NB = 207 * 1024
        arena_t = es.enter_context(nc.sbuf_tensor("arena", [128, NB // 2], BF16))
        ar = Arena(arena_t[:, :], NB)
        ps = es.enter_context(nc.psum_tensor("ps", [128, 8, 512], F32))
        pst = [Trk() for _ in range(8)]
        rot = {"i": 0, "n": 6}

        held = []

        def bank():
            while True:
                b = rot["i"] % rot["n"]
                rot["i"] += 1
                if b not in held:
                    return b, pst[b]

        rotB = {"i": 0}

        def bankB():
            b, t = bank()
            held.append(b)
            if len(held) > 3:
                held.pop(0)
            return b, t

        def psf(b):
            return ps[:, b, :]

        def psb(b):
            return ps[:, b, :].bitcast(BF16)

        dbg_stage = {}

        def dump(name, ap_f32, trk):
            if name in dbg:
                k.dma("sp", dbg[name], ap_f32, "dbg_" + name, reads=[trk])

        try:
            ident_b = ar.get([128, 128], BF16)
            masks_f = ar.get([128, 4, 128], F32)
            t_const = Trk()
            t_const2 = Trk()
            k.dma("pool", ident_b, c_ident, "c1", writes=[t_const2])
            t_masks = Trk()
            k.dma("sp", masks_f.rearrange("p a b -> p (a b)"), c_masks, "c2", writes=[t_masks])
            h_mark = ar.mark()
            hT = ar.get([128, KC, T + 1], BF16)
            t_hT = [Trk() for _ in range(NT)]
            t_hz = Trk()
            k.op("dve", lambda e: e.memset(hT[:, :, 0:1], 0.0), writes=[t_hz])
            base_mark = ar.mark()

            def rms_tile(src_ap, t_src, gbc, t_g, ss, t_ss, hb, t_hb, sq_junk, t_junk, j, dstT, t_dst, col0, eps=1e-6):
                k.op("act", lambda e: e.activation(out=sq_junk, in_=src_ap, func=AF.Square, accum_out=ss[:, j:j + 1]),
                     reads=[t_src], writes=[t_junk, t_ss])
                k.op("dve", lambda e: e.tensor_scalar(out=ss[:, j:j + 1], in0=ss[:, j:j + 1], scalar1=1.0 / D, scalar2=eps,
                                                      op0=ALU.mult, op1=ALU.add), reads=[t_ss], writes=[t_ss])
                k.op("act", lambda e: e.sqrt(ss[:, j:j + 1], ss[:, j:j + 1]), reads=[t_ss], writes=[t_ss])
                k.op("dve", lambda e: e.reciprocal(ss[:, j:j + 1], ss[:, j:j + 1]), reads=[t_ss], writes=[t_ss])
                k.op("dve", lambda e: e.scalar_tensor_tensor(out=hb, in0=src_ap, scalar=ss[:, j:j + 1], in1=gbc,
                                                             op0=ALU.mult, op1=ALU.mult),
                     reads=[t_src, t_ss, t_g], writes=[t_hb])
                b, tb = bank()
                for c in range(KC):
                    k.op("pe", lambda e, c=c: e.transpose(psb(b)[:, c * 128:(c + 1) * 128], hb[:, c * 128:(c + 1) * 128], ident_b),
                         reads=[t_hb, t_const2], writes=[tb], inc=(c == KC - 1))
                k.op("act", lambda e: e.activation(out=dstT[:, :, col0:col0 + 128],
                                                   in_=psb(b).rearrange("p (a b) -> p a b", a=KC), func=AF.Copy),
                     reads=[tb], writes=[t_dst])

            def TT(eng, out, in0, in1, op, reads, writes):
                k.op(eng, lambda e: e.tensor_tensor(out=out, in0=in0, in1=in1, op=op), reads=reads, writes=writes)

            def ACT(out, in_, func, reads, writes, scale=1.0):
                k.op("act", lambda e: e.activation(out=out, in_=in_, func=func, scale=scale), reads=reads, writes=writes)

            def CP(on_act, out, in_, reads, writes):
                if on_act:
                    k.op("act", lambda e: e.activation(out=out, in_=in_, func=AF.Copy), reads=reads, writes=writes)
                else:
                    k.op("dve", lambda e: e.tensor_copy(out, in_), reads=reads, writes=writes)

            def h8(ap):
                return ap.rearrange("p (h d) -> p h d", h=8)

            def bc8(ap8):
                return ap8.unsqueeze(2).to_broadcast([128, 8, 64])

            a_save = ar.mark()
            ar.reset(NB - 20 * 1024)
            gbc = ar.get([128, D], F32)
            t_g = Trk()
            k.dma("sp", gbc, ln_mix_g.partition_broadcast(128), "gbc", writes=[t_g])
            ssA = ar.get([128, NT], F32)
            t_ssl = [Trk() for _ in range(NT)]
            k.op("dve", lambda e: e.memset(ssA, 0.0), writes=t_ssl)
            xs = [ar.get([128, D], F32) for _ in range(2)]
            t_xs = [Trk() for _ in range(2)]
            hb = [ar.get([128, D], BF16) for _ in range(2)]
            t_hb = [Trk() for _ in range(2)]
            junk = ar.get([128, D], BF16)
            t_junk = Trk()
            ar.reset(a_save)
            for j in range(NT):
                s = j % 2
                k.dma("sp", xs[s], x[j * 128:(j + 1) * 128, :], "xs%d" % s, writes=[t_xs[s]])
                rms_tile(xs[s], t_xs[s], gbc, t_g, ssA, t_ssl[j], hb[s], t_hb[s], junk, t_junk, j, hT, t_hT[j], 1 + j * 128)
            if "hT" in dbg:
                st = ar.get([128, T], F32)
                tt = Trk()
                k.op("dve", lambda e: e.tensor_copy(st, hT[:, 0, 1:T + 1]), reads=t_hT + [t_hz], writes=[tt])
                dump("hT", st, tt)
            ar.reset(base_mark)
            t_hall = Trk()
            top_limit = NB - 20 * 1024

            def hT_deps(t_lo, t_hi):
                j0 = max(0, (t_lo - 1) // 128)
                j1 = (t_hi - 1) // 128
                return [t_hz] + [t_hT[jj] for jj in range(j0, j1 + 1)]

            if STOP == 'A':
                raise _Stop()
            rwkvT = ar.get([128, 4, T], BF16)
            t_rwT = [Trk() for _ in range(NT)]
            c_mark = ar.mark()
            rkv = ar.get([128, NT, 1536], BF16)
            t_rkv = Trk()
            w2a2 = ar.get([128, 512], BF16)
            g2w = ar.get([128, 512], BF16)
            t_sw = Trk()
            k.dma("pool", w2a2[0:64, :], rwkv_w2, "sw0", writes=[t_sw])
            k.dma("pool", w2a2[64:128, :], rwkv_a2, "sw1", writes=[t_sw])
            k.dma("pool", g2w, rwkv_g2, "sw2", writes=[t_sw])
            bcs = {}
            t_bc = Trk()
            for i, (nm, src) in enumerate([("w0", rwkv_w0), ("a0", rwkv_a0), ("kk", rwkv_k_k), ("ka", rwkv_k_a),
                                           ("rk", rwkv_r_k), ("gng", rwkv_gn_g), ("gnb", rwkv_gn_b)]):
                bcs[nm] = ar.get([128, 512], F32)
                k.dma("sp", bcs[nm], src.partition_broadcast(128), "bc%d" % i, writes=[t_bc])
            i2rep = ar.get([128, 512], F32)
            k.dma("sp", i2rep, c_i2, "bci2", writes=[t_bc])
            loraT = ar.get([128, 2, T], BF16)
            t_lora = Trk()
            m0 = ar.mark()
            W1 = ar.get([128, KC, 1536], BF16)
            t_W = Trk()
            k.dma("pool", W1, w_in[:, 1536:3072].rearrange("(c p) n -> p c n", p=128), "wrkv", writes=[t_W])
            mu_bc = ar.get([128, 1792], F32)
            t_mu = Trk()
            k.dma("sp", mu_bc, rwkv_mu.partition_broadcast(128), "mu", writes=[t_mu])
            WL1 = ar.get([128, KC, 256], BF16)
            WL2 = ar.get([128, KC, 256], BF16)
            t_WL = Trk()
            omu_l = ar.get([128, 256], F32)
            stg = [ar.get([128, 256], F32) for _ in range(2)]
            t_stg = [Trk() for _ in range(2)]
            t_omu = Trk()
            k.op("dve", lambda e: e.tensor_scalar(out=omu_l, in0=mu_bc[:, 1536:1792], scalar1=-1.0, scalar2=1.0, op0=ALU.mult, op1=ALU.add),
                 reads=[t_mu], writes=[t_omu])
            for c in range(KC):
                s = c % 2
                k.dma("sp", stg[s], w_in[c * 128:(c + 1) * 128, 3072:3328], "stg%d" % s, writes=[t_stg[s]])
                k.op("dve", lambda e, c=c, s=s: e.tensor_tensor(out=WL1[:, c, :], in0=stg[s], in1=omu_l, op=ALU.mult),
                     reads=[t_stg[s], t_omu], writes=[t_WL])
                k.op("pool", lambda e, c=c, s=s: e.tensor_tensor(out=WL2[:, c, :], in0=stg[s], in1=mu_bc[:, 1536:1792], op=ALU.mult),
                     reads=[t_stg[s], t_mu], writes=[t_WL])
            for tb in range(4):
                for lc in range(2):
                    b, tbk = bank()
                    c0 = lc * 128
                    n = 0
                    for w, sh in ((WL1, 1), (WL2, 0)):
                        for c in range(KC):
                            k.op("pe", lambda e, w=w, sh=sh, c=c, n=n: e.matmul(
                                psf(b), w[:, c, c0:c0 + 128], hT[:, c, sh + tb * 512: sh + tb * 512 + 512],
                                start=(n == 0), stop=(n == 15)), reads=[t_WL] + hT_deps(tb * 512, (tb + 1) * 512), writes=[tbk], inc=(n == 15))
                            n += 1
                    if lc == 0:
                        k.op("act", lambda e: e.activation(out=loraT[0:64, 0, tb * 512:(tb + 1) * 512], in_=psf(b)[0:64, :],
                                                           func=AF.Tanh), reads=[tbk], writes=[t_lora])
                        k.op("act", lambda e: e.activation(out=loraT[64:128, 0, tb * 512:(tb + 1) * 512], in_=psf(b)[64:128, :],
                                                           func=AF.Copy), reads=[tbk], writes=[t_lora])
                    else:
                        k.op("act", lambda e: e.activation(out=loraT[:, 1, tb * 512:(tb + 1) * 512], in_=psf(b),
                                                           func=AF.Sigmoid), reads=[tbk], writes=[t_lora])
            xb_ = [(ar.get([128, 512], F32), Trk()) for _ in range(2)]
            db_ = [(ar.get([128, 512], F32), Trk()) for _ in range(2)]
            it = 0
            for j in range(NT):
                cur = hT[:, :, 1 + j * 128: 1 + (j + 1) * 128]
                prv = hT[:, :, j * 128:(j + 1) * 128]
                for blk in range(3):
                    bP, tP = bank()
                    for c in range(KC):
                        k.op("pe", lambda e, c=c: e.matmul(
                            psf(bP), cur[:, c, :], W1[:, c, blk * 512:(blk + 1) * 512], start=(c == 0), stop=(c == KC - 1)),
                            reads=[t_W] + hT_deps(j * 128, (j + 1) * 128), writes=[tP], inc=(c == KC - 1))
                    bQ, tQ = bank()
                    for c in range(KC):
                        k.op("pe", lambda e, c=c: e.matmul(
                            psf(bQ), prv[:, c, :], W1[:, c, blk * 512:(blk + 1) * 512], start=(c == 0), stop=(c == KC - 1)),
                            reads=[t_W] + hT_deps(j * 128, (j + 1) * 128), writes=[tQ], inc=(c == KC - 1))
                    (x_, t_x), (dq, t_dq) = xb_[it % 2], db_[it % 2]
                    it += 1
                    ACT(x_, psf(bP), AF.Copy, [tP], [t_x])
                    TT("dve", dq, psf(bQ), x_, ALU.subtract, [tQ, t_x], [t_dq])
                    TT("pool", dq, dq, mu_bc[:, blk * 512:(blk + 1) * 512], ALU.mult, [t_dq, t_mu], [t_dq])
                    TT("pool", rkv[:, j, blk * 512:(blk + 1) * 512], x_, dq, ALU.add, [t_x, t_dq], [t_rkv])
            assert ar.mark() <= top_limit, (ar.mark(), top_limit)
            k.barrier()
            ar.reset(m0)
            t_rkv = Trk()

            if STOP == 'C0':
                raise _Stop()

            def f32t():
                return ar.get([128, 512], F32), Trk()

            def bf16t():
                return ar.get([128, 512], BF16), Trk()

            NBUF = 2
            tm = {}
            for nm in ("kmod", "tz", "sg", "Lp", "eLm", "eLC", "kk", "tmp", "kkn"):
                tm[nm] = [f32t()]
            tm["g"] = [f32t(), f32t()]
            tm["eL"] = tm["kkn"]
            tm["GD"] = tm["tz"]
            tm["a"] = tm["tz"]
            tm["eLm1"] = tm["sg"]
            tm["Ysb"] = tm["Lp"]
            tm["ka"] = tm["kk"]
            tb_ = {}
            for nm in ("aT", "bT", "kT", "rT", "bh", "kh", "GDb", "GDl"):
                tb_[nm] = [bf16t() for _ in range(NBUF)]
            tb_["y4"] = [bf16t()] * 2
            sm = {}
            for nm in ("ss8", "bs8", "ysum", "yss", "mean", "rstd"):
                sm[nm] = [(ar.get([128, 8], F32), Trk()) for _ in range(NBUF)]
            NU = 8 // NGRP
            ub = []
            for u in range(NU):
                d_ = {}
                d_["FM"] = (ar.get([64, 512], BF16), Trk())
                d_["NAKA"] = (ar.get([128, 512], BF16), Trk())
                d_["NN"] = [(ar.get([128, 256], BF16), Trk()) for _ in range(2)]
                d_["T"] = [(ar.get([128, 128], BF16), Trk()) for _ in range(2)]
                d_["X"] = (ar.get([128, 64], BF16), Trk())
                d_["DU"] = (ar.get([128, 128], BF16), Trk())
                d_["MB"] = (ar.get([64, 256], F32), Trk())
                d_["RQ"] = (ar.get([64, 2, 128], BF16), Trk())
                k.op("dve", lambda e, d_=d_: e.memset(d_["RQ"][0], 0.0), writes=[d_["RQ"][1]])
                ub.append(d_)
            Sst = [[(ar.get([64, 64], F32), Trk()) for _ in range(2)] for _ in range(8)]
            Sb = [[(ar.get([64, 64], BF16), Trk()) for _ in range(2)] for _ in range(8)]
            for h in range(8):
                k.op("dve", lambda e, h=h: e.memset(Sst[h][0][0], 0.0), writes=[Sst[h][0][1]])
            s_cur = [0] * 8
            ucount = 0
            YB = [6, 7]
            rot["n"] = 7


            def prep_gen(j):
                s = j % NBUF
                cur = hT[:, :, 1 + j * 128: 1 + (j + 1) * 128]
                prv = hT[:, :, j * 128:(j + 1) * 128]
                r_, k_, v_ = rkv[:, j, 0:512], rkv[:, j, 512:1024], rkv[:, j, 1024:1536]
                t_r = t_k = t_v = t_rkv
                tok = slice(j * 128, (j + 1) * 128)
                b, tbk = bankB()
                k.op("pe", lambda e: e.matmul(psf(b), loraT[0:64, 0, tok], w2a2[0:64, :], start=True, stop=True),
                     reads=[t_lora, t_sw], writes=[tbk])
                yield
                tz, t_tz = tm["tz"][0]
                TT("dve", tz, psf(b), bcs["w0"], ALU.add, [tbk, t_bc], [t_tz])
                yield
                sg, t_sg = tm["sg"][0]
                ACT(sg, tz, AF.Sigmoid, [t_tz], [t_sg])
                yield
                b, tbk = bankB()
                k.op("pe", lambda e: e.matmul(psf(b), loraT[64:128, 0, tok], w2a2[64:128, :], start=True, stop=True),
                     reads=[t_lora, t_sw], writes=[tbk])
                yield
                TT("dve", tz, psf(b), bcs["a0"], ALU.add, [tbk, t_bc], [t_tz])
                yield
                a_, t_a = tm["a"][0]
                ACT(a_, tz, AF.Sigmoid, [t_tz], [t_a])
                yield
                b, tbk = bankB()
                k.op("pe", lambda e: e.matmul(psf(b), loraT[:, 1, tok], g2w, start=True, stop=True),
                     reads=[t_lora, t_sw], writes=[tbk])
                yield
                g_, t_g_ = tm["g"][s]
                ACT(g_, psf(b), AF.Copy, [tbk], [t_g_])
                yield
                bL, tL = bankB()
                k.op("pe", lambda e: e.matmul(psf(bL), masks_f[:, 1, :], sg, start=True, stop=True),
                     reads=[t_masks, t_sg], writes=[tL])
                yield
                bC, tC = bankB()
                k.op("pe", lambda e: e.matmul(psf(bC), masks_f[:, 3, :], sg, start=True, stop=True),
                     reads=[t_masks, t_sg], writes=[tC])
                yield
                Lp, t_Lp = tm["Lp"][0]
                ACT(Lp, psf(bL), AF.Copy, [tL], [t_Lp])
                yield
                eL, t_eL = tm["eL"][0]
                ACT(eL, psf(bL), AF.Exp, [tL], [t_eL], scale=-CDEC)
                yield
                rT, t_rT = tb_["rT"][s]
                TT("dve", rT, r_, eL, ALU.mult, [t_r, t_eL], [t_rT])
                yield
                eLm, t_eLm = tm["eLm"][0]
                ACT(eLm, psf(bL), AF.Exp, [tL], [t_eLm], scale=CDEC)
                yield
                tmp, t_tmp = tm["tmp"][0]
                TT("dve", tmp, Lp, sg, ALU.subtract, [t_Lp, t_sg], [t_tmp])
                yield
                eLm1, t_eLm1 = tm["eLm1"][0]
                ACT(eLm1, tmp, AF.Exp, [t_tmp], [t_eLm1], scale=-CDEC)
                yield
                TT("dve", tmp, psf(bC), Lp, ALU.subtract, [tC, t_Lp], [t_tmp])
                yield
                eLC, t_eLC = tm["eLC"][0]
                ACT(eLC, tmp, AF.Exp, [t_tmp], [t_eLC], scale=-CDEC)
                yield
                kk, t_kk = tm["kk"][0]
                TT("dve", kk, k_, bcs["kk"], ALU.mult, [t_k, t_bc], [t_kk])
                yield
                TT("dve", tmp, kk, kk, ALU.mult, [t_kk], [t_tmp])
                yield
                ss8, t_ss8 = sm["ss8"][s]
                k.op("dve", lambda e: e.tensor_reduce(out=ss8, in_=h8(tmp), axis=AX.X, op=ALU.add), reads=[t_tmp], writes=[t_ss8])
                yield
                k.op("dve", lambda e: e.tensor_scalar(out=ss8, in0=ss8, scalar1=1e-24, scalar2=None, op0=ALU.max),
                     reads=[t_ss8], writes=[t_ss8])
                yield
                k.op("act", lambda e: e.sqrt(ss8, ss8), reads=[t_ss8], writes=[t_ss8])
                yield
                k.op("dve", lambda e: e.reciprocal(ss8, ss8), reads=[t_ss8], writes=[t_ss8])
                yield
                kkn, t_kkn = tm["kkn"][0]
                TT("dve", h8(kkn), h8(kk), bc8(ss8), ALU.mult, [t_kk, t_ss8], [t_kkn])
                yield
                kmod, t_kmod = tm["kmod"][0]
                k.op("dve", lambda e: e.scalar_tensor_tensor(out=tmp, in0=a_, scalar=-1.0, in1=bcs["ka"], op0=ALU.add, op1=ALU.mult),
                     reads=[t_a, t_bc], writes=[t_tmp])
                yield
                k.op("dve", lambda e: e.scalar_tensor_tensor(out=kmod, in0=tmp, scalar=1.0, in1=k_, op0=ALU.add, op1=ALU.mult),
                     reads=[t_tmp, t_k], writes=[t_kmod])
                yield
                ka, t_ka = tm["ka"][0]
                TT("dve", ka, kkn, a_, ALU.mult, [t_kkn, t_a], [t_ka])
                yield
                GD, t_GD = tm["GD"][0]
                ACT(GD, psf(bC), AF.Exp, [tC], [t_GD], scale=-CDEC)
                yield
                GDb, t_GDb = tb_["GDb"][s]
                TT("dve", GD, GD, i2rep, ALU.mult, [t_GD, t_bc], [t_GD])
                yield
                GDl, t_GDl = tb_["GDl"][s]
                k.op("dve", lambda e: e.tensor_copy(GDb, GD), reads=[t_GD], writes=[t_GDb])
                yield
                TT("dve", GD, GD, GDb, ALU.subtract, [t_GD, t_GDb], [t_GD])
                yield
                k.op("dve", lambda e: e.tensor_copy(GDl, GD), reads=[t_GD], writes=[t_GDl])
                yield
                aT, t_aT = tb_["aT"][s]
                k.op("dve", lambda e: e.scalar_tensor_tensor(out=aT, in0=kkn, scalar=-1.0, in1=eLm1, op0=ALU.mult, op1=ALU.mult),
                     reads=[t_kkn, t_eLm1], writes=[t_aT])
                yield
                bT, t_bT = tb_["bT"][s]
                TT("dve", bT, ka, eLm, ALU.mult, [t_ka, t_eLm], [t_bT])
                yield
                kT_, t_kT = tb_["kT"][s]
                TT("dve", kT_, kmod, eLm, ALU.mult, [t_kmod, t_eLm], [t_kT])
                yield
                bh, t_bh = tb_["bh"][s]
                TT("dve", bh, ka, eLC, ALU.mult, [t_ka, t_eLC], [t_bh])
                yield
                kh, t_kh = tb_["kh"][s]
                TT("dve", kh, kmod, eLC, ALU.mult, [t_kmod, t_eLC], [t_kh])
                yield
                TT("dve", tmp, r_, kmod, ALU.mult, [t_r, t_kmod], [t_tmp])
                yield
                TT("dve", tmp, tmp, bcs["rk"], ALU.mult, [t_tmp, t_bc], [t_tmp])
                yield
                bs8, t_bs8 = sm["bs8"][s]
                k.op("dve", lambda e: e.tensor_reduce(out=bs8, in_=h8(tmp), axis=AX.X, op=ALU.add), reads=[t_tmp], writes=[t_bs8])
                yield
                if j == 0:
                    dump("sg", sg, t_sg)
                    yield
                    dump("kkn", kkn, t_kkn)
                    yield
                    dump("kmod", kmod, t_kmod)
                    yield
                    dump("eLm1", eLm1, t_eLm1)
                    yield
                    dump("eLC", eLC, t_eLC)
                    yield

                if STOP == 'C1':
                    raise _Stop()
            def post_gen(j):
                s = j % NBUF
                yb = 7
                t_yb = pst[yb]
                v_, t_v = rkv[:, j, 1024:1536], t_rkv
                g_, t_g_ = tm['g'][s]
                bs8, t_bs8 = sm['bs8'][s]
                tmp, t_tmp = tm['tmp'][0]
                Ysb, t_Y = tm["Ysb"][0]
                ACT(Ysb, psf(yb), AF.Copy, [t_yb], [t_Y])
                yield
                if j == 0:
                    dump("Y0", Ysb, t_Y)
                    yield
                ysum, t_ysum = sm["ysum"][s]
                k.op("dve", lambda e: e.tensor_reduce(out=ysum, in_=h8(Ysb), axis=AX.X, op=ALU.add), reads=[t_Y], writes=[t_ysum])
                yield
                TT("dve", tmp, Ysb, Ysb, ALU.mult, [t_Y], [t_tmp])
                yield
                yss, t_yss = sm["yss"][s]
                k.op("dve", lambda e: e.tensor_reduce(out=yss, in_=h8(tmp), axis=AX.X, op=ALU.add), reads=[t_tmp], writes=[t_yss])
                yield
                mean, t_mean = sm["mean"][s]
                k.op("dve", lambda e: e.tensor_scalar(out=mean, in0=ysum, scalar1=1.0 / 64, scalar2=None, op0=ALU.mult),
                     reads=[t_ysum], writes=[t_mean])
                yield
                rstd, t_rstd = sm["rstd"][s]
                TT("dve", rstd, mean, mean, ALU.mult, [t_mean], [t_rstd])
                yield
                k.op("dve", lambda e: e.scalar_tensor_tensor(out=rstd, in0=yss, scalar=1.0 / 64, in1=rstd, op0=ALU.mult, op1=ALU.subtract),
                     reads=[t_yss, t_rstd], writes=[t_rstd])
                yield
                k.op("dve", lambda e: e.tensor_scalar(out=rstd, in0=rstd, scalar1=64e-5, scalar2=None, op0=ALU.add),
                     reads=[t_rstd], writes=[t_rstd])
                yield
                k.op("act", lambda e: e.sqrt(rstd, rstd), reads=[t_rstd], writes=[t_rstd])
                yield
                k.op("dve", lambda e: e.reciprocal(rstd, rstd), reads=[t_rstd], writes=[t_rstd])
                yield
                TT("dve", h8(Ysb), h8(Ysb), bc8(mean), ALU.subtract, [t_Y, t_mean], [t_Y])
                yield
                TT("dve", h8(Ysb), h8(Ysb), bc8(rstd), ALU.mult, [t_Y, t_rstd], [t_Y])
                yield
                TT("dve", Ysb, Ysb, bcs["gng"], ALU.mult, [t_Y, t_bc], [t_Y])
                yield
                TT("dve", Ysb, Ysb, bcs["gnb"], ALU.add, [t_Y, t_bc], [t_Y])
                yield
                TT("dve", h8(tmp), h8(v_), bc8(bs8), ALU.mult, [t_v, t_bs8], [t_tmp])
                yield
                TT("dve", Ysb, Ysb, tmp, ALU.add, [t_Y, t_tmp], [t_Y])
                yield
                y4, t_y4 = tb_["y4"][s]
                TT("dve", y4, Ysb, g_, ALU.mult, [t_Y, t_g_], [t_y4])
                yield
                if j == 0:
                    st = tm["eLC"][0][0]
                    k.op("dve", lambda e: e.tensor_copy(st, y4), reads=[t_y4], writes=[tm["eLC"][0][1]])
                    yield
                    dump("rwkv0", st, tm["eLC"][0][1])
                    yield
                b, tbk = bankB()
                for c in range(4):
                    k.op("pe", lambda e, c=c: e.transpose(psb(b)[:, c * 128:(c + 1) * 128], y4[:, c * 128:(c + 1) * 128], ident_b),
                         reads=[t_y4, t_const2], writes=[tbk], inc=(c == 3))
                    yield
                k.op("act", lambda e: e.activation(out=rwkvT[:, :, j * 128:(j + 1) * 128],
                                                   in_=psb(b)[:, 0:512].rearrange("p (a b) -> p a b", a=4), func=AF.Copy),
                     reads=[tbk], writes=[t_rwT[j]])
                yield
            def units(j, pump):
                s = j % NBUF
                aT, t_aT = tb_['aT'][s]
                bT, t_bT = tb_['bT'][s]
                kT_, t_kT = tb_['kT'][s]
                rT, t_rT = tb_['rT'][s]
                bh, t_bh = tb_['bh'][s]
                kh, t_kh = tb_['kh'][s]
                vb, t_vb = rkv[:, j, 1024:1536], t_rkv
                GDb, t_GDb = tb_['GDb'][s]
                GDl, t_GDl = tb_['GDl'][s]
                YB1 = 7
                yb = YB1
                t_yb = pst[yb]
                for grp in range(NGRP):
                    HPG = 8 // NGRP
                    heads = [HPG * grp + i_ for i_ in range(HPG)]
                    UH = [(ub[i_], heads[i_], slice(heads[i_] * 64, heads[i_] * 64 + 64)) for i_ in range(HPG)]
                    pump()
                    for U, h, hs in UH:
                        FM, t_FM = U["FM"]
                        b, tbk = bank()
                        for i, (src, tsrc) in enumerate(((aT, t_aT), (rT, t_rT), (bT, t_bT), (kT_, t_kT))):
                            k.op("pe", lambda e, i=i, src=src: e.transpose(psb(b)[0:64, i * 128:(i + 1) * 128], src[:, hs], ident_b),
                                 reads=[tsrc, t_const2], writes=[tbk], inc=(i == 3))
                        CP(h % 2 == 0, FM, psb(b)[0:64, 0:512], [tbk], [t_FM])
                    pump()
                    for U, h, hs in UH:
                        FM, t_FM = U["FM"]
                        b, tbk = bank()
                        k.op("pe", lambda e: e.matmul(psf(b)[:, 0:256], FM[:, 256:384], FM[:, 0:256], start=True, stop=True),
                             reads=[t_FM], writes=[tbk], inc=False)
                        k.op("pe", lambda e: e.matmul(psf(b)[:, 256:512], FM[:, 384:512], FM[:, 0:256], start=True, stop=True),
                             reads=[t_FM], writes=[tbk])
                        NAKA, t_NAKA = U["NAKA"]
                        k.op("dve", lambda e: e.tensor_tensor(
                            out=NAKA.rearrange("p (r a b) -> p r a b", r=2, a=2), in0=psf(b).rearrange("p (r a b) -> p r a b", r=2, a=2),
                            in1=masks_f[:, 0:2, :].unsqueeze(1).to_broadcast([128, 2, 2, 128]), op=ALU.mult),
                            reads=[tbk, t_masks], writes=[t_NAKA])
                    for U, h, hs in UH:
                        FM, t_FM = U["FM"]
                        NAKA, t_NAKA = U["NAKA"]
                        b, tbk = bank()
                        k.op("pe", lambda e: e.matmul(psf(b)[:, 0:128], FM[:, 0:128], FM[:, 256:384], start=True, stop=True),
                             reads=[t_FM], writes=[tbk])
                        bx, tbx = bank()
                        k.op("pe", lambda e: e.matmul(psf(bx)[:, 128:192], NAKA[:, 256:384], vb[:, hs], start=True, stop=True),
                             reads=[t_NAKA, t_vb], writes=[tbx])
                        NN0, t_NN0 = U["NN"][0]
                        TT("dve", NN0[:, 0:128], psf(b)[:, 0:128], masks_f[:, 2, :], ALU.mult, [tbk, t_masks], [t_NN0])
                        T0, t_T0 = U["T"][0]
                        TT("dve", T0, NAKA[:, 0:128], ident_b, ALU.add, [t_NAKA, t_const2], [t_T0])
                        X, t_X = U["X"]
                        CP(h % 2 == 1, X, psf(bx)[:, 128:192], [tbx], [t_X])
                    if STOP == 'C2':
                        raise _Stop()
                    pump()
                    for lv in range(1, 6):
                        bks = []
                        for U, h, hs in UH:
                            NNp, t_NNp = U["NN"][(lv - 1) % 2]
                            NNc, t_NNc = U["NN"][lv % 2]
                            NTprev = NNp[:, 0:128]
                            if lv == 1:
                                Nprev, rdp = U["NAKA"][0][:, 0:128], [t_NNp, U["NAKA"][1]]
                            else:
                                Nprev, rdp = NNp[:, 128:256], [t_NNp]
                            b, tbk = bank()
                            bks.append((b, tbk))
                            k.op("pe", lambda e: e.matmul(psf(b)[:, 0:128], Nprev, NTprev, start=True, stop=True),
                                 reads=rdp, writes=[tbk], inc=(lv == 5))
                            if lv < 5:
                                k.op("pe", lambda e: e.matmul(psf(b)[:, 128:256], NTprev, Nprev, start=True, stop=True),
                                     reads=rdp, writes=[tbk])
                                ACT(NNc, psf(b)[:, 0:256], AF.Copy, [tbk], [t_NNc])
                            else:
                                ACT(NNc[:, 0:128], psf(b)[:, 0:128], AF.Copy, [tbk], [t_NNc])
                        for (U, h, hs), (b, tbk) in zip(UH, bks):
                            NNc, t_NNc = U["NN"][lv % 2]
                            Tp, t_Tp = U["T"][(lv - 1) % 2]
                            Tc, t_Tc = U["T"][lv % 2]
                            k.op("pe", lambda e: e.matmul(psf(b)[:, 256:384], NNc[:, 0:128], Tp, start=True, stop=True),
                                 reads=[t_NNc, t_Tp], writes=[tbk])
                            TT("dve", Tc, psf(b)[:, 256:384], Tp, ALU.add, [tbk, t_Tp], [t_Tc])
                    pump()
                    for U, h, hs in UH:
                        T5, t_T5 = U["T"][1]
                        X, t_X = U["X"]
                        DU, t_DU = U["DU"]
                        b, tbk = bank()
                        k.op("pe", lambda e: e.matmul(psf(b)[:, 0:64], T5, aT[:, hs], start=True, stop=True),
                             reads=[t_T5, t_aT], writes=[tbk], inc=False)
                        k.op("pe", lambda e: e.matmul(psf(b)[:, 64:128], T5, X, start=True, stop=True),
                             reads=[t_T5, t_X], writes=[tbk])
                        CP(h % 2 == 0, DU, psf(b)[:, 0:128], [tbk], [t_DU])
                    if STOP == 'C3':
                        raise _Stop()
                    pump()
                    for U, h, hs in UH:
                        DU, t_DU = U["DU"]
                        MB, t_MB = U["MB"]
                        for i in range(2):
                            b, tbk = bank()
                            rs = slice(64 * i, 64 * i + 64)
                            mms = [(0, DU[rs, 0:64], bh[rs, hs], [t_DU, t_bh]),
                                   (0, ident_b[rs, rs], GDb[rs, hs], [t_const2, t_GDb]),
                                   (0, ident_b[rs, rs], GDl[rs, hs], [t_const2, t_GDl]),
                                   (64, bh[rs, hs], DU[rs, 64:128], [t_DU, t_bh]),
                                   (64, kh[rs, hs], vb[rs, hs], [t_kh, t_vb])]
                            for n_, (co, l_, r_2, rd) in enumerate(mms):
                                first = (n_ == 0) or (mms[n_ - 1][0] != co)
                                lastg = (n_ == len(mms) - 1) or (mms[n_ + 1][0] != co)
                                k.op("pe", lambda e: e.matmul(psf(b)[0:64, co:co + 64], l_, r_2, start=first, stop=lastg),
                                     reads=rd, writes=[tbk], inc=(n_ == len(mms) - 1))
                            CP(i == 0, MB[:, i * 128:(i + 1) * 128], psf(b)[0:64, 0:128], [tbk], [t_MB])
                    if STOP == 'C4':
                        raise _Stop()
                    for U, h, hs in UH:
                        FM, t_FM = U["FM"]
                        DU, t_DU = U["DU"]
                        NAKA, t_NAKA = U["NAKA"]
                        RQ, t_RQ = U["RQ"]
                        b, tbk = bank()
                        k.op("pe", lambda e: e.matmul(psf(b)[0:64, 0:128], DU[:, 0:64], NAKA[:, 128:256], start=True, stop=True),
                             reads=[t_DU, t_NAKA], writes=[tbk])
                        k.op("dve", lambda e: e.tensor_tensor(
                            out=RQ.rearrange("p a b -> p (a b)")[:, 0:256].rearrange("p (a b) -> p a b", a=2)[:, :, 0:64] if False else RQ[:, 0, 0:64],
                            in0=psf(b)[0:64, 0:64], in1=FM[:, 128:192], op=ALU.add), reads=[tbk, t_FM], writes=[t_RQ])
                        k.op("dve", lambda e: e.tensor_tensor(out=RQ[:, 1, 64:128], in0=psf(b)[0:64, 64:128], in1=FM[:, 192:256], op=ALU.add),
                             reads=[tbk, t_FM], writes=[t_RQ])
                    pump()
                    for i in range(2):
                        for U, h, hs in UH:
                            MB, t_MB = U["MB"]
                            Sc, t_Sc = Sst[h][s_cur[h]]
                            Sn, t_Sn = Sst[h][1 - s_cur[h]]
                            Sbi, t_Sbi = Sb[h][i]
                            k.op("act", lambda e: e.activation(out=Sbi, in_=Sc, func=AF.Copy), reads=[t_Sc], writes=[t_Sbi])
                            b, tbk = bank()
                            k.op("pe", lambda e: e.matmul(psf(b)[0:64, 0:64], MB[:, i * 128:i * 128 + 64], Sc, start=True, stop=True),
                                 reads=[t_MB, t_Sc], writes=[tbk])
                            TT("dve", Sn, psf(b)[0:64, 0:64], MB[:, i * 128 + 64:i * 128 + 128], ALU.add, [tbk, t_MB], [t_Sn])
                            s_cur[h] = 1 - s_cur[h]
                    pump()
                    for U, h, hs in UH:
                        DU, t_DU = U["DU"]
                        NAKA, t_NAKA = U["NAKA"]
                        RQ, t_RQ = U["RQ"]
                        yo = psf(yb)[:, hs]
                        k.op("pe", lambda e: e.matmul(yo, RQ[:, 0, :], Sb[h][0][0], start=True, stop=False),
                             reads=[t_RQ, Sb[h][0][1]], writes=[t_yb], inc=False)
                        k.op("pe", lambda e: e.matmul(yo, RQ[:, 1, :], Sb[h][1][0], start=False, stop=False),
                             reads=[t_RQ, Sb[h][1][1]], writes=[t_yb], inc=False)
                        k.op("pe", lambda e: e.matmul(yo, NAKA[:, 128:256], DU[:, 64:128], start=False, stop=False),
                             reads=[t_NAKA, t_DU], writes=[t_yb], inc=False)
                        k.op("pe", lambda e: e.matmul(yo, NAKA[:, 384:512], vb[:, hs], start=False, stop=True),
                             reads=[t_NAKA, t_vb], writes=[t_yb])
                pump()
            def exhaust(g_):
                for _ in g_:
                    pass
            rot["n"] = 7
            exhaust(prep_gen(0))
            pend_post = None
            for j in range(NT):
                gens = []
                if pend_post is not None:
                    gens.append(pend_post)
                if j + 1 < NT:
                    gens.append(prep_gen(j + 1))
                def pump(n=PUMP):
                    for _ in range(n):
                        while gens:
                            try:
                                next(gens[0])
                                break
                            except StopIteration:
                                gens.pop(0)
                units(j, pump)
                for g_ in gens:
                    exhaust(g_)
                if STOP == 'C5':
                    raise _Stop()
                pend_post = post_gen(j)
                next(pend_post)
            exhaust(pend_post)
            del held[:]
            k.barrier()
            ar.reset(c_mark)

            if STOP == 'C':
                raise _Stop()
            attT = ar.get([128, 4, T], BF16)
            rot["n"] = 6
            b_mark = ar.mark()
            Wqk = ar.get([128, KC, 1024], BF16)
            Wv = ar.get([128, KC, 512], BF16)
            t_Wqkc = [Trk() for _ in range(8)]
            t_Wv = Trk()
            for c in range(8):
                k.dma("pool", Wqk[:, :, c * 128:(c + 1) * 128], w_in[:, c * 128:(c + 1) * 128].rearrange("(c p) n -> p c n", p=128),
                      "wqk%d" % c, writes=[t_Wqkc[c]])
            k.dma("pool", Wv, w_in[:, 1024:1536].rearrange("(c p) n -> p c n", p=128), "wv", writes=[t_Wv])
            biasT = ar.get([128, 5, 8, 128], F32)
            am = ar.get([128, 5, 128], F32)
            t_bias = Trk()
            t_am = Trk()
            k.dma("sp", biasT.rearrange("p a b c -> p (a b c)"), biasg, "bias", writes=[t_bias])
            k.dma("sp", am.rearrange("p a b -> p (a b)"), amask, "am", writes=[t_am])
            k.op("dve", lambda e: e.tensor_tensor(out=biasT, in0=biasT, in1=am.unsqueeze(2).to_broadcast([128, 5, 8, 128]), op=ALU.add),
                 reads=[t_bias, t_am], writes=[t_bias])
            qkT = ar.get([128, 8, T], BF16)
            t_qk = Trk()
            Vaug = ar.get([128, NT, 8, 65], BF16)
            t_V = Trk()
            k.op("dve", lambda e: e.memset(Vaug.rearrange("p a b c -> p (a b c)"), 1.0), writes=[t_V])
            for c in range(8):
                for tb in range(4):
                    b, tbk = bank()
                    for kc in range(KC):
                        k.op("pe", lambda e, kc=kc: e.matmul(psf(b), Wqk[:, kc, c * 128:(c + 1) * 128],
                                                             hT[:, kc, 1 + tb * 512:1 + (tb + 1) * 512],
                                                             start=(kc == 0), stop=(kc == KC - 1)),
                             reads=[t_Wqkc[c], t_hall], writes=[tbk], inc=(kc == KC - 1))
                    ACT(qkT[:, c, tb * 512:(tb + 1) * 512], psf(b), AF.Copy, [tbk], [t_qk])
            for j in range(NT):
                b, tbk = bank()
                for kc in range(KC):
                    k.op("pe", lambda e, kc=kc: e.matmul(psf(b), hT[:, kc, 1 + j * 128:1 + (j + 1) * 128], Wv[:, kc, :],
                                                         start=(kc == 0), stop=(kc == KC - 1)),
                         reads=[t_Wv, t_hall], writes=[tbk], inc=(kc == KC - 1))
                k.op("act", lambda e: e.activation(out=Vaug[:, j, :, 0:64], in_=h8(psf(b)), func=AF.Copy), reads=[tbk], writes=[t_V])
            rden_b = [(ar.get([128, 4], F32), Trk()) for _ in range(2)]
            atok = [(ar.get([128, 512], BF16), Trk()) for _ in range(2)]
            items_att = []
            for j in range(NT):
                o_first = max(0, 4 - j)
                for o in range(o_first, 5):
                    items_att.append((j, o, o_first))
            sc_b = [[(ar.get([128, 512], F32), Trk()) for _ in range(2)] for _ in range(2)]
            pt_b = [[(ar.get([128, 512], BF16), Trk()) for _ in range(3)] for _ in range(2)]
            att_bk = {}

            def att_S(idx):
                j, o, o_first = items_att[idx]
                kt = j - 4 + o
                bE, tE = bank()
                bO, tO = bank()
                att_bk[idx] = None
                for hh in range(4):
                    for half, (b, tbk) in enumerate(((bE, tE), (bO, tO))):
                        h = 2 * hh + half
                        pr = slice(64 * half, 64 * half + 64)
                        k.op("pe", lambda e, hh=hh, h=h, pr=pr, b=b: e.matmul(
                            psf(b)[:, hh * 128:(hh + 1) * 128], qkT[pr, 4 + h // 2, kt * 128:(kt + 1) * 128],
                            qkT[pr, h // 2, j * 128:(j + 1) * 128], start=True, stop=True),
                            reads=[t_qk], writes=[tbk], inc=(hh == 3))
                for half, (b, tbk) in enumerate(((bE, tE), (bO, tO))):
                    sc, t_sc = sc_b[half][idx % 2]
                    pt, t_pt = pt_b[half][idx % 3]
                    k.op("dve", lambda e, b=b, half=half, sc=sc: e.scalar_tensor_tensor(
                        out=sc.rearrange("p (a b) -> p a b", a=4), in0=psf(b).rearrange("p (a b) -> p a b", a=4), scalar=0.125,
                        in1=biasT[:, o, half::2, :],
                        op0=ALU.mult, op1=ALU.add), reads=[tbk, t_bias], writes=[t_sc])
                    ACT(pt, sc, AF.Exp, [t_sc], [t_pt])

            def att_P(idx):
                j, o, o_first = items_att[idx]
                kt = j - 4 + o
                at, t_at = atok[j % 2]
                for half in range(2):
                    pt, t_pt = pt_b[half][idx % 3]
                    ob = YB[half]
                    t_ob = pst[ob]
                    for hh in range(4):
                        h = 2 * hh + half
                        k.op("pe", lambda e, hh=hh, h=h, ob=ob, pt=pt: e.matmul(
                            psf(ob)[:, hh * 65:(hh + 1) * 65], pt[:, hh * 128:(hh + 1) * 128], Vaug[:, kt, h, :],
                            start=(o == o_first and hh == 0), stop=(o == 4), skip_group_check=True), reads=[t_pt, t_V], writes=[t_ob],
                            inc=(o == 4 and hh == 3))
                if o != 4:
                    return
                for half in range(2):
                    ob = YB[half]
                    t_ob = pst[ob]
                    rden, t_rden = rden_b[half]
                    ov = psf(ob)[:, 0:260].rearrange("p (a b) -> p a b", a=4)
                    k.op("dve", lambda e, ov=ov, rden=rden: e.reciprocal(rden, ov[:, :, 64]), reads=[t_ob], writes=[t_rden])
                    k.op("dve", lambda e, ov=ov, rden=rden, half=half: e.tensor_tensor(
                        out=at.rearrange("p (a b d) -> p a b d", a=4, b=2)[:, :, half, :], in0=ov[:, :, 0:64],
                        in1=rden.unsqueeze(2).to_broadcast([128, 4, 64]), op=ALU.mult),
                        reads=[t_ob, t_rden], writes=[t_at])
                if j == 5:
                    stt = sc_b[0][0][0]
                    k.op("dve", lambda e: e.tensor_copy(stt, at), reads=[t_at], writes=[sc_b[0][0][1]])
                    dump("att5", stt, sc_b[0][0][1])
                b, tbk = bank()
                for c in range(4):
                    k.op("pe", lambda e, c=c: e.transpose(psb(b)[:, c * 128:(c + 1) * 128], at[:, c * 128:(c + 1) * 128], ident_b),
                         reads=[t_at, t_const2], writes=[tbk], inc=(c == 3))
                k.op("act", lambda e: e.activation(out=attT[:, :, j * 128:(j + 1) * 128],
                                                   in_=psb(b)[:, 0:512].rearrange("p (a b) -> p a b", a=4), func=AF.Copy),
                     reads=[tbk], writes=[t_V])

            n_att = len(items_att)
            SK = 2
            for step in range(n_att + SK):
                if step < n_att:
                    att_S(step)
                if step >= SK:
                    att_P(step - SK)
            k.barrier()
            ar.reset(b_mark)

            if STOP == 'B':
                raise _Stop()
            md_mark = ar.mark()
            mergedT = ar.get([128, KC, T], BF16)
            t_mg = Trk()
            Wo = ar.get([128, KC, D], BF16)
            t_Wo = Trk()
            d_mark = ar.mark()
            Wg = ar.get([128, KC, 2048], BF16)
            Wba = ar.get([128, 4, D], BF16)
            Wbr = ar.get([128, 4, D], BF16)
            t_WgA = [Trk() for _ in range(8)]
            t_WgB = [Trk() for _ in range(8)]
            t_Wb = Trk()
            t_Wb2 = Trk()
            k.dma("pool", Wba, w_branch_att.rearrange("(c p) n -> p c n", p=128), "wba", writes=[t_Wb])
            k.dma("pool", Wbr, w_branch_rwkv.rearrange("(c p) n -> p c n", p=128), "wbr", writes=[t_Wb2])
            for m in range(8):
                for off in (0, 1024):
                    cs = slice(off + m * 128, off + (m + 1) * 128)
                    k.dma("pool", Wg[:, :, cs], w_in[:, 3328 + cs.start:3328 + cs.stop].rearrange("(c p) n -> p c n", p=128),
                          "wg%d_%d" % (m, off // 1024), writes=[t_WgA[m] if off == 0 else t_WgB[m]])
            k.dma("pool", Wo, w_out.rearrange("(c p) n -> p c n", p=128), "wo", writes=[t_Wo])
            ga_b = [(ar.get([128, 512], F32), Trk()) for _ in range(2)]
            gb_b = [(ar.get([128, 512], F32), Trk()) for _ in range(2)]
            t_all = Trk()
            it = 0
            rot["n"] = 8
            for tb in range(4):
                cols = slice(tb * 512, (tb + 1) * 512)
                hcols = slice(1 + tb * 512, 1 + (tb + 1) * 512)
                for m in range(8):
                    bA, tA = bank()
                    for c in range(4):
                        k.op("pe", lambda e, c=c: e.matmul(psf(bA), Wba[:, c, m * 128:(m + 1) * 128], attT[:, c, cols],
                                                           start=(c == 0), stop=(c == 3)), reads=[t_Wb, t_all], writes=[tA], inc=(c == 3))
                    bB, tB = bank()
                    for c in range(4):
                        k.op("pe", lambda e, c=c: e.matmul(psf(bB), Wbr[:, c, m * 128:(m + 1) * 128], rwkvT[:, c, cols],
                                                           start=(c == 0), stop=(c == 3)), reads=[t_Wb2, t_all], writes=[tB], inc=(c == 3))
                    bGA, tGA = bank()
                    for c in range(KC):
                        k.op("pe", lambda e, c=c: e.matmul(psf(bGA), Wg[:, c, m * 128:(m + 1) * 128], hT[:, c, hcols],
                                                           start=(c == 0), stop=(c == KC - 1)), reads=[t_WgA[m], t_all], writes=[tGA], inc=(c == KC - 1))
                    bGB, tGB = bank()
                    for c in range(KC):
                        k.op("pe", lambda e, c=c: e.matmul(psf(bGB), Wg[:, c, 1024 + m * 128:1024 + (m + 1) * 128], hT[:, c, hcols],
                                                           start=(c == 0), stop=(c == KC - 1)), reads=[t_WgB[m], t_all], writes=[tGB], inc=(c == KC - 1))
                    ga, t_ga = ga_b[it % 2]
                    gb, t_gb = gb_b[it % 2]
                    it += 1
                    ACT(ga, psf(bGA), AF.Sigmoid, [tGA], [t_ga])
                    ACT(gb, psf(bGB), AF.Sigmoid, [tGB], [t_gb])
                    TT("dve", ga, ga, psf(bA), ALU.mult, [t_ga, tA], [t_ga])
                    TT("dve", gb, gb, psf(bB), ALU.mult, [t_gb, tB], [t_gb])
                    TT("dve", mergedT[:, m, cols], ga, gb, ALU.add, [t_ga, t_gb], [t_mg])
            if "merged" in dbg:
                st = ga_b[0][0]
                k.op("dve", lambda e: e.tensor_copy(st, mergedT[:, 0, 0:512]), reads=[t_mg], writes=[ga_b[0][1]])
                dump("merged", st, ga_b[0][1])
            k.barrier()
            ar.reset(d_mark)

            if STOP == 'D1':
                raise _Stop()
            ar.reset(h_mark)
            x2 = ar.get([128, NT, D], F32)
            assert ar.mark() <= md_mark, (ar.mark(), md_mark)
            x2_end = ar.mark()
            t_x2 = [Trk() for _ in range(NT)]
            ar.reset(d_mark)
            xs = [ar.get([128, D], F32) for _ in range(2)]
            for j in range(NT):
                s = j % 2
                k.dma("sp", xs[s], x[j * 128:(j + 1) * 128, :], "xs%d" % s, writes=[t_xs[s]])
                for half in range(2):
                    b, tbk = bank()
                    for m in range(KC):
                        k.op("pe", lambda e, m=m: e.matmul(psf(b), mergedT[:, m, j * 128:(j + 1) * 128], Wo[:, m, half * 512:(half + 1) * 512],
                                                           start=(m == 0), stop=(m == KC - 1)), reads=[t_Wo, t_all], writes=[tbk], inc=(m == KC - 1))
                    TT("dve", x2[:, j, half * 512:(half + 1) * 512], psf(b), xs[s][:, half * 512:(half + 1) * 512], ALU.add,
                       [tbk, t_xs[s]], [t_x2[j]])
            dump("x2", x2[:, 0, :], t_x2[0])
            k.barrier()
            ar.reset(x2_end)

            if STOP == 'D2':
                raise _Stop()
            h2T = ar.get([128, KC, T], BF16)
            t_h2 = [Trk() for _ in range(NT)]
            ssE = ar.get([128, NT], F32)
            t_ssE = [Trk() for _ in range(NT)]
            k.op("dve", lambda e: e.memset(ssE, 0.0), writes=t_ssE)
            Wr = ar.get([128, KC, 36], BF16)
            rbc = ar.get([128, 36], F32)
            lg = ar.get([128, NT, 36], F32)
            NG = NT * 4
            gmax = ar.get([128, NT], F32)
            gsh = ar.get([128, NT, 4], F32)
            gm = ar.get([128, NT, 4], F32)
            gsum = ar.get([128, NT], F32)
            el = ar.get([128, NG, 8], F32)
            m1 = ar.get([128, NG], F32)
            m2 = ar.get([128, NG], F32)
            e1 = ar.get([128, NG, 8], F32)
            e2 = ar.get([128, NG, 8], F32)
            w2_ = ar.get([128, NG], F32)
            comb = ar.get([128, NG, 8], F32)
            ex_mark = ar.mark()
            NS = 6
            GS = 3
            slots = []
            for s_ in range(NS):
                slots.append(dict(gu=ar.get([128, KC, 512], BF16), d=ar.get([128, 2, D], BF16),
                                  tg=Trk(), tu=Trk(), td=Trk()))
            sl_b = [(ar.get([128, 256], F32), Trk()) for _ in range(3)]
            av_b = [(ar.get([128, 256], BF16), Trk()) for _ in range(3)]
            avT_b = [(ar.get([128, 256], BF16), Trk()) for _ in range(3)]

            def load_expert(e_):
                sl = slots[e_ % NS]
                k.dma("pool", sl["gu"][:, :, 0:256], expert_w_gate[e_].rearrange("(c p) f -> p c f", p=128), "eg%d" % (e_ % NS),
                      writes=[sl["tg"]])
                k.dma("pool", sl["gu"][:, :, 256:512], expert_w_up[e_].rearrange("(c p) f -> p c f", p=128), "eu%d" % (e_ % NS),
                      writes=[sl["tu"]])
                k.dma("pool", sl["d"], expert_w_down[e_].rearrange("(c p) n -> p c n", p=128), "ed%d" % (e_ % NS),
                      writes=[sl["td"]])

            for e_ in range(NS):
                load_expert(e_)
            g2bc = ar.get([128, D], F32)
            k.dma("sp", g2bc, ln_ffn_g.partition_broadcast(128), "gbc", writes=[t_g])
            hb = [ar.get([128, D], BF16) for _ in range(2)]
            junk = ar.get([128, D], BF16)
            ost = [(ar.get([128, D], F32), Trk()) for _ in range(2)]
            ssF = ar.get([128, NT], F32)
            t_ssFl = [Trk() for _ in range(NT)]
            k.op("dve", lambda e: e.memset(ssF, 0.0), writes=t_ssFl)
            rot["n"] = 8
            for j in range(NT):
                s = j % 2
                rms_tile(x2[:, j, :], t_x2[j], g2bc, t_g, ssE, t_ssE[j], hb[s], t_hb[s], junk, t_junk, j, h2T, t_h2[j], j * 128)
            t_Wr = Trk()
            with nc.allow_non_contiguous_dma(reason="tiny router weights"):
                k.dma("pool", Wr[:, :, 0:4], router_group_w.rearrange("(c p) n -> p c n", p=128), "wr0", writes=[t_Wr])
                k.dma("pool", Wr[:, :, 4:36], router_expert_w.rearrange("(c p) n -> p c n", p=128), "wr1", writes=[t_Wr])
            t_rb = Trk()
            k.dma("sp", rbc[:, 0:4], router_group_b.partition_broadcast(128), "rb0", writes=[t_rb])
            k.dma("sp", rbc[:, 4:36], router_expert_b.partition_broadcast(128), "rb1", writes=[t_rb])
            t_lg = Trk()
            for j in range(NT):
                b, tbk = bank()
                for c in range(KC):
                    k.op("pe", lambda e, c=c: e.matmul(psf(b)[:, 0:36], h2T[:, c, j * 128:(j + 1) * 128], Wr[:, c, :],
                                                       start=(c == 0), stop=(c == KC - 1)), reads=[t_h2[j], t_Wr], writes=[tbk], inc=(c == KC - 1))
                TT("dve", lg[:, j, :], psf(b)[:, 0:36], rbc, ALU.add, [tbk, t_rb], [t_lg])
            gl = lg[:, :, 0:4]
            t_r = Trk()
            R = [t_lg, t_r]

            def V(fn):
                k.op("dve", fn, reads=R, writes=[t_r])

            def bcg(ap):
                return ap.unsqueeze(2).to_broadcast([128, NT, 4])

            def bce(ap):
                return ap.unsqueeze(2).to_broadcast([128, NG, 8])

            V(lambda e: e.tensor_copy(el.rearrange("p (a g) x -> p a (g x)", a=NT), lg[:, :, 4:36]))
            V(lambda e: e.tensor_reduce(out=gmax, in_=gl, axis=AX.X, op=ALU.max))
            V(lambda e: e.tensor_tensor(out=gsh, in0=gl, in1=bcg(gmax), op=ALU.subtract))
            V(lambda e: e.tensor_tensor(out=gm, in0=gl, in1=bcg(gmax), op=ALU.is_equal))
            k.op("act", lambda e: e.activation(out=gsh, in_=gsh, func=AF.Exp), reads=R, writes=[t_r])
            V(lambda e: e.tensor_reduce(out=gsum, in_=gsh, axis=AX.X, op=ALU.add))
            V(lambda e: e.reciprocal(gsum, gsum))
            V(lambda e: e.tensor_tensor(out=gm, in0=gm, in1=bcg(gsum), op=ALU.mult))
            V(lambda e: e.tensor_reduce(out=m1, in_=el, axis=AX.X, op=ALU.max))
            V(lambda e: e.tensor_tensor(out=e1, in0=el, in1=bce(m1), op=ALU.is_equal))
            V(lambda e: e.scalar_tensor_tensor(out=e2, in0=e1, scalar=NEG, in1=el, op0=ALU.mult, op1=ALU.add))
            V(lambda e: e.tensor_reduce(out=m2, in_=e2, axis=AX.X, op=ALU.max))
            V(lambda e: e.tensor_tensor(out=e1, in0=el, in1=bce(m2), op=ALU.is_ge))
            V(lambda e: e.tensor_tensor(out=e2, in0=el, in1=bce(m1), op=ALU.subtract))
            k.op("act", lambda e: e.activation(out=e2, in_=e2, func=AF.Exp), reads=R, writes=[t_r])
            V(lambda e: e.tensor_tensor(out=e2, in0=e2, in1=e1, op=ALU.mult))
            V(lambda e: e.tensor_tensor(out=m2, in0=m2, in1=m1, op=ALU.subtract))
            k.op("act", lambda e: e.activation(out=m2, in_=m2, func=AF.Exp), reads=R, writes=[t_r])
            V(lambda e: e.tensor_scalar(out=m2, in0=m2, scalar1=1.0, scalar2=None, op0=ALU.add))
            V(lambda e: e.reciprocal(m2, m2))
            V(lambda e: e.tensor_tensor(out=w2_, in0=m2, in1=gm.rearrange("p a g -> p (a g)"), op=ALU.mult))
            V(lambda e: e.tensor_tensor(out=comb, in0=e2, in1=bce(w2_), op=ALU.mult))
            combv = comb.rearrange("p (a g) x -> p a (g x)", a=NT)
            dump("comb", combv[:, 0, :], t_r)
            k.barrier(skip=("eg", "eu", "ed"))
            t_h2a = Trk()
            gfbc = g2bc
            t_gf = Trk()
            k.dma("sp", gfbc, ln_final_g.partition_broadcast(128), "gbc", writes=[t_gf])

            def final_tile(j):
                o_, t_o = ost[j % 2]
                t_s = t_ssFl[j]
                k.op("act", lambda e: e.activation(out=junk, in_=x2[:, j, :], func=AF.Square, accum_out=ssF[:, j:j + 1]),
                     reads=[t_x2[j]], writes=[t_junk, t_s])
                k.op("dve", lambda e: e.tensor_scalar(out=ssF[:, j:j + 1], in0=ssF[:, j:j + 1], scalar1=1.0 / D, scalar2=1e-6,
                                                      op0=ALU.mult, op1=ALU.add), reads=[t_s], writes=[t_s])
                k.op("act", lambda e: e.sqrt(ssF[:, j:j + 1], ssF[:, j:j + 1]), reads=[t_s], writes=[t_s])
                k.op("dve", lambda e: e.reciprocal(ssF[:, j:j + 1], ssF[:, j:j + 1]), reads=[t_s], writes=[t_s])
                k.op("dve", lambda e: e.scalar_tensor_tensor(out=o_, in0=x2[:, j, :], scalar=ssF[:, j:j + 1], in1=gfbc,
                                                             op0=ALU.mult, op1=ALU.mult), reads=[t_x2[j], t_s, t_gf], writes=[t_o])
                k.dma("sp", out[j * 128:(j + 1) * 128, :], o_, "ost%d" % (j % 2), reads=[t_o])
            groups = []
            e0 = 0
            while e0 < 32:
                groups.append(list(range(e0, min(32, e0 + GS))))
                e0 += GS
            items = []
            for g in groups:
                for j in range(NT):
                    for ei, e_ in enumerate(g):
                        items.append((g, j, ei, e_))
            loaded = NS
            UB = [0, 1, 2]
            TBK = 3
            yb_of = lambda j: (4, 5) if j % 2 == 0 else (6, 7)
            st_ = {}

            def stage_U(idx):
                g, j, ei, e_ = items[idx]
                sl = slots[e_ % NS]
                b = UB[idx % 3]
                for c in range(KC):
                    k.op("pe", lambda e, c=c: e.matmul(psf(b), h2T[:, c, j * 128:(j + 1) * 128], sl["gu"][:, c, :],
                                                       start=(c == 0), stop=(c == KC - 1)),
                         reads=[t_h2a, sl["tg"], sl["tu"]], writes=[pst[b]], inc=(c == KC - 1))
                s_, t_s = sl_b[idx % 3]
                av, t_av = av_b[idx % 3]
                ACT(s_, psf(b)[:, 0:256], AF.Silu, [pst[b]], [t_s])
                k.op("dve", lambda e: e.scalar_tensor_tensor(out=av, in0=psf(b)[:, 256:512], scalar=combv[:, j, e_:e_ + 1], in1=s_,
                                                             op0=ALU.mult, op1=ALU.mult), reads=[pst[b], t_s, t_r], writes=[t_av])

            def stage_T(idx):
                av, t_av = av_b[idx % 3]
                avT, t_avT = avT_b[idx % 3]
                off = (idx % 2) * 256
                tt = st_.setdefault(("tt", idx % 2), Trk())
                for f in range(2):
                    k.op("pe", lambda e, f=f: e.transpose(psb(TBK)[:, off + f * 128:off + (f + 1) * 128], av[:, f * 128:(f + 1) * 128], ident_b),
                         reads=[t_av, t_const2], writes=[tt], inc=(f == 1))
                ACT(avT, psb(TBK)[:, off:off + 256], AF.Copy, [tt], [t_avT])

            def stage_D(idx):
                g, j, ei, e_ = items[idx]
                sl = slots[e_ % NS]
                avT, t_avT = avT_b[idx % 3]
                yb2 = yb_of(j)
                last = (ei == len(g) - 1)
                for f in range(2):
                    for half in range(2):
                        k.op("pe", lambda e, f=f, half=half: e.matmul(psf(yb2[half]), avT[:, f * 128:(f + 1) * 128],
                                                                        sl["d"][:, f, half * 512:(half + 1) * 512],
                                                                        start=(ei == 0 and f == 0), stop=(last and f == 1)),
                             reads=[t_avT, sl["td"]], writes=[pst[yb2[half]]], inc=(last and f == 1))
                if last:
                    for half in range(2):
                        TT("dve", x2[:, j, half * 512:(half + 1) * 512], x2[:, j, half * 512:(half + 1) * 512], psf(yb2[half]),
                           ALU.add, [pst[yb2[half]], t_x2[j]], [t_x2[j]])
                    if g is groups[-1]:
                        final_tile(j)

            nonlocal_loaded = [loaded]
            n_items = len(items)
            for step in range(n_items + 2):
                if step < n_items:
                    stage_U(step)
                if 1 <= step <= n_items:
                    stage_T(step - 1)
                if 2 <= step <= n_items + 1:
                    idx = step - 2
                    stage_D(idx)
                    g, j, ei, e_ = items[idx]
                    if j == NT - 1 and ei == len(g) - 1:
                        for _ in g:
                            if nonlocal_loaded[0] < 32:
                                load_expert(nonlocal_loaded[0])
                                nonlocal_loaded[0] += 1

            if STOP == 'E':
                raise _Stop()
            k.barrier()
        except _Stop:
            k.barrier()
    return nc


def _consts(att_rel_bias):
    ident = np.eye(128, dtype=np.float32)
    s = np.arange(128)[:, None]
    t = np.arange(128)[None, :]
    same = (s // 64) == (t // 64)
    Ms = (same & (s < t)).astype(np.float32)
    Mi = (same & (s <= t)).astype(np.float32)
    MsT = (same & (s > t)).astype(np.float32)
    Blk = same.astype(np.float32)
    masks = np.concatenate([Ms, Mi, MsT, Blk], axis=1)
    i2 = (np.arange(128)[:, None] % 64 == np.arange(64)[None, :]).astype(np.float32)
    i2rep = np.tile(i2, (1, 8))
    kl = np.arange(128)[:, None]
    ql = np.arange(128)[None, :]
    tab = np.asarray(att_rel_bias, dtype=np.float32)[0]
    biasg = np.zeros((128, 5, 8, 128), np.float32)
    am = np.zeros((128, 5, 128), np.float32)
    for o in range(5):
        bpos = 2 * o + kl // 64 - ql // 64
        kpos = bpos * 64 + kl % 64
        rel = 512 + (ql % 64) - kpos
        idx = np.clip(rel, -64, 64) + 64
        valid = (bpos >= 0) & (bpos <= 8)
        biasg[:, o, :, :] = np.transpose(tab[:, idx], (1, 0, 2))
        am[:, o, :] = np.where(valid, 0.0, NEG)
    return ident, masks, i2rep, biasg.reshape(128, -1), am.reshape(128, -1)


_DEBUG_SPEC = []


def kernel(**inputs):
    inp = {k_: np.asarray(v) for k_, v in inputs.items()}
    n = 8
    ident, masks, i2rep, biasg, am = _consts(inp["att_rel_bias"])
    nc = build_program(_DEBUG_SPEC)
    shared = {k_: np.ascontiguousarray(v, dtype=np.float32) for k_, v in inp.items() if k_ not in ("x", "att_rel_bias")}
    shared["rwkv_r_k"] = shared["rwkv_r_k"].reshape(1, 512)
    shared.update(c_ident=ident, c_masks=masks, c_i2=i2rep, biasg=biasg, amask=am)
    ncores = int(os.environ.get("MK_CORES", n))
    in_maps = []
    for b in range(ncores):
        m = dict(shared)
        m["x"] = np.ascontiguousarray(inp["x"][b], dtype=np.float32)
        in_maps.append(m)
    res = run_bass_kernel_spmd(nc, in_maps, core_ids=list(range(ncores)))
    if _DEBUG_SPEC:
        kernel.last = res.results
    outs = [r["out"] for r in res.results]
    while len(outs) < n:
        outs.append(np.zeros_like(outs[0]))
    return np.stack(outs, axis=0).astype(np.float32)
```

```python
import math
import os
from contextlib import ExitStack

import numpy as np
import concourse.bass as bass
import concourse.mybir as mybir
from concourse.bass_utils import run_bass_kernel_spmd

F32 = mybir.dt.float32
BF16 = mybir.dt.bfloat16
AF = mybir.ActivationFunctionType
ALU = mybir.AluOpType
AX = mybir.AxisListType

T = 2048
NT = 16
D = 1024
KC = 8
DIN = 5376
CDEC = math.exp(-0.5)
NEG = -1.0e30
DEBUG = bool(os.environ.get("MK_DEBUG"))
STOP = os.environ.get("MK_STOP", "")
PUMP = int(os.environ.get("MK_PUMP", "4"))
NGRP = int(os.environ.get("MK_GRP", "2"))
CPACT = int(os.environ.get("MK_CPACT", "4"))


class _Stop(Exception):
    pass


class Trk:
    __slots__ = ("w", "r")

    def __init__(self):
        self.w = {}
        self.r = {}


class K:
    def __init__(self, nc, es):
        self.nc = nc
        self.es = es
        self.E = dict(pe=nc.tensor, act=nc.scalar, dve=nc.vector, pool=nc.gpsimd, sp=nc.sync)
        self.sems = {}
        self.cnt = {}
        self.seen = {e: {} for e in self.E}
        for e in ("pe", "act", "dve", "pool"):
            self.newsem(e)

    def newsem(self, key):
        if key not in self.sems:
            self.sems[key] = self.es.enter_context(self.nc.semaphore("s_" + key))
            self.cnt[key] = 0
        return key

    def _waits(self, eng, reads, writes):
        need = {}
        for t in reads:
            for k, v in t.w.items():
                if need.get(k, 0) < v:
                    need[k] = v
        for t in writes:
            for d in (t.w, t.r):
                for k, v in d.items():
                    if need.get(k, 0) < v:
                        need[k] = v
        e = self.E[eng]
        seen = self.seen[eng]
        if eng == "pe":
            need.pop("pe", None)
        for k, v in need.items():
            if seen.get(k, 0) < v:
                e.wait_ge(self.sems[k], v)
                seen[k] = v
        return e

    def op(self, eng, fn, reads=(), writes=(), inc=True):
        e = self._waits(eng, reads, writes)
        inst = fn(e)
        if inc:
            self.cnt[eng] += 1
            inst.then_inc(self.sems[eng], 1)
            tk = self.cnt[eng]
        else:
            tk = self.cnt[eng] + 1
        for t in reads:
            if t.r.get(eng, 0) < tk:
                t.r[eng] = tk
        for t in writes:
            t.w = {eng: tk}
            t.r = {}
        return inst

    def dma(self, q, out, in_, key, reads=(), writes=()):
        self.newsem(key)
        e = self._waits(q, reads, writes)
        self.cnt[key] += 16
        e.dma_start(out=out, in_=in_).then_inc(self.sems[key], 16)
        tk = self.cnt[key]
        for t in reads:
            t.r[key] = tk
        for t in writes:
            t.w = {key: tk}
            t.r = {}

    def barrier(self, skip=()):
        for eng, e in self.E.items():
            seen = self.seen[eng]
            for k, v in self.cnt.items():
                if k[:2] in skip:
                    continue
                if v > 0 and seen.get(k, 0) < v:
                    e.wait_ge(self.sems[k], v)
                    seen[k] = v


class Arena:
    def __init__(self, ap_bf16, nbytes):
        self.ap = ap_bf16
        self.n = nbytes
        self.off = 0

    def mark(self):
        return self.off

    def reset(self, m):
        self.off = m

    def get(self, shape, dt):
        esz = 4 if dt == F32 else 2
        nel = 1
        for s in shape[1:]:
            nel *= s
        nb = (nel * esz + 63) // 64 * 64
        assert self.off + nb <= self.n, ("arena overflow", self.off, nb, self.n)
        v = self.ap[0:shape[0], self.off // 2:(self.off + nel * esz) // 2]
        self.off += nb
        if dt == F32:
            v = v.bitcast(F32)
        if len(shape) == 3:
            v = v.rearrange("p (a b) -> p a b", a=shape[1])
        elif len(shape) == 4:
            v = v.rearrange("p (a b c) -> p a b c", a=shape[1], b=shape[2])
        return v


def build_program(debug_names=()):
    nc = bass.Bass("TRN2", target_bir_lowering=False)

    def din(name, shape):
        return nc.dram_tensor(name, list(shape), F32, kind="ExternalInput").ap()

    x = din("x", [T, D])
    ln_mix_g = din("ln_mix_g", [1, D])[0]
    w_in = din("w_in", [1, D, DIN])[0]
    biasg = din("biasg", [128, 5 * 8 * 128])
    amask = din("amask", [128, 5 * 128])
    rwkv_mu = din("rwkv_mu", [1, 1792])[0]
    rwkv_w0 = din("rwkv_w0", [1, 512])[0]
    rwkv_w2 = din("rwkv_w2", [1, 64, 512])[0]
    rwkv_a0 = din("rwkv_a0", [1, 512])[0]
    rwkv_a2 = din("rwkv_a2", [1, 64, 512])[0]
    rwkv_g2 = din("rwkv_g2", [1, 128, 512])[0]
    rwkv_k_k = din("rwkv_k_k", [1, 512])[0]
    rwkv_k_a = din("rwkv_k_a", [1, 512])[0]
    rwkv_r_k = din("rwkv_r_k", [1, 512])[0]
    rwkv_gn_g = din("rwkv_gn_g", [1, 512])[0]
    rwkv_gn_b = din("rwkv_gn_b", [1, 512])[0]
    w_branch_att = din("w_branch_att", [1, 512, D])[0]
    w_branch_rwkv = din("w_branch_rwkv", [1, 512, D])[0]
    w_out = din("w_out", [1, D, D])[0]
    ln_ffn_g = din("ln_ffn_g", [1, D])[0]
    router_group_w = din("router_group_w", [1, D, 4])[0]
    router_group_b = din("router_group_b", [1, 4])[0]
    router_expert_w = din("router_expert_w", [1, D, 32])[0]
    router_expert_b = din("router_expert_b", [1, 32])[0]
    expert_w_gate = din("expert_w_gate", [1, 32, D, 256])[0]
    expert_w_up = din("expert_w_up", [1, 32, D, 256])[0]
    expert_w_down = din("expert_w_down", [1, 32, 256, D])[0]
    ln_final_g = din("ln_final_g", [D])
    c_ident = din("c_ident", [128, 128])
    c_masks = din("c_masks", [128, 4 * 128])
    c_i2 = din("c_i2", [128, 512])
    out = nc.dram_tensor("out", [T, D], F32, kind="ExternalOutput").ap()
    dbg = {}
    for nm, shp in debug_names:
        dbg[nm] = nc.dram_tensor("dbg_" + nm, list(shp), F32, kind="ExternalOutput").ap()

    with ExitStack() as es:
        k = K(nc, es)
        NB = 207 * 1024
        arena_t = es.enter_context(nc.sbuf_tensor("arena", [128, NB // 2], BF16))
        ar = Arena(arena_t[:, :], NB)
        ps = es.enter_context(nc.psum_tensor("ps", [128, 8, 512], F32))
        pst = [Trk() for _ in range(8)]
        rot = {"i": 0, "n": 6}

        held = []

        def bank():
            while True:
                b = rot["i"] % rot["n"]
                rot["i"] += 1
                if b not in held:
                    return b, pst[b]

        rotB = {"i": 0}

        def bankB():
            b, t = bank()
            held.append(b)
            if len(held) > 3:
                held.pop(0)
            return b, t

        def psf(b):
            return ps[:, b, :]

        def psb(b):
            return ps[:, b, :].bitcast(BF16)

        dbg_stage = {}

        def dump(name, ap_f32, trk):
            if name in dbg:
                k.dma("sp", dbg[name], ap_f32, "dbg_" + name, reads=[trk])

        try:
            ident_b = ar.get([128, 128], BF16)
            masks_f = ar.get([128, 4, 128], F32)
            t_const = Trk()
            t_const2 = Trk()
            k.dma("pool", ident_b, c_ident, "c1", writes=[t_const2])
            t_masks = Trk()
            k.dma("sp", masks_f.rearrange("p a b -> p (a b)"), c_masks, "c2", writes=[t_masks])
            h_mark = ar.mark()
            hT = ar.get([128, KC, T + 1], BF16)
            t_hT = [Trk() for _ in range(NT)]
            t_hz = Trk()
            k.op("dve", lambda e: e.memset(hT[:, :, 0:1], 0.0), writes=[t_hz])
            base_mark = ar.mark()

            def rms_tile(src_ap, t_src, gbc, t_g, ss, t_ss, hb, t_hb, sq_junk, t_junk, j, dstT, t_dst, col0, eps=1e-6):
                k.op("act", lambda e: e.activation(out=sq_junk, in_=src_ap, func=AF.Square, accum_out=ss[:, j:j + 1]),
                     reads=[t_src], writes=[t_junk, t_ss])
                k.op("dve", lambda e: e.tensor_scalar(out=ss[:, j:j + 1], in0=ss[:, j:j + 1], scalar1=1.0 / D, scalar2=eps,
                                                      op0=ALU.mult, op1=ALU.add), reads=[t_ss], writes=[t_ss])
                k.op("act", lambda e: e.sqrt(ss[:, j:j + 1], ss[:, j:j + 1]), reads=[t_ss], writes=[t_ss])
                k.op("dve", lambda e: e.reciprocal(ss[:, j:j + 1], ss[:, j:j + 1]), reads=[t_ss], writes=[t_ss])
                k.op("dve", lambda e: e.scalar_tensor_tensor(out=hb, in0=src_ap, scalar=ss[:, j:j + 1], in1=gbc,
                                                             op0=ALU.mult, op1=ALU.mult),
                     reads=[t_src, t_ss, t_g], writes=[t_hb])
                b, tb = bank()
                for c in range(KC):
                    k.op("pe", lambda e, c=c: e.transpose(psb(b)[:, c * 128:(c + 1) * 128], hb[:, c * 128:(c + 1) * 128], ident_b),
                         reads=[t_hb, t_const2], writes=[tb], inc=(c == KC - 1))
                k.op("act", lambda e: e.activation(out=dstT[:, :, col0:col0 + 128],
                                                   in_=psb(b).rearrange("p (a b) -> p a b", a=KC), func=AF.Copy),
                     reads=[tb], writes=[t_dst])

            def TT(eng, out, in0, in1, op, reads, writes):
                k.op(eng, lambda e: e.tensor_tensor(out=out, in0=in0, in1=in1, op=op), reads=reads, writes=writes)

            def ACT(out, in_, func, reads, writes, scale=1.0):
                k.op("act", lambda e: e.activation(out=out, in_=in_, func=func, scale=scale), reads=reads, writes=writes)

            def CP(on_act, out, in_, reads, writes):
                if on_act:
                    k.op("act", lambda e: e.activation(out=out, in_=in_, func=AF.Copy), reads=reads, writes=writes)
                else:
                    k.op("dve", lambda e: e.tensor_copy(out, in_), reads=reads, writes=writes)

            def h8(ap):
                return ap.rearrange("p (h d) -> p h d", h=8)

            def bc8(ap8):
                return ap8.unsqueeze(2).to_broadcast([128, 8, 64])

            a_save = ar.mark()
            ar.reset(NB - 20 * 1024)
            gbc = ar.get([128, D], F32)
            t_g = Trk()
            k.dma("sp", gbc, ln_mix_g.partition_broadcast(128), "gbc", writes=[t_g])
            ssA = ar.get([128, NT], F32)
            t_ssl = [Trk() for _ in range(NT)]
            k.op("dve", lambda e: e.memset(ssA, 0.0), writes=t_ssl)
            xs = [ar.get([128, D], F32) for _ in range(2)]
            t_xs = [Trk() for _ in range(2)]
            hb = [ar.get([128, D], BF16) for _ in range(2)]
            t_hb = [Trk() for _ in range(2)]
            junk = ar.get([128, D], BF16)
            t_junk = Trk()
            ar.reset(a_save)
            for j in range(NT):
                s = j % 2
                k.dma("sp", xs[s], x[j * 128:(j + 1) * 128, :], "xs%d" % s, writes=[t_xs[s]])
                rms_tile(xs[s], t_xs[s], gbc, t_g, ssA, t_ssl[j], hb[s], t_hb[s], junk, t_junk, j, hT, t_hT[j], 1 + j * 128)
            if "hT" in dbg:
                st = ar.get([128, T], F32)
                tt = Trk()
                k.op("dve", lambda e: e.tensor_copy(st, hT[:, 0, 1:T + 1]), reads=t_hT + [t_hz], writes=[tt])
                dump("hT", st, tt)
            ar.reset(base_mark)
            t_hall = Trk()
            top_limit = NB - 20 * 1024

            def hT_deps(t_lo, t_hi):
                j0 = max(0, (t_lo - 1) // 128)
                j1 = (t_hi - 1) // 128
                return [t_hz] + [t_hT[jj] for jj in range(j0, j1 + 1)]

            if STOP == 'A':
                raise _Stop()
            rwkvT = ar.get([128, 4, T], BF16)
            t_rwT = [Trk() for _ in range(NT)]
            c_mark = ar.mark()
            rkv = ar.get([128, NT, 1536], BF16)
            t_rkv = Trk()
            w2a2 = ar.get([128, 512], BF16)
            g2w = ar.get([128, 512], BF16)
            t_sw = Trk()
            k.dma("pool", w2a2[0:64, :], rwkv_w2, "sw0", writes=[t_sw])
            k.dma("pool", w2a2[64:128, :], rwkv_a2, "sw1", writes=[t_sw])
            k.dma("pool", g2w, rwkv_g2, "sw2", writes=[t_sw])
            bcs = {}
            t_bc = Trk()
            for i, (nm, src) in enumerate([("w0", rwkv_w0), ("a0", rwkv_a0), ("kk", rwkv_k_k), ("ka", rwkv_k_a),
                                           ("rk", rwkv_r_k), ("gng", rwkv_gn_g), ("gnb", rwkv_gn_b)]):
                bcs[nm] = ar.get([128, 512], F32)
                k.dma("sp", bcs[nm], src.partition_broadcast(128), "bc%d" % i, writes=[t_bc])
            i2rep = ar.get([128, 512], F32)
            k.dma("sp", i2rep, c_i2, "bci2", writes=[t_bc])
            loraT = ar.get([128, 2, T], BF16)
            t_lora = Trk()
            m0 = ar.mark()
            W1 = ar.get([128, KC, 1536], BF16)
            t_W = Trk()
            k.dma("pool", W1, w_in[:, 1536:3072].rearrange("(c p) n -> p c n", p=128), "wrkv", writes=[t_W])
            mu_bc = ar.get([128, 1792], F32)
            t_mu = Trk()
            k.dma("sp", mu_bc, rwkv_mu.partition_broadcast(128), "mu", writes=[t_mu])
            WL1 = ar.get([128, KC, 256], BF16)
            WL2 = ar.get([128, KC, 256], BF16)
            t_WL = Trk()
            omu_l = ar.get([128, 256], F32)
            stg = [ar.get([128, 256], F32) for _ in range(2)]
            t_stg = [Trk() for _ in range(2)]
            t_omu = Trk()
            k.op("dve", lambda e: e.tensor_scalar(out=omu_l, in0=mu_bc[:, 1536:1792], scalar1=-1.0, scalar2=1.0, op0=ALU.mult, op1=ALU.add),
                 reads=[t_mu], writes=[t_omu])
            for c in range(KC):
                s = c % 2
                k.dma("sp", stg[s], w_in[c * 128:(c + 1) * 128, 3072:3328], "stg%d" % s, writes=[t_stg[s]])
                k.op("dve", lambda e, c=c, s=s: e.tensor_tensor(out=WL1[:, c, :], in0=stg[s], in1=omu_l, op=ALU.mult),
                     reads=[t_stg[s], t_omu], writes=[t_WL])
                k.op("pool", lambda e, c=c, s=s: e.tensor_tensor(out=WL2[:, c, :], in0=stg[s], in1=mu_bc[:, 1536:1792], op=ALU.mult),
                     reads=[t_stg[s], t_mu], writes=[t_WL])
            for tb in range(4):
                for lc in range(2):
                    b, tbk = bank()
                    c0 = lc * 128
                    n = 0
                    for w, sh in ((WL1, 1), (WL2, 0)):
                        for c in range(KC):
                            k.op("pe", lambda e, w=w, sh=sh, c=c, n=n: e.matmul(
                                psf(b), w[:, c, c0:c0 + 128], hT[:, c, sh + tb * 512: sh + tb * 512 + 512],
                                start=(n == 0), stop=(n == 15)), reads=[t_WL] + hT_deps(tb * 512, (tb + 1) * 512), writes=[tbk], inc=(n == 15))
                            n += 1
                    if lc == 0:
                        k.op("act", lambda e: e.activation(out=loraT[0:64, 0, tb * 512:(tb + 1) * 512], in_=psf(b)[0:64, :],
                                                           func=AF.Tanh), reads=[tbk], writes=[t_lora])
                        k.op("act", lambda e: e.activation(out=loraT[64:128, 0, tb * 512:(tb + 1) * 512], in_=psf(b)[64:128, :],
                                                           func=AF.Copy), reads=[tbk], writes=[t_lora])
                    else:
                        k.op("act", lambda e: e.activation(out=loraT[:, 1, tb * 512:(tb + 1) * 512], in_=psf(b),
                                                           func=AF.Sigmoid), reads=[tbk], writes=[t_lora])
            xb_ = [(ar.get([128, 512], F32), Trk()) for _ in range(2)]
            db_ = [(ar.get([128, 512], F32), Trk()) for _ in range(2)]
            it = 0
            for j in range(NT):
                cur = hT[:, :, 1 + j * 128: 1 + (j + 1) * 128]
                prv = hT[:, :, j * 128:(j + 1) * 128]
                for blk in range(3):
                    bP, tP = bank()
                    for c in range(KC):
                        k.op("pe", lambda e, c=c: e.matmul(
                            psf(bP), cur[:, c, :], W1[:, c, blk * 512:(blk + 1) * 512], start=(c == 0), stop=(c == KC - 1)),
                            reads=[t_W] + hT_deps(j * 128, (j + 1) * 128), writes=[tP], inc=(c == KC - 1))
                    bQ, tQ = bank()
                    for c in range(KC):
                        k.op("pe", lambda e, c=c: e.matmul(
                            psf(bQ), prv[:, c, :], W1[:, c, blk * 512:(blk + 1) * 512], start=(c == 0), stop=(c == KC - 1)),
                            reads=[t_W] + hT_deps(j * 128, (j + 1) * 128), writes=[tQ], inc=(c == KC - 1))
                    (x_, t_x), (dq, t_dq) = xb_[it % 2], db_[it % 2]
                    it += 1
                    ACT(x_, psf(bP), AF.Copy, [tP], [t_x])
                    TT("dve", dq, psf(bQ), x_, ALU.subtract, [tQ, t_x], [t_dq])
                    TT("pool", dq, dq, mu_bc[:, blk * 512:(blk + 1) * 512], ALU.mult, [t_dq, t_mu], [t_dq])
                    TT("pool", rkv[:, j, blk * 512:(blk + 1) * 512], x_, dq, ALU.add, [t_x, t_dq], [t_rkv])
            assert ar.mark() <= top_limit, (ar.mark(), top_limit)
            k.barrier()
            ar.reset(m0)
            t_rkv = Trk()

            if STOP == 'C0':
                raise _Stop()

            def f32t():
                return ar.get([128, 512], F32), Trk()

            def bf16t():
                return ar.get([128, 512], BF16), Trk()

            NBUF = 2
            tm = {}
            for nm in ("kmod", "tz", "sg", "Lp", "eLm", "eLC", "kk", "tmp", "kkn"):
                tm[nm] = [f32t()]
            tm["g"] = [f32t(), f32t()]
            tm["eL"] = tm["kkn"]
            tm["GD"] = tm["tz"]
            tm["a"] = tm["tz"]
            tm["eLm1"] = tm["sg"]
            tm["Ysb"] = tm["Lp"]
            tm["ka"] = tm["kk"]
            tb_ = {}
            for nm in ("aT", "bT", "kT", "rT", "bh", "kh", "GDb", "GDl"):
                tb_[nm] = [bf16t() for _ in range(NBUF)]
            tb_["y4"] = [bf16t()] * 2
            sm = {}
            for nm in ("ss8", "bs8", "ysum", "yss", "mean", "rstd"):
                sm[nm] = [(ar.get([128, 8], F32), Trk()) for _ in range(NBUF)]
            NU = 8 // NGRP
            ub = []
            for u in range(NU):
                d_ = {}
                d_["FM"] = (ar.get([64, 512], BF16), Trk())
                d_["NAKA"] = (ar.get([128, 512], BF16), Trk())
                d_["NN"] = [(ar.get([128, 256], BF16), Trk()) for _ in range(2)]
                d_["T"] = [(ar.get([128, 128], BF16), Trk()) for _ in range(2)]
                d_["X"] = (ar.get([128, 64], BF16), Trk())
                d_["DU"] = (ar.get([128, 128], BF16), Trk())
                d_["MB"] = (ar.get([64, 256], F32), Trk())
                d_["RQ"] = (ar.get([64, 2, 128], BF16), Trk())
                k.op("dve", lambda e, d_=d_: e.memset(d_["RQ"][0], 0.0), writes=[d_["RQ"][1]])
                ub.append(d_)
            Sst = [[(ar.get([64, 64], F32), Trk()) for _ in range(2)] for _ in range(8)]
            Sb = [[(ar.get([64, 64], BF16), Trk()) for _ in range(2)] for _ in range(8)]
            for h in range(8):
                k.op("dve", lambda e, h=h: e.memset(Sst[h][0][0], 0.0), writes=[Sst[h][0][1]])
            s_cur = [0] * 8
            ucount = 0
            YB = [6, 7]
            rot["n"] = 7


            def prep_gen(j):
                s = j % NBUF
                cur = hT[:, :, 1 + j * 128: 1 + (j + 1) * 128]
                prv = hT[:, :, j * 128:(j + 1) * 128]
                r_, k_, v_ = rkv[:, j, 0:512], rkv[:, j, 512:1024], rkv[:, j, 1024:1536]
                t_r = t_k = t_v = t_rkv
                tok = slice(j * 128, (j + 1) * 128)
                b, tbk = bankB()
                k.op("pe", lambda e: e.matmul(psf(b), loraT[0:64, 0, tok], w2a2[0:64, :], start=True, stop=True),
                     reads=[t_lora, t_sw], writes=[tbk])
                yield
                tz, t_tz = tm["tz"][0]
                TT("dve", tz, psf(b), bcs["w0"], ALU.add, [tbk, t_bc], [t_tz])
                yield
                sg, t_sg = tm["sg"][0]
                ACT(sg, tz, AF.Sigmoid, [t_tz], [t_sg])
                yield
                b, tbk = bankB()
                k.op("pe", lambda e: e.matmul(psf(b), loraT[64:128, 0, tok], w2a2[64:128, :], start=True, stop=True),
                     reads=[t_lora, t_sw], writes=[tbk])
                yield
                TT("dve", tz, psf(b), bcs["a0"], ALU.add, [tbk, t_bc], [t_tz])
                yield
                a_, t_a = tm["a"][0]
                ACT(a_, tz, AF.Sigmoid, [t_tz], [t_a])
                yield
                b, tbk = bankB()
                k.op("pe", lambda e: e.matmul(psf(b), loraT[:, 1, tok], g2w, start=True, stop=True),
                     reads=[t_lora, t_sw], writes=[tbk])
                yield
                g_, t_g_ = tm["g"][s]
                ACT(g_, psf(b), AF.Copy, [tbk], [t_g_])
                yield
                bL, tL = bankB()
                k.op("pe", lambda e: e.matmul(psf(bL), masks_f[:, 1, :], sg, start=True, stop=True),
                     reads=[t_masks, t_sg], writes=[tL])
                yield
                bC, tC = bankB()
                k.op("pe", lambda e: e.matmul(psf(bC), masks_f[:, 3, :], sg, start=True, stop=True),
                     reads=[t_masks, t_sg], writes=[tC])
                yield
                Lp, t_Lp = tm["Lp"][0]
                ACT(Lp, psf(bL), AF.Copy, [tL], [t_Lp])
                yield
                eL, t_eL = tm["eL"][0]
                ACT(eL, psf(bL), AF.Exp, [tL], [t_eL], scale=-CDEC)
                yield
                rT, t_rT = tb_["rT"][s]
                TT("dve", rT, r_, eL, ALU.mult, [t_r, t_eL], [t_rT])
                yield
                eLm, t_eLm = tm["eLm"][0]
                ACT(eLm, psf(bL), AF.Exp, [tL], [t_eLm], scale=CDEC)
                yield
                tmp, t_tmp = tm["tmp"][0]
                TT("dve", tmp, Lp, sg, ALU.subtract, [t_Lp, t_sg], [t_tmp])
                yield
                eLm1, t_eLm1 = tm["eLm1"][0]
                ACT(eLm1, tmp, AF.Exp, [t_tmp], [t_eLm1], scale=-CDEC)
                yield
                TT("dve", tmp, psf(bC), Lp, ALU.subtract, [tC, t_Lp], [t_tmp])
                yield
                eLC, t_eLC = tm["eLC"][0]
                ACT(eLC, tmp, AF.Exp, [t_tmp], [t_eLC], scale=-CDEC)
                yield
                kk, t_kk = tm["kk"][0]
                TT("dve", kk, k_, bcs["kk"], ALU.mult, [t_k, t_bc], [t_kk])
                yield
                TT("dve", tmp, kk, kk, ALU.mult, [t_kk], [t_tmp])
                yield
                ss8, t_ss8 = sm["ss8"][s]
                k.op("dve", lambda e: e.tensor_reduce(out=ss8, in_=h8(tmp), axis=AX.X, op=ALU.add), reads=[t_tmp], writes=[t_ss8])
                yield
                k.op("dve", lambda e: e.tensor_scalar(out=ss8, in0=ss8, scalar1=1e-24, scalar2=None, op0=ALU.max),
                     reads=[t_ss8], writes=[t_ss8])
                yield
                k.op("act", lambda e: e.sqrt(ss8, ss8), reads=[t_ss8], writes=[t_ss8])
                yield
                k.op("dve", lambda e: e.reciprocal(ss8, ss8), reads=[t_ss8], writes=[t_ss8])
                yield
                kkn, t_kkn = tm["kkn"][0]
                TT("dve", h8(kkn), h8(kk), bc8(ss8), ALU.mult, [t_kk, t_ss8], [t_kkn])
                yield
                kmod, t_kmod = tm["kmod"][0]
                k.op("dve", lambda e: e.scalar_tensor_tensor(out=tmp, in0=a_, scalar=-1.0, in1=bcs["ka"], op0=ALU.add, op1=ALU.mult),
                     reads=[t_a, t_bc], writes=[t_tmp])
                yield
                k.op("dve", lambda e: e.scalar_tensor_tensor(out=kmod, in0=tmp, scalar=1.0, in1=k_, op0=ALU.add, op1=ALU.mult),
                     reads=[t_tmp, t_k], writes=[t_kmod])
                yield
                ka, t_ka = tm["ka"][0]
                TT("dve", ka, kkn, a_, ALU.mult, [t_kkn, t_a], [t_ka])
                yield
                GD, t_GD = tm["GD"][0]
                ACT(GD, psf(bC), AF.Exp, [tC], [t_GD], scale=-CDEC)
                yield
                GDb, t_GDb = tb_["GDb"][s]
                TT("dve", GD, GD, i2rep, ALU.mult, [t_GD, t_bc], [t_GD])
                yield
                GDl, t_GDl = tb_["GDl"][s]
                k.op("dve", lambda e: e.tensor_copy(GDb, GD), reads=[t_GD], writes=[t_GDb])
                yield
                TT("dve", GD, GD, GDb, ALU.subtract, [t_GD, t_GDb], [t_GD])
                yield
                k.op("dve", lambda e: e.tensor_copy(GDl, GD), reads=[t_GD], writes=[t_GDl])
                yield
                aT, t_aT = tb_["aT"][s]
                k.op("dve", lambda e: e.scalar_tensor_tensor(out=aT, in0=kkn, scalar=-1.0, in1=eLm1, op0=ALU.mult, op1=ALU.mult),
                     reads=[t_kkn, t_eLm1], writes=[t_aT])
                yield
                bT, t_bT = tb_["bT"][s]
                TT("dve", bT, ka, eLm, ALU.mult, [t_ka, t_eLm], [t_bT])
                yield
                kT_, t_kT = tb_["kT"][s]
                TT("dve", kT_, kmod, eLm, ALU.mult, [t_kmod, t_eLm], [t_kT])
                yield
                bh, t_bh = tb_["bh"][s]
                TT("dve", bh, ka, eLC, ALU.mult, [t_ka, t_eLC], [t_bh])
                yield
                kh, t_kh = tb_["kh"][s]
                TT("dve", kh, kmod, eLC, ALU.mult, [t_kmod, t_eLC], [t_kh])
                yield
                TT("dve", tmp, r_, kmod, ALU.mult, [t_r, t_kmod], [t_tmp])
                yield
                TT("dve", tmp, tmp, bcs["rk"], ALU.mult, [t_tmp, t_bc], [t_tmp])
                yield
                bs8, t_bs8 = sm["bs8"][s]
                k.op("dve", lambda e: e.tensor_reduce(out=bs8, in_=h8(tmp), axis=AX.X, op=ALU.add), reads=[t_tmp], writes=[t_bs8])
                yield
                if j == 0:
                    dump("sg", sg, t_sg)
                    yield
                    dump("kkn", kkn, t_kkn)
                    yield
                    dump("kmod", kmod, t_kmod)
                    yield
                    dump("eLm1", eLm1, t_eLm1)
                    yield
                    dump("eLC", eLC, t_eLC)
                    yield

                if STOP == 'C1':
                    raise _Stop()
            def post_gen(j):
                s = j % NBUF
                yb = 7
                t_yb = pst[yb]
                v_, t_v = rkv[:, j, 1024:1536], t_rkv
                g_, t_g_ = tm['g'][s]
                bs8, t_bs8 = sm['bs8'][s]
                tmp, t_tmp = tm['tmp'][0]
                Ysb, t_Y = tm["Ysb"][0]
                ACT(Ysb, psf(yb), AF.Copy, [t_yb], [t_Y])
                yield
                if j == 0:
                    dump("Y0", Ysb, t_Y)
                    yield
                ysum, t_ysum = sm["ysum"][s]
                k.op("dve", lambda e: e.tensor_reduce(out=ysum, in_=h8(Ysb), axis=AX.X, op=ALU.add), reads=[t_Y], writes=[t_ysum])
                yield
                TT("dve", tmp, Ysb, Ysb, ALU.mult, [t_Y], [t_tmp])
                yield
                yss, t_yss = sm["yss"][s]
                k.op("dve", lambda e: e.tensor_reduce(out=yss, in_=h8(tmp), axis=AX.X, op=ALU.add), reads=[t_tmp], writes=[t_yss])
                yield
                mean, t_mean = sm["mean"][s]
                k.op("dve", lambda e: e.tensor_scalar(out=mean, in0=ysum, scalar1=1.0 / 64, scalar2=None, op0=ALU.mult),
                     reads=[t_ysum], writes=[t_mean])
                yield
                rstd, t_rstd = sm["rstd"][s]
                TT("dve", rstd, mean, mean, ALU.mult, [t_mean], [t_rstd])
                yield
                k.op("dve", lambda e: e.scalar_tensor_tensor(out=rstd, in0=yss, scalar=1.0 / 64, in1=rstd, op0=ALU.mult, op1=ALU.subtract),
                     reads=[t_yss, t_rstd], writes=[t_rstd])
                yield
                k.op("dve", lambda e: e.tensor_scalar(out=rstd, in0=rstd, scalar1=64e-5, scalar2=None, op0=ALU.add),
                     reads=[t_rstd], writes=[t_rstd])
                yield
                k.op("act", lambda e: e.sqrt(rstd, rstd), reads=[t_rstd], writes=[t_rstd])
                yield
                k.op("dve", lambda e: e.reciprocal(rstd, rstd), reads=[t_rstd], writes=[t_rstd])
                yield
                TT("dve", h8(Ysb), h8(Ysb), bc8(mean), ALU.subtract, [t_Y, t_mean], [t_Y])
                yield
                TT("dve", h8(Ysb), h8(Ysb), bc8(rstd), ALU.mult, [t_Y, t_rstd], [t_Y])
                yield
                TT("dve", Ysb, Ysb, bcs["gng"], ALU.mult, [t_Y, t_bc], [t_Y])
                yield
                TT("dve", Ysb, Ysb, bcs["gnb"], ALU.add, [t_Y, t_bc], [t_Y])
                yield
                TT("dve", h8(tmp), h8(v_), bc8(bs8), ALU.mult, [t_v, t_bs8], [t_tmp])
                yield
                TT("dve", Ysb, Ysb, tmp, ALU.add, [t_Y, t_tmp], [t_Y])
                yield
                y4, t_y4 = tb_["y4"][s]
                TT("dve", y4, Ysb, g_, ALU.mult, [t_Y, t_g_], [t_y4])
                yield
                if j == 0:
                    st = tm["eLC"][0][0]
                    k.op("dve", lambda e: e.tensor_copy(st, y4), reads=[t_y4], writes=[tm["eLC"][0][1]])
                    yield
                    dump("rwkv0", st, tm["eLC"][0][1])
                    yield
                b, tbk = bankB()
                for c in range(4):
                    k.op("pe", lambda e, c=c: e.transpose(psb(b)[:, c * 128:(c + 1) * 128], y4[:, c * 128:(c + 1) * 128], ident_b),
                         reads=[t_y4, t_const2], writes=[tbk], inc=(c == 3))
                    yield
                k.op("act", lambda e: e.activation(out=rwkvT[:, :, j * 128:(j + 1) * 128],
                                                   in_=psb(b)[:, 0:512].rearrange("p (a b) -> p a b", a=4), func=AF.Copy),
                     reads=[tbk], writes=[t_rwT[j]])
                yield
            def units(j, pump):
                s = j % NBUF
                aT, t_aT = tb_['aT'][s]
                bT, t_bT = tb_['bT'][s]
                kT_, t_kT = tb_['kT'][s]
                rT, t_rT = tb_['rT'][s]
                bh, t_bh = tb_['bh'][s]
                kh, t_kh = tb_['kh'][s]
                vb, t_vb = rkv[:, j, 1024:1536], t_rkv
                GDb, t_GDb = tb_['GDb'][s]
                GDl, t_GDl = tb_['GDl'][s]
                YB1 = 7
                yb = YB1
                t_yb = pst[yb]
                for grp in range(NGRP):
                    HPG = 8 // NGRP
                    heads = [HPG * grp + i_ for i_ in range(HPG)]
                    UH = [(ub[i_], heads[i_], slice(heads[i_] * 64, heads[i_] * 64 + 64)) for i_ in range(HPG)]
                    pump()
                    for U, h, hs in UH:
                        FM, t_FM = U["FM"]
                        b, tbk = bank()
                        for i, (src, tsrc) in enumerate(((aT, t_aT), (rT, t_rT), (bT, t_bT), (kT_, t_kT))):
                            k.op("pe", lambda e, i=i, src=src: e.transpose(psb(b)[0:64, i * 128:(i + 1) * 128], src[:, hs], ident_b),
                                 reads=[tsrc, t_const2], writes=[tbk], inc=(i == 3))
                        CP(h % 2 == 0 or CPACT >= 3, FM, psb(b)[0:64, 0:512], [tbk], [t_FM])
                    pump()
                    for U, h, hs in UH:
                        FM, t_FM = U["FM"]
                        b, tbk = bank()
                        k.op("pe", lambda e: e.matmul(psf(b)[:, 0:256], FM[:, 256:384], FM[:, 0:256], start=True, stop=True),
                             reads=[t_FM], writes=[tbk], inc=False)
                        k.op("pe", lambda e: e.matmul(psf(b)[:, 256:512], FM[:, 384:512], FM[:, 0:256], start=True, stop=True),
                             reads=[t_FM], writes=[tbk])
                        NAKA, t_NAKA = U["NAKA"]
                        k.op("dve", lambda e: e.tensor_tensor(
                            out=NAKA.rearrange("p (r a b) -> p r a b", r=2, a=2), in0=psf(b).rearrange("p (r a b) -> p r a b", r=2, a=2),
                            in1=masks_f[:, 0:2, :].unsqueeze(1).to_broadcast([128, 2, 2, 128]), op=ALU.mult),
                            reads=[tbk, t_masks], writes=[t_NAKA])
                    for U, h, hs in UH:
                        FM, t_FM = U["FM"]
                        NAKA, t_NAKA = U["NAKA"]
                        b, tbk = bank()
                        k.op("pe", lambda e: e.matmul(psf(b)[:, 0:128], FM[:, 0:128], FM[:, 256:384], start=True, stop=True),
                             reads=[t_FM], writes=[tbk])
                        bx, tbx = bank()
                        k.op("pe", lambda e: e.matmul(psf(bx)[:, 128:192], NAKA[:, 256:384], vb[:, hs], start=True, stop=True),
                             reads=[t_NAKA, t_vb], writes=[tbx])
                        NN0, t_NN0 = U["NN"][0]
                        TT("dve", NN0[:, 0:128], psf(b)[:, 0:128], masks_f[:, 2, :], ALU.mult, [tbk, t_masks], [t_NN0])
                        T0, t_T0 = U["T"][0]
                        TT("dve", T0, NAKA[:, 0:128], ident_b, ALU.add, [t_NAKA, t_const2], [t_T0])
                        X, t_X = U["X"]
                        CP(h % 2 == 1 or CPACT >= 4, X, psf(bx)[:, 128:192], [tbx], [t_X])
                    if STOP == 'C2':
                        raise _Stop()
                    pump()
                    for lv in range(1, 6):
                        bks = []
                        for U, h, hs in UH:
                            NNp, t_NNp = U["NN"][(lv - 1) % 2]
                            NNc, t_NNc = U["NN"][lv % 2]
                            NTprev = NNp[:, 0:128]
                            if lv == 1:
                                Nprev, rdp = U["NAKA"][0][:, 0:128], [t_NNp, U["NAKA"][1]]
                            else:
                                Nprev, rdp = NNp[:, 128:256], [t_NNp]
                            b, tbk = bank()
                            bks.append((b, tbk))
                            k.op("pe", lambda e: e.matmul(psf(b)[:, 0:128], Nprev, NTprev, start=True, stop=True),
                                 reads=rdp, writes=[tbk], inc=(lv == 5))
                            if lv < 5:
                                k.op("pe", lambda e: e.matmul(psf(b)[:, 128:256], NTprev, Nprev, start=True, stop=True),
                                     reads=rdp, writes=[tbk])
                                ACT(NNc, psf(b)[:, 0:256], AF.Copy, [tbk], [t_NNc])
                            else:
                                ACT(NNc[:, 0:128], psf(b)[:, 0:128], AF.Copy, [tbk], [t_NNc])
                        for (U, h, hs), (b, tbk) in zip(UH, bks):
                            NNc, t_NNc = U["NN"][lv % 2]
                            Tp, t_Tp = U["T"][(lv - 1) % 2]
                            Tc, t_Tc = U["T"][lv % 2]
                            k.op("pe", lambda e: e.matmul(psf(b)[:, 256:384], NNc[:, 0:128], Tp, start=True, stop=True),
                                 reads=[t_NNc, t_Tp], writes=[tbk])
                            TT("dve", Tc, psf(b)[:, 256:384], Tp, ALU.add, [tbk, t_Tp], [t_Tc])
                    pump()
                    for U, h, hs in UH:
                        T5, t_T5 = U["T"][1]
                        X, t_X = U["X"]
                        DU, t_DU = U["DU"]
                        b, tbk = bank()
                        k.op("pe", lambda e: e.matmul(psf(b)[:, 0:64], T5, aT[:, hs], start=True, stop=True),
                             reads=[t_T5, t_aT], writes=[tbk], inc=False)
                        k.op("pe", lambda e: e.matmul(psf(b)[:, 64:128], T5, X, start=True, stop=True),
                             reads=[t_T5, t_X], writes=[tbk])
                        CP(h % 2 == 0 or CPACT >= 1, DU, psf(b)[:, 0:128], [tbk], [t_DU])
                    if STOP == 'C3':
                        raise _Stop()
                    pump()
                    for U, h, hs in UH:
                        DU, t_DU = U["DU"]
                        MB, t_MB = U["MB"]
                        for i in range(2):
                            b, tbk = bank()
                            rs = slice(64 * i, 64 * i + 64)
                            mms = [(0, DU[rs, 0:64], bh[rs, hs], [t_DU, t_bh]),
                                   (0, ident_b[rs, rs], GDb[rs, hs], [t_const2, t_GDb]),
                                   (0, ident_b[rs, rs], GDl[rs, hs], [t_const2, t_GDl]),
                                   (64, bh[rs, hs], DU[rs, 64:128], [t_DU, t_bh]),
                                   (64, kh[rs, hs], vb[rs, hs], [t_kh, t_vb])]
                            for n_, (co, l_, r_2, rd) in enumerate(mms):
                                first = (n_ == 0) or (mms[n_ - 1][0] != co)
                                lastg = (n_ == len(mms) - 1) or (mms[n_ + 1][0] != co)
                                k.op("pe", lambda e: e.matmul(psf(b)[0:64, co:co + 64], l_, r_2, start=first, stop=lastg),
                                     reads=rd, writes=[tbk], inc=(n_ == len(mms) - 1))
                            CP(i == 0 or CPACT >= 2, MB[:, i * 128:(i + 1) * 128], psf(b)[0:64, 0:128], [tbk], [t_MB])
                    if STOP == 'C4':
                        raise _Stop()
                    for U, h, hs in UH:
                        FM, t_FM = U["FM"]
                        DU, t_DU = U["DU"]
                        NAKA, t_NAKA = U["NAKA"]
                        RQ, t_RQ = U["RQ"]
                        b, tbk = bank()
                        k.op("pe", lambda e: e.matmul(psf(b)[0:64, 0:128], DU[:, 0:64], NAKA[:, 128:256], start=True, stop=True),
                             reads=[t_DU, t_NAKA], writes=[tbk])
                        k.op("dve", lambda e: e.tensor_tensor(
                            out=RQ.rearrange("p a b -> p (a b)")[:, 0:256].rearrange("p (a b) -> p a b", a=2)[:, :, 0:64] if False else RQ[:, 0, 0:64],
                            in0=psf(b)[0:64, 0:64], in1=FM[:, 128:192], op=ALU.add), reads=[tbk, t_FM], writes=[t_RQ])
                        k.op("dve", lambda e: e.tensor_tensor(out=RQ[:, 1, 64:128], in0=psf(b)[0:64, 64:128], in1=FM[:, 192:256], op=ALU.add),
                             reads=[tbk, t_FM], writes=[t_RQ])
                    pump()
                    for i in range(2):
                        for U, h, hs in UH:
                            MB, t_MB = U["MB"]
                            Sc, t_Sc = Sst[h][s_cur[h]]
                            Sn, t_Sn = Sst[h][1 - s_cur[h]]
                            Sbi, t_Sbi = Sb[h][i]
                            k.op("act", lambda e: e.activation(out=Sbi, in_=Sc, func=AF.Copy), reads=[t_Sc], writes=[t_Sbi])
                            b, tbk = bank()
                            k.op("pe", lambda e: e.matmul(psf(b)[0:64, 0:64], MB[:, i * 128:i * 128 + 64], Sc, start=True, stop=True),
                                 reads=[t_MB, t_Sc], writes=[tbk])
                            TT("dve", Sn, psf(b)[0:64, 0:64], MB[:, i * 128 + 64:i * 128 + 128], ALU.add, [tbk, t_MB], [t_Sn])
                            s_cur[h] = 1 - s_cur[h]
                    pump()
                    for U, h, hs in UH:
                        DU, t_DU = U["DU"]
                        NAKA, t_NAKA = U["NAKA"]
                        RQ, t_RQ = U["RQ"]
                        yo = psf(yb)[:, hs]
                        k.op("pe", lambda e: e.matmul(yo, RQ[:, 0, :], Sb[h][0][0], start=True, stop=False),
                             reads=[t_RQ, Sb[h][0][1]], writes=[t_yb], inc=False)
                        k.op("pe", lambda e: e.matmul(yo, RQ[:, 1, :], Sb[h][1][0], start=False, stop=False),
                             reads=[t_RQ, Sb[h][1][1]], writes=[t_yb], inc=False)
                        k.op("pe", lambda e: e.matmul(yo, NAKA[:, 128:256], DU[:, 64:128], start=False, stop=False),
                             reads=[t_NAKA, t_DU], writes=[t_yb], inc=False)
                        k.op("pe", lambda e: e.matmul(yo, NAKA[:, 384:512], vb[:, hs], start=False, stop=True),
                             reads=[t_NAKA, t_vb], writes=[t_yb])
                pump()
            def exhaust(g_):
                for _ in g_:
                    pass
            rot["n"] = 7
            exhaust(prep_gen(0))
            pend_post = None
            for j in range(NT):
                gens = []
                if pend_post is not None:
                    gens.append(pend_post)
                if j + 1 < NT:
                    gens.append(prep_gen(j + 1))
                def pump(n=PUMP):
                    for _ in range(n):
                        while gens:
                            try:
                                next(gens[0])
                                break
                            except StopIteration:
                                gens.pop(0)
                units(j, pump)
                for g_ in gens:
                    exhaust(g_)
                if STOP == 'C5':
                    raise _Stop()
                pend_post = post_gen(j)
                next(pend_post)
            exhaust(pend_post)
            del held[:]
            k.barrier()
            ar.reset(c_mark)

            if STOP == 'C':
                raise _Stop()
            attT = ar.get([128, 4, T], BF16)
            rot["n"] = 6
            b_mark = ar.mark()
            Wqk = ar.get([128, KC, 1024], BF16)
            Wv = ar.get([128, KC, 512], BF16)
            t_Wqkc = [Trk() for _ in range(8)]
            t_Wv = Trk()
            for c in range(8):
                k.dma("pool", Wqk[:, :, c * 128:(c + 1) * 128], w_in[:, c * 128:(c + 1) * 128].rearrange("(c p) n -> p c n", p=128),
                      "wqk%d" % c, writes=[t_Wqkc[c]])
            k.dma("pool", Wv, w_in[:, 1024:1536].rearrange("(c p) n -> p c n", p=128), "wv", writes=[t_Wv])
            biasT = ar.get([128, 5, 8, 128], F32)
            am = ar.get([128, 5, 128], F32)
            t_bias = Trk()
            t_am = Trk()
            k.dma("sp", biasT.rearrange("p a b c -> p (a b c)"), biasg, "bias", writes=[t_bias])
            k.dma("sp", am.rearrange("p a b -> p (a b)"), amask, "am", writes=[t_am])
            k.op("dve", lambda e: e.tensor_tensor(out=biasT, in0=biasT, in1=am.unsqueeze(2).to_broadcast([128, 5, 8, 128]), op=ALU.add),
                 reads=[t_bias, t_am], writes=[t_bias])
            qkT = ar.get([128, 8, T], BF16)
            t_qk = Trk()
            Vaug = ar.get([128, NT, 8, 65], BF16)
            t_V = Trk()
            k.op("dve", lambda e: e.memset(Vaug.rearrange("p a b c -> p (a b c)"), 1.0), writes=[t_V])
            for c in range(8):
                for tb in range(4):
                    b, tbk = bank()
                    for kc in range(KC):
                        k.op("pe", lambda e, kc=kc: e.matmul(psf(b), Wqk[:, kc, c * 128:(c + 1) * 128],
                                                             hT[:, kc, 1 + tb * 512:1 + (tb + 1) * 512],
                                                             start=(kc == 0), stop=(kc == KC - 1)),
                             reads=[t_Wqkc[c], t_hall], writes=[tbk], inc=(kc == KC - 1))
                    ACT(qkT[:, c, tb * 512:(tb + 1) * 512], psf(b), AF.Copy, [tbk], [t_qk])
            for j in range(NT):
                b, tbk = bank()
                for kc in range(KC):
                    k.op("pe", lambda e, kc=kc: e.matmul(psf(b), hT[:, kc, 1 + j * 128:1 + (j + 1) * 128], Wv[:, kc, :],
                                                         start=(kc == 0), stop=(kc == KC - 1)),
                         reads=[t_Wv, t_hall], writes=[tbk], inc=(kc == KC - 1))
                k.op("act", lambda e: e.activation(out=Vaug[:, j, :, 0:64], in_=h8(psf(b)), func=AF.Copy), reads=[tbk], writes=[t_V])
            rden_b = [(ar.get([128, 4], F32), Trk()) for _ in range(2)]
            atok = [(ar.get([128, 512], BF16), Trk()) for _ in range(2)]
            items_att = []
            for j in range(NT):
                o_first = max(0, 4 - j)
                for o in range(o_first, 5):
                    items_att.append((j, o, o_first))
            sc_b = [[(ar.get([128, 512], F32), Trk()) for _ in range(2)] for _ in range(2)]
            pt_b = [[(ar.get([128, 512], BF16), Trk()) for _ in range(3)] for _ in range(2)]
            att_bk = {}

            def att_S(idx):
                j, o, o_first = items_att[idx]
                kt = j - 4 + o
                bE, tE = bank()
                bO, tO = bank()
                att_bk[idx] = None
                for hh in range(4):
                    for half, (b, tbk) in enumerate(((bE, tE), (bO, tO))):
                        h = 2 * hh + half
                        pr = slice(64 * half, 64 * half + 64)
                        k.op("pe", lambda e, hh=hh, h=h, pr=pr, b=b: e.matmul(
                            psf(b)[:, hh * 128:(hh + 1) * 128], qkT[pr, 4 + h // 2, kt * 128:(kt + 1) * 128],
                            qkT[pr, h // 2, j * 128:(j + 1) * 128], start=True, stop=True),
                            reads=[t_qk], writes=[tbk], inc=(hh == 3))
                for half, (b, tbk) in enumerate(((bE, tE), (bO, tO))):
                    sc, t_sc = sc_b[half][idx % 2]
                    pt, t_pt = pt_b[half][idx % 3]
                    k.op("dve", lambda e, b=b, half=half, sc=sc: e.scalar_tensor_tensor(
                        out=sc.rearrange("p (a b) -> p a b", a=4), in0=psf(b).rearrange("p (a b) -> p a b", a=4), scalar=0.125,
                        in1=biasT[:, o, half::2, :],
                        op0=ALU.mult, op1=ALU.add), reads=[tbk, t_bias], writes=[t_sc])
                    ACT(pt, sc, AF.Exp, [t_sc], [t_pt])

            def att_P(idx):
                j, o, o_first = items_att[idx]
                kt = j - 4 + o
                at, t_at = atok[j % 2]
                for half in range(2):
                    pt, t_pt = pt_b[half][idx % 3]
                    ob = YB[half]
                    t_ob = pst[ob]
                    for hh in range(4):
                        h = 2 * hh + half
                        k.op("pe", lambda e, hh=hh, h=h, ob=ob, pt=pt: e.matmul(
                            psf(ob)[:, hh * 65:(hh + 1) * 65], pt[:, hh * 128:(hh + 1) * 128], Vaug[:, kt, h, :],
                            start=(o == o_first and hh == 0), stop=(o == 4), skip_group_check=True), reads=[t_pt, t_V], writes=[t_ob],
                            inc=(o == 4 and hh == 3))
                if o != 4:
                    return
                for half in range(2):
                    ob = YB[half]
                    t_ob = pst[ob]
                    rden, t_rden = rden_b[half]
                    ov = psf(ob)[:, 0:260].rearrange("p (a b) -> p a b", a=4)
                    k.op("dve", lambda e, ov=ov, rden=rden: e.reciprocal(rden, ov[:, :, 64]), reads=[t_ob], writes=[t_rden])
                    k.op("dve", lambda e, ov=ov, rden=rden, half=half: e.tensor_tensor(
                        out=at.rearrange("p (a b d) -> p a b d", a=4, b=2)[:, :, half, :], in0=ov[:, :, 0:64],
                        in1=rden.unsqueeze(2).to_broadcast([128, 4, 64]), op=ALU.mult),
                        reads=[t_ob, t_rden], writes=[t_at])
                if j == 5:
                    stt = sc_b[0][0][0]
                    k.op("dve", lambda e: e.tensor_copy(stt, at), reads=[t_at], writes=[sc_b[0][0][1]])
                    dump("att5", stt, sc_b[0][0][1])
                b, tbk = bank()
                for c in range(4):
                    k.op("pe", lambda e, c=c: e.transpose(psb(b)[:, c * 128:(c + 1) * 128], at[:, c * 128:(c + 1) * 128], ident_b),
                         reads=[t_at, t_const2], writes=[tbk], inc=(c == 3))
                k.op("act", lambda e: e.activation(out=attT[:, :, j * 128:(j + 1) * 128],
                                                   in_=psb(b)[:, 0:512].rearrange("p (a b) -> p a b", a=4), func=AF.Copy),
                     reads=[tbk], writes=[t_V])

            n_att = len(items_att)
            SK = 2
            for step in range(n_att + SK):
                if step < n_att:
                    att_S(step)
                if step >= SK:
                    att_P(step - SK)
            k.barrier()
            ar.reset(b_mark)

            if STOP == 'B':
                raise _Stop()
            md_mark = ar.mark()
            mergedT = ar.get([128, KC, T], BF16)
            t_mg = Trk()
            Wo = ar.get([128, KC, D], BF16)
            t_Wo = Trk()
            d_mark = ar.mark()
            Wg = ar.get([128, KC, 2048], BF16)
            Wba = ar.get([128, 4, D], BF16)
            Wbr = ar.get([128, 4, D], BF16)
            t_WgA = [Trk() for _ in range(8)]
            t_WgB = [Trk() for _ in range(8)]
            t_Wb = Trk()
            t_Wb2 = Trk()
            k.dma("pool", Wba, w_branch_att.rearrange("(c p) n -> p c n", p=128), "wba", writes=[t_Wb])
            k.dma("pool", Wbr, w_branch_rwkv.rearrange("(c p) n -> p c n", p=128), "wbr", writes=[t_Wb2])
            for m in range(8):
                for off in (0, 1024):
                    cs = slice(off + m * 128, off + (m + 1) * 128)
                    k.dma("pool", Wg[:, :, cs], w_in[:, 3328 + cs.start:3328 + cs.stop].rearrange("(c p) n -> p c n", p=128),
                          "wg%d_%d" % (m, off // 1024), writes=[t_WgA[m] if off == 0 else t_WgB[m]])
            k.dma("pool", Wo, w_out.rearrange("(c p) n -> p c n", p=128), "wo", writes=[t_Wo])
            ga_b = [(ar.get([128, 512], F32), Trk()) for _ in range(2)]
            gb_b = [(ar.get([128, 512], F32), Trk()) for _ in range(2)]
            t_all = Trk()
            it = 0
            rot["n"] = 8
            for tb in range(4):
                cols = slice(tb * 512, (tb + 1) * 512)
                hcols = slice(1 + tb * 512, 1 + (tb + 1) * 512)
                for m in range(8):
                    bA, tA = bank()
                    for c in range(4):
                        k.op("pe", lambda e, c=c: e.matmul(psf(bA), Wba[:, c, m * 128:(m + 1) * 128], attT[:, c, cols],
                                                           start=(c == 0), stop=(c == 3)), reads=[t_Wb, t_all], writes=[tA], inc=(c == 3))
                    bB, tB = bank()
                    for c in range(4):
                        k.op("pe", lambda e, c=c: e.matmul(psf(bB), Wbr[:, c, m * 128:(m + 1) * 128], rwkvT[:, c, cols],
                                                           start=(c == 0), stop=(c == 3)), reads=[t_Wb2, t_all], writes=[tB], inc=(c == 3))
                    bGA, tGA = bank()
                    for c in range(KC):
                        k.op("pe", lambda e, c=c: e.matmul(psf(bGA), Wg[:, c, m * 128:(m + 1) * 128], hT[:, c, hcols],
                                                           start=(c == 0), stop=(c == KC - 1)), reads=[t_WgA[m], t_all], writes=[tGA], inc=(c == KC - 1))
                    bGB, tGB = bank()
                    for c in range(KC):
                        k.op("pe", lambda e, c=c: e.matmul(psf(bGB), Wg[:, c, 1024 + m * 128:1024 + (m + 1) * 128], hT[:, c, hcols],
                                                           start=(c == 0), stop=(c == KC - 1)), reads=[t_WgB[m], t_all], writes=[tGB], inc=(c == KC - 1))
                    ga, t_ga = ga_b[it % 2]
                    gb, t_gb = gb_b[it % 2]
                    it += 1
                    ACT(ga, psf(bGA), AF.Sigmoid, [tGA], [t_ga])
                    ACT(gb, psf(bGB), AF.Sigmoid, [tGB], [t_gb])
                    TT("dve", ga, ga, psf(bA), ALU.mult, [t_ga, tA], [t_ga])
                    TT("dve", gb, gb, psf(bB), ALU.mult, [t_gb, tB], [t_gb])
                    TT("dve", mergedT[:, m, cols], ga, gb, ALU.add, [t_ga, t_gb], [t_mg])
            if "merged" in dbg:
                st = ga_b[0][0]
                k.op("dve", lambda e: e.tensor_copy(st, mergedT[:, 0, 0:512]), reads=[t_mg], writes=[ga_b[0][1]])
                dump("merged", st, ga_b[0][1])
            k.barrier()
            ar.reset(d_mark)

            if STOP == 'D1':
                raise _Stop()
            ar.reset(h_mark)
            x2 = ar.get([128, NT, D], F32)
            assert ar.mark() <= md_mark, (ar.mark(), md_mark)
            x2_end = ar.mark()
            t_x2 = [Trk() for _ in range(NT)]
            ar.reset(d_mark)
            xs = [ar.get([128, D], F32) for _ in range(2)]
            for j in range(NT):
                s = j % 2
                k.dma("sp", xs[s], x[j * 128:(j + 1) * 128, :], "xs%d" % s, writes=[t_xs[s]])
                for half in range(2):
                    b, tbk = bank()
                    for m in range(KC):
                        k.op("pe", lambda e, m=m: e.matmul(psf(b), mergedT[:, m, j * 128:(j + 1) * 128], Wo[:, m, half * 512:(half + 1) * 512],
                                                           start=(m == 0), stop=(m == KC - 1)), reads=[t_Wo, t_all], writes=[tbk], inc=(m == KC - 1))
                    TT("dve", x2[:, j, half * 512:(half + 1) * 512], psf(b), xs[s][:, half * 512:(half + 1) * 512], ALU.add,
                       [tbk, t_xs[s]], [t_x2[j]])
            dump("x2", x2[:, 0, :], t_x2[0])
            k.barrier()
            ar.reset(x2_end)

            if STOP == 'D2':
                raise _Stop()
            h2T = ar.get([128, KC, T], BF16)
            t_h2 = [Trk() for _ in range(NT)]
            ssE = ar.get([128, NT], F32)
            t_ssE = [Trk() for _ in range(NT)]
            k.op("dve", lambda e: e.memset(ssE, 0.0), writes=t_ssE)
            Wr = ar.get([128, KC, 36], BF16)
            rbc = ar.get([128, 36], F32)
            lg = ar.get([128, NT, 36], F32)
            NG = NT * 4
            gmax = ar.get([128, NT], F32)
            gsh = ar.get([128, NT, 4], F32)
            gm = ar.get([128, NT, 4], F32)
            gsum = ar.get([128, NT], F32)
            el = ar.get([128, NG, 8], F32)
            m1 = ar.get([128, NG], F32)
            m2 = ar.get([128, NG], F32)
            e1 = ar.get([128, NG, 8], F32)
            e2 = ar.get([128, NG, 8], F32)
            w2_ = ar.get([128, NG], F32)
            comb = ar.get([128, NG, 8], F32)
            ex_mark = ar.mark()
            NS = 6
            GS = 3
            slots = []
            for s_ in range(NS):
                slots.append(dict(gu=ar.get([128, KC, 512], BF16), d=ar.get([128, 2, D], BF16),
                                  tg=Trk(), tu=Trk(), td=Trk()))
            sl_b = [(ar.get([128, 256], F32), Trk()) for _ in range(3)]
            av_b = [(ar.get([128, 256], BF16), Trk()) for _ in range(3)]
            avT_b = [(ar.get([128, 256], BF16), Trk()) for _ in range(3)]

            def load_expert(e_):
                sl = slots[e_ % NS]
                k.dma("pool", sl["gu"][:, :, 0:256], expert_w_gate[e_].rearrange("(c p) f -> p c f", p=128), "eg%d" % (e_ % NS),
                      writes=[sl["tg"]])
                k.dma("pool", sl["gu"][:, :, 256:512], expert_w_up[e_].rearrange("(c p) f -> p c f", p=128), "eu%d" % (e_ % NS),
                      writes=[sl["tu"]])
                k.dma("pool", sl["d"], expert_w_down[e_].rearrange("(c p) n -> p c n", p=128), "ed%d" % (e_ % NS),
                      writes=[sl["td"]])

            for e_ in range(NS):
                load_expert(e_)
            g2bc = ar.get([128, D], F32)
            k.dma("sp", g2bc, ln_ffn_g.partition_broadcast(128), "gbc", writes=[t_g])
            hb = [ar.get([128, D], BF16) for _ in range(2)]
            junk = ar.get([128, D], BF16)
            ost = [(ar.get([128, D], F32), Trk()) for _ in range(2)]
            ssF = ar.get([128, NT], F32)
            t_ssFl = [Trk() for _ in range(NT)]
            k.op("dve", lambda e: e.memset(ssF, 0.0), writes=t_ssFl)
            rot["n"] = 8
            for j in range(NT):
                s = j % 2
                rms_tile(x2[:, j, :], t_x2[j], g2bc, t_g, ssE, t_ssE[j], hb[s], t_hb[s], junk, t_junk, j, h2T, t_h2[j], j * 128)
            t_Wr = Trk()
            with nc.allow_non_contiguous_dma(reason="tiny router weights"):
                k.dma("pool", Wr[:, :, 0:4], router_group_w.rearrange("(c p) n -> p c n", p=128), "wr0", writes=[t_Wr])
                k.dma("pool", Wr[:, :, 4:36], router_expert_w.rearrange("(c p) n -> p c n", p=128), "wr1", writes=[t_Wr])
            t_rb = Trk()
            k.dma("sp", rbc[:, 0:4], router_group_b.partition_broadcast(128), "rb0", writes=[t_rb])
            k.dma("sp", rbc[:, 4:36], router_expert_b.partition_broadcast(128), "rb1", writes=[t_rb])
            t_lg = Trk()
            for j in range(NT):
                b, tbk = bank()
                for c in range(KC):
                    k.op("pe", lambda e, c=c: e.matmul(psf(b)[:, 0:36], h2T[:, c, j * 128:(j + 1) * 128], Wr[:, c, :],
                                                       start=(c == 0), stop=(c == KC - 1)), reads=[t_h2[j], t_Wr], writes=[tbk], inc=(c == KC - 1))
                TT("dve", lg[:, j, :], psf(b)[:, 0:36], rbc, ALU.add, [tbk, t_rb], [t_lg])
            gl = lg[:, :, 0:4]
            t_r = Trk()
            R = [t_lg, t_r]

            def V(fn):
                k.op("dve", fn, reads=R, writes=[t_r])

            def bcg(ap):
                return ap.unsqueeze(2).to_broadcast([128, NT, 4])

            def bce(ap):
                return ap.unsqueeze(2).to_broadcast([128, NG, 8])

            V(lambda e: e.tensor_copy(el.rearrange("p (a g) x -> p a (g x)", a=NT), lg[:, :, 4:36]))
            V(lambda e: e.tensor_reduce(out=gmax, in_=gl, axis=AX.X, op=ALU.max))
            V(lambda e: e.tensor_tensor(out=gsh, in0=gl, in1=bcg(gmax), op=ALU.subtract))
            V(lambda e: e.tensor_tensor(out=gm, in0=gl, in1=bcg(gmax), op=ALU.is_equal))
            k.op("act", lambda e: e.activation(out=gsh, in_=gsh, func=AF.Exp), reads=R, writes=[t_r])
            V(lambda e: e.tensor_reduce(out=gsum, in_=gsh, axis=AX.X, op=ALU.add))
            V(lambda e: e.reciprocal(gsum, gsum))
            V(lambda e: e.tensor_tensor(out=gm, in0=gm, in1=bcg(gsum), op=ALU.mult))
            V(lambda e: e.tensor_reduce(out=m1, in_=el, axis=AX.X, op=ALU.max))
            V(lambda e: e.tensor_tensor(out=e1, in0=el, in1=bce(m1), op=ALU.is_equal))
            V(lambda e: e.scalar_tensor_tensor(out=e2, in0=e1, scalar=NEG, in1=el, op0=ALU.mult, op1=ALU.add))
            V(lambda e: e.tensor_reduce(out=m2, in_=e2, axis=AX.X, op=ALU.max))
            V(lambda e: e.tensor_tensor(out=e1, in0=el, in1=bce(m2), op=ALU.is_ge))
            V(lambda e: e.tensor_tensor(out=e2, in0=el, in1=bce(m1), op=ALU.subtract))
            k.op("act", lambda e: e.activation(out=e2, in_=e2, func=AF.Exp), reads=R, writes=[t_r])
            V(lambda e: e.tensor_tensor(out=e2, in0=e2, in1=e1, op=ALU.mult))
            V(lambda e: e.tensor_tensor(out=m2, in0=m2, in1=m1, op=ALU.subtract))
            k.op("act", lambda e: e.activation(out=m2, in_=m2, func=AF.Exp), reads=R, writes=[t_r])
            V(lambda e: e.tensor_scalar(out=m2, in0=m2, scalar1=1.0, scalar2=None, op0=ALU.add))
            V(lambda e: e.reciprocal(m2, m2))
            V(lambda e: e.tensor_tensor(out=w2_, in0=m2, in1=gm.rearrange("p a g -> p (a g)"), op=ALU.mult))
            V(lambda e: e.tensor_tensor(out=comb, in0=e2, in1=bce(w2_), op=ALU.mult))
            combv = comb.rearrange("p (a g) x -> p a (g x)", a=NT)
            dump("comb", combv[:, 0, :], t_r)
            k.barrier(skip=("eg", "eu", "ed"))
            t_h2a = Trk()
            gfbc = g2bc
            t_gf = Trk()
            k.dma("sp", gfbc, ln_final_g.partition_broadcast(128), "gbc", writes=[t_gf])

            def final_tile(j):
                o_, t_o = ost[j % 2]
                t_s = t_ssFl[j]
                k.op("act", lambda e: e.activation(out=junk, in_=x2[:, j, :], func=AF.Square, accum_out=ssF[:, j:j + 1]),
                     reads=[t_x2[j]], writes=[t_junk, t_s])
                k.op("dve", lambda e: e.tensor_scalar(out=ssF[:, j:j + 1], in0=ssF[:, j:j + 1], scalar1=1.0 / D, scalar2=1e-6,
                                                      op0=ALU.mult, op1=ALU.add), reads=[t_s], writes=[t_s])
                k.op("act", lambda e: e.sqrt(ssF[:, j:j + 1], ssF[:, j:j + 1]), reads=[t_s], writes=[t_s])
                k.op("dve", lambda e: e.reciprocal(ssF[:, j:j + 1], ssF[:, j:j + 1]), reads=[t_s], writes=[t_s])
                k.op("dve", lambda e: e.scalar_tensor_tensor(out=o_, in0=x2[:, j, :], scalar=ssF[:, j:j + 1], in1=gfbc,
                                                             op0=ALU.mult, op1=ALU.mult), reads=[t_x2[j], t_s, t_gf], writes=[t_o])
                k.dma("sp", out[j * 128:(j + 1) * 128, :], o_, "ost%d" % (j % 2), reads=[t_o])
            groups = []
            e0 = 0
            while e0 < 32:
                groups.append(list(range(e0, min(32, e0 + GS))))
                e0 += GS
            items = []
            for g in groups:
                for j in range(NT):
                    for ei, e_ in enumerate(g):
                        items.append((g, j, ei, e_))
            loaded = NS
            UB = [0, 1, 2]
            TBK = 3
            yb_of = lambda j: (4, 5) if j % 2 == 0 else (6, 7)
            st_ = {}

            def stage_U(idx):
                g, j, ei, e_ = items[idx]
                sl = slots[e_ % NS]
                b = UB[idx % 3]
                for c in range(KC):
                    k.op("pe", lambda e, c=c: e.matmul(psf(b), h2T[:, c, j * 128:(j + 1) * 128], sl["gu"][:, c, :],
                                                       start=(c == 0), stop=(c == KC - 1)),
                         reads=[t_h2a, sl["tg"], sl["tu"]], writes=[pst[b]], inc=(c == KC - 1))
                s_, t_s = sl_b[idx % 3]
                av, t_av = av_b[idx % 3]
                ACT(s_, psf(b)[:, 0:256], AF.Silu, [pst[b]], [t_s])
                k.op("dve", lambda e: e.scalar_tensor_tensor(out=av, in0=psf(b)[:, 256:512], scalar=combv[:, j, e_:e_ + 1], in1=s_,
                                                             op0=ALU.mult, op1=ALU.mult), reads=[pst[b], t_s, t_r], writes=[t_av])

            def stage_T(idx):
                av, t_av = av_b[idx % 3]
                avT, t_avT = avT_b[idx % 3]
                off = (idx % 2) * 256
                tt = st_.setdefault(("tt", idx % 2), Trk())
                for f in range(2):
                    k.op("pe", lambda e, f=f: e.transpose(psb(TBK)[:, off + f * 128:off + (f + 1) * 128], av[:, f * 128:(f + 1) * 128], ident_b),
                         reads=[t_av, t_const2], writes=[tt], inc=(f == 1))
                ACT(avT, psb(TBK)[:, off:off + 256], AF.Copy, [tt], [t_avT])

            def stage_D(idx):
                g, j, ei, e_ = items[idx]
                sl = slots[e_ % NS]
                avT, t_avT = avT_b[idx % 3]
                yb2 = yb_of(j)
                last = (ei == len(g) - 1)
                for f in range(2):
                    for half in range(2):
                        k.op("pe", lambda e, f=f, half=half: e.matmul(psf(yb2[half]), avT[:, f * 128:(f + 1) * 128],
                                                                        sl["d"][:, f, half * 512:(half + 1) * 512],
                                                                        start=(ei == 0 and f == 0), stop=(last and f == 1)),
                             reads=[t_avT, sl["td"]], writes=[pst[yb2[half]]], inc=(last and f == 1))
                if last:
                    for half in range(2):
                        TT("dve", x2[:, j, half * 512:(half + 1) * 512], x2[:, j, half * 512:(half + 1) * 512], psf(yb2[half]),
                           ALU.add, [pst[yb2[half]], t_x2[j]], [t_x2[j]])
                    if g is groups[-1]:
                        final_tile(j)

            nonlocal_loaded = [loaded]
            n_items = len(items)
            for step in range(n_items + 2):
                if step < n_items:
                    stage_U(step)
                if 1 <= step <= n_items:
                    stage_T(step - 1)
                if 2 <= step <= n_items + 1:
                    idx = step - 2
                    stage_D(idx)
                    g, j, ei, e_ = items[idx]
                    if j == NT - 1 and ei == len(g) - 1:
                        for _ in g:
                            if nonlocal_loaded[0] < 32:
                                load_expert(nonlocal_loaded[0])
                                nonlocal_loaded[0] += 1

            if STOP == 'E':
                raise _Stop()
            k.barrier()
        except _Stop:
            k.barrier()
    return nc


def _consts(att_rel_bias):
    ident = np.eye(128, dtype=np.float32)
    s = np.arange(128)[:, None]
    t = np.arange(128)[None, :]
    same = (s // 64) == (t // 64)
    Ms = (same & (s < t)).astype(np.float32)
    Mi = (same & (s <= t)).astype(np.float32)
    MsT = (same & (s > t)).astype(np.float32)
    Blk = same.astype(np.float32)
    masks = np.concatenate([Ms, Mi, MsT, Blk], axis=1)
    i2 = (np.arange(128)[:, None] % 64 == np.arange(64)[None, :]).astype(np.float32)
    i2rep = np.tile(i2, (1, 8))
    kl = np.arange(128)[:, None]
    ql = np.arange(128)[None, :]
    tab = np.asarray(att_rel_bias, dtype=np.float32)[0]
    biasg = np.zeros((128, 5, 8, 128), np.float32)
    am = np.zeros((128, 5, 128), np.float32)
    for o in range(5):
        bpos = 2 * o + kl // 64 - ql // 64
        kpos = bpos * 64 + kl % 64
        rel = 512 + (ql % 64) - kpos
        idx = np.clip(rel, -64, 64) + 64
        valid = (bpos >= 0) & (bpos <= 8)
        biasg[:, o, :, :] = np.transpose(tab[:, idx], (1, 0, 2))
        am[:, o, :] = np.where(valid, 0.0, NEG)
    return ident, masks, i2rep, biasg.reshape(128, -1), am.reshape(128, -1)


_DEBUG_SPEC = []


def kernel(**inputs):
    inp = {k_: np.asarray(v) for k_, v in inputs.items()}
    n = 8
    ident, masks, i2rep, biasg, am = _consts(inp["att_rel_bias"])
    nc = build_program(_DEBUG_SPEC)
    shared = {k_: np.ascontiguousarray(v, dtype=np.float32) for k_, v in inp.items() if k_ not in ("x", "att_rel_bias")}
    shared["rwkv_r_k"] = shared["rwkv_r_k"].reshape(1, 512)
    shared.update(c_ident=ident, c_masks=masks, c_i2=i2rep, biasg=biasg, amask=am)
    ncores = int(os.environ.get("MK_CORES", n))
    in_maps = []
    for b in range(ncores):
        m = dict(shared)
        m["x"] = np.ascontiguousarray(inp["x"][b], dtype=np.float32)
        in_maps.append(m)
    res = run_bass_kernel_spmd(nc, in_maps, core_ids=list(range(ncores)))
    if _DEBUG_SPEC:
        kernel.last = res.results
    outs = [r["out"] for r in res.results]
    while len(outs) < n:
        outs.append(np.zeros_like(outs[0]))
    return np.stack(outs, axis=0).astype(np.float32)
```
